# Optimizing a Trainium2 kernel written in Bass

```python
import math
import jax, jax.numpy as jnp
from jax import lax
import numpy as np

D_MODEL = 1024
BATCH = 4
SEQ = 4096
DEPTH = 4

D_BRANCH = 512
N_BRANCHES = 3
EPS = 1e-6
GMLP_CHUNK = 128
GMLP_GROUPS = 4
GMLP_GDIM = D_BRANCH // GMLP_GROUPS
GLA_HEADS = 4
GLA_DK = D_BRANCH // 2 // GLA_HEADS
GLA_DV = D_BRANCH // GLA_HEADS
GLA_GATE_RANK = 16
GLA_GATE_TAU = 16.0
GLA_CHUNK = 64
DSA_HEADS = 4
DSA_HDIM = D_BRANCH // DSA_HEADS
DSA_IDX_HEADS = 4
DSA_IDX_DIM = 64
DSA_QBLOCK = 128
DSA_TOPK_MAX = 256
N_BUCKETS = 32
MAX_DISTANCE = 128
N_GROUPS = 4
EXPERTS_PER_GROUP = 4
N_EXPERTS = N_GROUPS * EXPERTS_PER_GROUP
D_EXPERT = 256
TOPK_IN_GROUP = 2

IN_SIZES = (
    D_BRANCH, D_BRANCH,
    GLA_HEADS * GLA_DK, GLA_HEADS * GLA_DK,
    GLA_HEADS * GLA_DV, GLA_HEADS * GLA_DV,
    GLA_GATE_RANK,
    DSA_HEADS * DSA_HDIM, DSA_HEADS * DSA_HDIM, DSA_HEADS * DSA_HDIM,
    DSA_IDX_HEADS * DSA_IDX_DIM, DSA_IDX_DIM, DSA_IDX_HEADS,
    N_BRANCHES * D_MODEL,
)
N_IN = sum(IN_SIZES)

kernel_name = "hybrid_gmlp_gla_dsa_hier_moe"


def rms_norm(x, g):
    xf = x.astype(jnp.float32)
    y = xf * lax.rsqrt(jnp.mean(xf * xf, axis=-1, keepdims=True) + EPS)
    return (y * g.astype(jnp.float32)).astype(x.dtype)


def layer_norm(x, g, b):
    xf = x.astype(jnp.float32)
    mu = jnp.mean(xf, axis=-1, keepdims=True)
    var = jnp.mean(jnp.square(xf - mu), axis=-1, keepdims=True)
    y = (xf - mu) * lax.rsqrt(var + EPS)
    return (y * g.astype(jnp.float32) + b.astype(jnp.float32)).astype(x.dtype)


def t5_bucket(rel):
    n = jnp.maximum(rel, 0)
    max_exact = N_BUCKETS // 2
    large = max_exact + (
        jnp.log(jnp.maximum(n, max_exact).astype(jnp.float32) / max_exact)
        / math.log(MAX_DISTANCE / max_exact) * (N_BUCKETS - max_exact)
    ).astype(jnp.int32)
    large = jnp.minimum(large, N_BUCKETS - 1)
    return jnp.where(n < max_exact, n, large)


def gmlp_branch(u, v, ln_g, ln_b, w_s, b_s):
    bsz, s, _ = v.shape
    v = layer_norm(v, ln_g, ln_b)
    vc = v.reshape(bsz, s // GMLP_CHUNK, GMLP_CHUNK, GMLP_GROUPS, GMLP_GDIM)
    causal = jnp.tril(jnp.ones((GMLP_CHUNK, GMLP_CHUNK), dtype=bool))
    w = jnp.where(causal[None], w_s, jnp.zeros_like(w_s))
    mixed = jnp.einsum('gts,bcsgd->bctgd', w, vc) + b_s.T[:, :, None]
    return u * mixed.reshape(bsz, s, D_BRANCH)


def gla_scan(q, k, v, g):
    bsz, s, h, _ = q.shape
    out_dtype = v.dtype
    q = q.astype(jnp.float32) * (GLA_DK ** -0.5)

    def to_chunks(t):
        t = t.astype(jnp.float32)
        return t.reshape(bsz, s // GLA_CHUNK, GLA_CHUNK, h, t.shape[-1]).transpose(1, 0, 3, 2, 4)

    causal = jnp.tril(jnp.ones((GLA_CHUNK, GLA_CHUNK), dtype=bool))[None, None, :, :, None]

    def step(state, inp):
        qc, kc, vc, gc = inp
        b = jnp.cumsum(gc, axis=2)
        b_last = b[:, :, -1:, :]
        o_inter = jnp.einsum('bhtd,bhde->bhte', qc * jnp.exp(b), state)
        diff = b[:, :, :, None, :] - b[:, :, None, :, :]
        decay = jnp.where(causal, jnp.exp(jnp.minimum(diff, 0.0)), 0.0)
        attn = jnp.einsum('bhtd,bhsd,bhtsd->bhts', qc, kc, decay)
        o = o_inter + jnp.einsum('bhts,bhse->bhte', attn, vc)
        new_state = (jnp.exp(b_last[:, :, 0, :])[..., None] * state
                     + jnp.einsum('bhsd,bhse->bhde', kc * jnp.exp(b_last - b), vc))
        return new_state, o

    state0 = jnp.zeros((bsz, h, GLA_DK, GLA_DV), jnp.float32)
    _, o = lax.scan(step, state0, (to_chunks(q), to_chunks(k), to_chunks(v), to_chunks(g)))
    return o.transpose(1, 0, 3, 2, 4).reshape(bsz, s, h, GLA_DV).astype(out_dtype)


def dsa_attention(q, k, v, iq, ik, iw, rel_bias):
    bsz, s, h, hd = q.shape
    n_blocks = s // DSA_QBLOCK
    topk = min(DSA_TOPK_MAX, s // 4)
    key_pos = jnp.arange(s, dtype=jnp.int32)
    gather = jax.vmap(lambda arr, idx: arr[idx])

    def block(i):
        start = i * DSA_QBLOCK
        qb = lax.dynamic_slice_in_dim(q, start, DSA_QBLOCK, axis=1)
        iqb = lax.dynamic_slice_in_dim(iq, start, DSA_QBLOCK, axis=1)
        iwb = lax.dynamic_slice_in_dim(iw, start, DSA_QBLOCK, axis=1)
        qpos = start + jnp.arange(DSA_QBLOCK, dtype=jnp.int32)
        raw = jnp.einsum('bthd,bsd->bths', iqb, ik)
        score = jnp.einsum('bths,bth->bts', jax.nn.relu(raw), iwb).astype(jnp.float32)
        admissible = key_pos[None, :] <= qpos[:, None]
        score = jnp.where(admissible[None], score, -jnp.inf)
        _, top_idx = lax.top_k(score, topk)
        valid = top_idx <= qpos[None, :, None]
        kg = gather(k, top_idx)
        vg = gather(v, top_idx)
        logits = jnp.einsum('bthd,btkhd->btkh', qb, kg).astype(jnp.float32) * (hd ** -0.5)
        bias = rel_bias[t5_bucket(qpos[None, :, None] - top_idx)].astype(jnp.float32)
        logits = jnp.where(valid[..., None], logits + bias, -1e30)
        p = jax.nn.softmax(logits, axis=2).astype(v.dtype)
        return jnp.einsum('btkh,btkhd->bthd', p, vg)

    out = lax.map(block, jnp.arange(n_blocks))
    return out.transpose(1, 0, 2, 3, 4).reshape(bsz, s, h * hd)


def hybrid_mixer(h, w_in, gmlp_ln_g, gmlp_ln_b, gmlp_w_s, gmlp_b_s, gla_w_gate2, gla_b_gate,
                 gla_norm_g, dsa_qnorm_g, dsa_knorm_g, rel_bias, w_branch, b_branch_gate, w_out):
    bsz, s, _ = h.shape
    proj = h @ w_in
    splits = np.cumsum(IN_SIZES)[:-1].tolist()
    (a_u, a_v, g_q, g_k, g_v, g_r, g_a, d_q, d_k, d_v, d_iq, d_ik, d_iw, gates) = jnp.split(
        proj, splits, axis=-1)

    y_a = gmlp_branch(jax.nn.gelu(a_u), jax.nn.gelu(a_v), gmlp_ln_g, gmlp_ln_b, gmlp_w_s, gmlp_b_s)

    log_alpha = jax.nn.log_sigmoid((g_a @ gla_w_gate2 + gla_b_gate).astype(jnp.float32)) / GLA_GATE_TAU
    o_b = gla_scan(g_q.reshape(bsz, s, GLA_HEADS, GLA_DK), g_k.reshape(bsz, s, GLA_HEADS, GLA_DK),
                   g_v.reshape(bsz, s, GLA_HEADS, GLA_DV),
                   log_alpha.reshape(bsz, s, GLA_HEADS, GLA_DK))
    o_b = rms_norm(o_b, gla_norm_g) * jax.nn.silu(g_r.reshape(bsz, s, GLA_HEADS, GLA_DV))
    y_b = o_b.reshape(bsz, s, D_BRANCH)

    q = rms_norm(d_q.reshape(bsz, s, DSA_HEADS, DSA_HDIM), dsa_qnorm_g)
    k = rms_norm(d_k.reshape(bsz, s, DSA_HEADS, DSA_HDIM), dsa_knorm_g)
    y_c = dsa_attention(q, k, d_v.reshape(bsz, s, DSA_HEADS, DSA_HDIM),
                        d_iq.reshape(bsz, s, DSA_IDX_HEADS, DSA_IDX_DIM), d_ik, d_iw, rel_bias)

    ys = jnp.stack([y_a, y_b.astype(y_a.dtype), y_c.astype(y_a.dtype)], axis=0)
    up = jnp.einsum('nbse,ned->bsnd', ys, w_branch)
    gate = jax.nn.sigmoid(gates.reshape(bsz, s, N_BRANCHES, D_MODEL) + b_branch_gate)
    merged = jnp.sum(gate * up, axis=2)
    return (merged @ w_out).astype(h.dtype)


def hier_moe(h, w_group, b_group, w_router, b_router, w_exp_gate, w_exp_up, w_exp_down):
    bsz, s, d = h.shape
    hf = h.reshape(-1, d)
    grp_prob = jax.nn.softmax((hf @ w_group + b_group).astype(jnp.float32), axis=-1)
    p_g, g_idx = lax.top_k(grp_prob, 1)
    g_onehot = jax.nn.one_hot(g_idx[:, 0], N_GROUPS, dtype=jnp.float32)
    exp_logits = (hf @ w_router + b_router).astype(jnp.float32).reshape(-1, N_GROUPS, EXPERTS_PER_GROUP)
    sel_logits = jnp.einsum('tge,tg->te', exp_logits, g_onehot)
    top_vals, e_idx = lax.top_k(sel_logits, TOPK_IN_GROUP)
    weights = p_g * jax.nn.softmax(top_vals, axis=-1)
    eid = g_idx * EXPERTS_PER_GROUP + e_idx
    combine = jnp.einsum('tk,tke->te', weights,
                         jax.nn.one_hot(eid, N_EXPERTS, dtype=jnp.float32)).astype(hf.dtype)
    y = jnp.zeros_like(hf)
    for gi in range(N_GROUPS):
        sl = slice(gi * EXPERTS_PER_GROUP, (gi + 1) * EXPERTS_PER_GROUP)
        hg = jnp.einsum('td,edf->etf', hf, w_exp_gate[sl])
        hu = jnp.einsum('td,edf->etf', hf, w_exp_up[sl])
        act = jax.nn.silu(hg) * hu * combine[:, sl].T[:, :, None]
        y = y + jnp.einsum('etf,efd->td', act, w_exp_down[sl]).astype(hf.dtype)
    return y.reshape(bsz, s, d)


def setup_inputs(seed: int = 0) -> dict:
    key = jax.random.key(seed)
    ks = jax.random.split(key, 28)
    L = DEPTH

    def nrm(k, shape, scale):
        return jax.random.normal(k, shape, jnp.float32) * scale

    return {
        "x": nrm(ks[0], (BATCH, SEQ, D_MODEL), 1.0),
        "c": nrm(ks[1], (BATCH, D_MODEL), 1.0),
        "w_mod": nrm(ks[2], (L, D_MODEL, 6 * D_MODEL), 0.5 * D_MODEL ** -0.5),
        "b_mod": nrm(ks[3], (L, 6 * D_MODEL), 0.02),
        "g_norm1": 1.0 + nrm(ks[4], (L, D_MODEL), 0.1),
        "g_norm2": 1.0 + nrm(ks[5], (L, D_MODEL), 0.1),
        "w_in": nrm(ks[6], (L, D_MODEL, N_IN), D_MODEL ** -0.5),
        "gmlp_ln_g": 1.0 + nrm(ks[7], (L, D_BRANCH), 0.1),
        "gmlp_ln_b": nrm(ks[8], (L, D_BRANCH), 0.02),
        "gmlp_w_s": nrm(ks[9], (L, GMLP_GROUPS, GMLP_CHUNK, GMLP_CHUNK), GMLP_CHUNK ** -0.5),
        "gmlp_b_s": 1.0 + nrm(ks[10], (L, GMLP_GROUPS, GMLP_CHUNK), 0.1),
        "gla_w_gate2": nrm(ks[11], (L, GLA_GATE_RANK, GLA_HEADS * GLA_DK), GLA_GATE_RANK ** -0.5),
        "gla_b_gate": nrm(ks[12], (L, GLA_HEADS * GLA_DK), 0.1),
        "gla_norm_g": 1.0 + nrm(ks[13], (L, GLA_DV), 0.1),
        "dsa_qnorm_g": 1.0 + nrm(ks[14], (L, DSA_HDIM), 0.1),
        "dsa_knorm_g": 1.0 + nrm(ks[15], (L, DSA_HDIM), 0.1),
        "rel_bias": nrm(ks[16], (N_BUCKETS, DSA_HEADS), 0.5),
        "w_branch": nrm(ks[17], (L, N_BRANCHES, D_BRANCH, D_MODEL), D_BRANCH ** -0.5),
        "b_branch_gate": nrm(ks[18], (L, N_BRANCHES, D_MODEL), 0.1),
        "w_out": nrm(ks[19], (L, D_MODEL, D_MODEL), D_MODEL ** -0.5),
        "w_group": nrm(ks[20], (L, D_MODEL, N_GROUPS), D_MODEL ** -0.5),
        "b_group": nrm(ks[21], (L, N_GROUPS), 0.01),
        "w_router": nrm(ks[22], (L, D_MODEL, N_EXPERTS), D_MODEL ** -0.5),
        "b_router": nrm(ks[23], (L, N_EXPERTS), 0.01),
        "w_exp_gate": nrm(ks[24], (L, N_EXPERTS, D_MODEL, D_EXPERT), D_MODEL ** -0.5),
        "w_exp_up": nrm(ks[25], (L, N_EXPERTS, D_MODEL, D_EXPERT), D_MODEL ** -0.5),
        "w_exp_down": nrm(ks[26], (L, N_EXPERTS, D_EXPERT, D_MODEL), D_EXPERT ** -0.5),
    }


def reference(x, c, w_mod, b_mod, g_norm1, g_norm2, w_in, gmlp_ln_g, gmlp_ln_b, gmlp_w_s, gmlp_b_s,
              gla_w_gate2, gla_b_gate, gla_norm_g, dsa_qnorm_g, dsa_knorm_g, rel_bias, w_branch,
              b_branch_gate, w_out, w_group, b_group, w_router, b_router, w_exp_gate, w_exp_up,
              w_exp_down):
    c_act = jax.nn.silu(c)
    for l in range(DEPTH):
        mod = c_act @ w_mod[l] + b_mod[l]
        sh1, sc1, gt1, sh2, sc2, gt2 = [m[:, None, :] for m in jnp.split(mod, 6, axis=-1)]
        h = rms_norm(x, g_norm1[l]) * (1.0 + sc1) + sh1
        mix = hybrid_mixer(h, w_in[l], gmlp_ln_g[l], gmlp_ln_b[l], gmlp_w_s[l], gmlp_b_s[l],
                           gla_w_gate2[l], gla_b_gate[l], gla_norm_g[l], dsa_qnorm_g[l],
                           dsa_knorm_g[l], rel_bias, w_branch[l], b_branch_gate[l], w_out[l])
        x = (x + gt1 * mix).astype(x.dtype)
        h2 = rms_norm(x, g_norm2[l]) * (1.0 + sc2) + sh2
        ffn = hier_moe(h2, w_group[l], b_group[l], w_router[l], b_router[l],
                       w_exp_gate[l], w_exp_up[l], w_exp_down[l])
        x = (x + gt2 * ffn).astype(x.dtype)
    return x
```

```python
import numpy as np
import concourse.bass as bass
import concourse.mybir as mybir

F32 = mybir.dt.float32
BF16 = mybir.dt.bfloat16
AF = mybir.ActivationFunctionType
ALU = mybir.AluOpType
AX = mybir.AxisListType

GRAN = 512
SEM_CAP = 30000
STRICT_SAME_ENGINE = True


class Region:
    __slots__ = ("lw", "rd")

    def __init__(self):
        self.lw = None
        self.rd = {}


class View:
    __slots__ = ("buf", "ap")

    def __init__(self, buf, ap):
        self.buf = buf
        self.ap = ap

    def __getitem__(self, idx):
        return View(self.buf, self.ap[idx])

    def rearrange(self, s, **kw):
        return View(self.buf, self.ap.rearrange(s, **kw))

    def bitcast(self, dt):
        return View(self.buf, self.ap.bitcast(dt))

    def to_broadcast(self, shape):
        return View(self.buf, self.ap.to_broadcast(shape))

    def partition_broadcast(self, n):
        return View(self.buf, self.ap.partition_broadcast(n))


class Buf:
    def __init__(self, name, h, regions, is_dram=False):
        self.name = name
        self.h = h
        self.regions = regions
        self.is_dram = is_dram

    def __getitem__(self, idx):
        base = self.h.ap() if self.is_dram else self.h
        return View(self, base[idx])

    @property
    def v(self):
        base = self.h.ap() if self.is_dram else self.h[:]
        return View(self, base)

    def sub(self, i, n=1):
        return Buf(self.name, self.h, self.regions[i:i + n], self.is_dram)


class Op:
    __slots__ = ("eng", "fn", "kind", "deps", "lidx", "flag", "waits", "clock",
                 "fnum", "dma_sem", "dma_val", "dq_n")


class Prog:
    ENGS = ("pe", "act", "dve", "pool", "sp")

    def __init__(self, nc, n_dma_sems=None):
        self.nc = nc
        self.h = {"pe": nc.tensor, "act": nc.scalar, "dve": nc.vector,
                  "pool": nc.gpsimd, "sp": nc.sync}
        self.ops = []
        self.n_dma_sems = n_dma_sems or {"sp": 16, "pool": 16, "act": 4}
        self.dma_count = {e: 0 for e in self.ENGS}
        self.dma_ids = {e: [] for e in self.ENGS}
        self.arena_base = None
        self.arena_regions = None
        self.psum_rot = []
        self.psum_i = 0

    def sb(self, name, shape, dtype):
        h = self.nc.alloc_sbuf_tensor(name, list(shape), dtype)
        return Buf(name, h, [Region()])

    def make_arena(self, nbytes):
        r = self.nc.bump_sbuf(nbytes)
        self.arena_base = r[0]
        self.arena_size = nbytes
        self.arena_regions = [Region() for _ in range((nbytes + GRAN - 1) // GRAN)]

    def ar(self, name, shape, dtype, off):
        key = (name, tuple(shape), str(dtype), off)
        if not hasattr(self, "_arc"):
            self._arc = {}
        if key in self._arc:
            return self._arc[key]
        b = self._ar(f"{name}_{len(self._arc)}", shape, dtype, off)
        self._arc[key] = b
        return b

    def _ar(self, name, shape, dtype, off):
        esz = {F32: 4, BF16: 2}[dtype]
        n = int(np.prod(shape[1:])) * esz
        assert off % 32 == 0 and off + n <= self.arena_size, (name, off, n, self.arena_size)
        h = self.nc.alloc_sbuf_tensor_at(name, list(shape), dtype, offset=self.arena_base + off)
        regs = self.arena_regions[off // GRAN:(off + n + GRAN - 1) // GRAN]
        return Buf(name, h, regs)

    def ps(self, name, shape=(128, 512), dtype=F32):
        h = self.nc.alloc_psum_tensor(name, list(shape), dtype)
        return Buf(name, h, [Region()])

    def dram(self, name, shape, dtype, kind="Internal", nreg=1):
        h = self.nc.dram_tensor(name, list(shape), dtype, kind=kind)
        return Buf(name, h, [Region() for _ in range(nreg)], is_dram=True)

    def fence(self, eng, bufs):
        h = self.h[eng]
        self.add(eng, lambda: h.nop(), bufs, [])

    def mark(self, name):
        if not getattr(self, "marks_on", False):
            return
        last = {}
        for oid in range(len(self.ops) - 1, -1, -1):
            o = self.ops[oid]
            if o.kind == "c" and o.eng != "sp" and o.eng not in last:
                last[o.eng] = oid
            if len(last) == 4:
                break
        h = self.h["sp"]
        oid = self.add("sp", lambda: h.nop(), [], [])
        for e, d in last.items():
            self.ops[oid].deps[d] = True
        if not hasattr(self, "marks"):
            self.marks = []
        self.marks.append(name)

    def next_ps(self):
        b = self.psum_rot[self.psum_i % len(self.psum_rot)]
        self.psum_i += 1
        return b

    def add(self, eng, fn, reads, writes, kind="c"):
        op = Op()
        op.eng = eng
        op.fn = fn
        op.kind = kind
        op.flag = False
        op.deps = {}
        oid = len(self.ops)
        rregs = []
        for b in reads:
            if b is None:
                continue
            b = b.buf if isinstance(b, View) else b
            rregs.extend(b.regions)
        wregs = []
        for b in writes:
            b = b.buf if isinstance(b, View) else b
            wregs.extend(b.regions)
        for r in rregs:
            if r.lw is not None:
                op.deps[r.lw] = True
        for r in wregs:
            if r.lw is not None:
                op.deps.setdefault(r.lw, False)
            for rid in r.rd.values():
                op.deps.setdefault(rid, False)
        op.deps.pop(oid, None)
        for r in rregs:
            key = eng if kind == "c" else ("dma", oid)
            r.rd[key] = oid
        for r in wregs:
            r.lw = oid
            r.rd = {}
        if kind == "dma":
            n = self.dma_count[eng]
            self.dma_count[eng] += 1
            op.dq_n = n
            self.dma_ids[eng].append(oid)
        self.ops.append(op)
        return oid

    @staticmethod
    def _a(x):
        return x.ap if isinstance(x, View) else x

    def mm(self, out, lhsT, rhs, start=True, stop=True, skip=False):
        nc = self.nc
        if skip:
            self.add("pe", lambda: nc.tensor.matmul(out.ap, lhsT.ap, rhs.ap, start=start, stop=stop,
                                                    skip_group_check=True), [lhsT, rhs], [out])
        else:
            self.add("pe", lambda: nc.tensor.matmul(out.ap, lhsT.ap, rhs.ap, start=start, stop=stop),
                     [lhsT, rhs], [out])

    def tr(self, out, in_, ident):
        nc = self.nc
        self.add("pe", lambda: nc.tensor.transpose(out.ap, in_.ap, ident.ap), [in_, ident], [out])

    def act(self, out, in_, func, bias=None, scale=1.0, accum=None):
        nc = self.nc
        a = self._a
        kw = {}
        if bias is not None:
            kw["bias"] = a(bias)
        if accum is not None:
            kw["accum_out"] = a(accum)
        rd = [in_] + [x for x in (bias, scale) if isinstance(x, View)]
        wr = [out] + ([accum] if accum is not None else [])
        self.add("act", lambda: nc.scalar.activation(out=out.ap, in_=in_.ap, func=func,
                                                      scale=a(scale), **kw), rd, wr)

    def _veng(self, eng):
        return self.nc.vector if eng == "dve" else self.nc.gpsimd

    def tt(self, out, in0, in1, op, eng="dve"):
        e = self._veng(eng)
        self.add(eng, lambda: e.tensor_tensor(out=out.ap, in0=in0.ap, in1=in1.ap, op=op),
                 [in0, in1], [out])

    def ts(self, out, in0, s1, s2, op0, op1=None, accum=None, eng="dve"):
        e = self._veng(eng)
        a = self._a
        kw = {}
        if op1 is not None:
            kw["op1"] = op1
        if accum is not None:
            kw["accum_out"] = a(accum)
        rd = [in0] + [x for x in (s1, s2) if isinstance(x, View)]
        wr = [out] + ([accum] if accum is not None else [])
        self.add(eng, lambda: e.tensor_scalar(out=out.ap, in0=in0.ap, scalar1=a(s1), scalar2=a(s2),
                                              op0=op0, **kw), rd, wr)

    def stt(self, out, in0, scalar, in1, op0, op1, eng="dve"):
        e = self._veng(eng)
        a = self._a
        rd = [in0, in1] + ([scalar] if isinstance(scalar, View) else [])
        self.add(eng, lambda: e.scalar_tensor_tensor(out=out.ap, in0=in0.ap, scalar=a(scalar),
                                                     in1=in1.ap, op0=op0, op1=op1), rd, [out])

    def copy(self, out, in_, eng="dve"):
        if eng == "act":
            nc = self.nc
            self.add("act", lambda: nc.scalar.copy(out=out.ap, in_=in_.ap), [in_], [out])
        else:
            e = self._veng(eng)
            self.add(eng, lambda: e.tensor_copy(out=out.ap, in_=in_.ap), [in_], [out])

    def memset(self, out, val, eng="dve"):
        e = self._veng(eng)
        self.add(eng, lambda: e.memset(out.ap, val), [], [out])

    def reduce(self, out, in_, op, eng="dve"):
        e = self._veng(eng)
        self.add(eng, lambda: e.tensor_reduce(out=out.ap, in_=in_.ap, axis=AX.X, op=op), [in_], [out])

    def recip(self, out, in_):
        nc = self.nc
        self.add("dve", lambda: nc.vector.reciprocal(out=out.ap, in_=in_.ap), [in_], [out])

    def dma(self, q, out, in_, **kw):
        h = self.h[q]
        self.add(q, lambda: h.dma_start(out=out.ap, in_=in_.ap, **kw), [in_], [out], kind="dma")

    def emit(self):
        nc = self.nc
        ops = self.ops
        known = {e: {} for e in self.ENGS}
        dma_known = {e: set() for e in self.ENGS}
        count = {e: 0 for e in self.ENGS}
        for oid, op in enumerate(ops):
            E = op.eng
            waits = []
            kn = known[E]
            if op.kind == "dma":
                ns = self.n_dma_sems[E]
                if op.dq_n >= ns:
                    prev = self.dma_ids[E][op.dq_n - ns]
                    if prev not in dma_known[E]:
                        waits.append(prev)
                        dma_known[E].add(prev)
            for d, raw in op.deps.items():
                p = ops[d]
                if p.kind == "dma":
                    if d in dma_known[E]:
                        continue
                    waits.append(d)
                    dma_known[E].add(d)
                else:
                    if p.eng == E and (E == "pe" or (not raw and not STRICT_SAME_ENGINE)):
                        continue
                    if kn.get(p.eng, 0) >= p.lidx:
                        continue
                    waits.append(d)
                    p.flag = True
                    for e2, v in p.clock.items():
                        if kn.get(e2, 0) < v:
                            kn[e2] = v
                    if kn.get(p.eng, 0) < p.lidx:
                        kn[p.eng] = p.lidx
            op.waits = waits
            count[E] += 1
            op.lidx = count[E]
            op.clock = dict(kn) if op.kind == "c" else None
        fcount = {e: 0 for e in self.ENGS}
        for op in ops:
            if op.kind == "c" and op.flag:
                fcount[op.eng] += 1
                op.fnum = fcount[op.eng]
        sems = {}
        for e in self.ENGS:
            n = (fcount[e] + SEM_CAP - 1) // SEM_CAP
            sems[e] = [nc.alloc_semaphore(f"s_{e}_{i}") for i in range(n)]
        dsems = {}
        for e in self.ENGS:
            if self.dma_count[e]:
                dsems[e] = [nc.alloc_semaphore(f"d_{e}_{i}")
                            for i in range(min(self.n_dma_sems[e], self.dma_count[e]))]
        for op in ops:
            if op.kind == "dma":
                ns = self.n_dma_sems[op.eng]
                op.dma_sem = dsems[op.eng][op.dq_n % ns]
                op.dma_val = 16 * (op.dq_n // ns + 1)
        nwaits = 0
        for op in ops:
            h = self.h[op.eng]
            for d in op.waits:
                p = ops[d]
                if p.kind == "dma":
                    h.wait_ge(p.dma_sem, p.dma_val)
                else:
                    n = p.fnum - 1
                    h.wait_ge(sems[p.eng][n // SEM_CAP], n % SEM_CAP + 1)
                nwaits += 1
            ins = op.fn()
            if op.kind == "dma":
                ins.then_inc(op.dma_sem, 16)
            elif op.flag:
                n = op.fnum - 1
                ins.then_inc(sems[op.eng][n // SEM_CAP], 1)
        self.stats = dict(n_ops=len(ops), n_waits=nwaits, flagged=dict(fcount),
                          per_eng=dict(count))
        return self.stats

    def final_wait(self, eng, opids):
        pass


from concourse.bass_utils import run_bass_kernel_spmd

D = 1024
EPS = 1e-6
N_IN = 7508
OFF = dict(u=0, v=512, gq=1024, gk=1280, gv=1536, gr=2048, ga=2560, dq=2576, dk=3088,
           dv=3600, diq=4112, dik=4368, diw=4432, gates=4436)
NEG = -30000.0
BIGNEG = -1.0e30
NBIS = 13
C_ID, C_TRI, C_ONES, C_NEGTRI, C_BKD, C_BKN, NCST = 0, 128, 256, 384, 512, 640, 768
PR_G1, PR_G2, PR_BMOD, PR_LNG, PR_LNB, PR_GNG, PR_GQ, PR_GK, PR_BBG, NPRM = 0, 8, 16, 64, 68, 72, 73, 74, 75, 99
RW_BS, RW_BG, RW_BR, NROW = 0, 512, 768, 788
WSM_GA, WSM_IQ, WSM_IK, WSM_IW, WSM_N = 0, 16, 272, 336, 340


def build_program(L, S, dbg=False, marks=False):
    NBLK = S // 128
    NG = S // 512
    nc = bass.Bass("TRN2", target_bir_lowering=False)
    P = Prog(nc)
    P.marks_on = marks
    EI = "ExternalInput"
    x_in = P.dram("x", [S, D], F32, kind=EI)
    ccol = P.dram("ccol", [128, 8], F32, kind=EI)
    cst = P.dram("cst", [128, NCST], F32, kind=EI)
    rbd = P.dram("rbrow", [1, 128], F32, kind=EI)
    w_mod = P.dram("w_mod", [L, D, 6 * D], F32, kind=EI)
    prm = P.dram("prm", [L, 128, NPRM], F32, kind=EI)
    prow = P.dram("prow", [L, 1, NROW], F32, kind=EI)
    w_in = P.dram("w_in", [L, D, N_IN], F32, kind=EI)
    wsT = P.dram("wsT", [L, 128, 4, 128], F32, kind=EI)
    w2 = P.dram("w2", [L, 16, 256], F32, kind=EI)
    w_br = P.dram("w_branch", [L, 3, 512, D], F32, kind=EI)
    w_out = P.dram("w_out", [L, D, D], F32, kind=EI)
    wr = P.dram("wr", [L, D, 20], F32, kind=EI)
    weg = P.dram("w_exp_gate", [L, 16, D, 256], F32, kind=EI)
    weu = P.dram("w_exp_up", [L, 16, D, 256], F32, kind=EI)
    wed = P.dram("w_exp_down", [L, 16, 256, D], F32, kind=EI)
    out = P.dram("out", [S, D], F32, kind="ExternalOutput", nreg=NBLK)
    dbgo = {}

    def dump(name, view, shape, dtype):
        if not dbg or name in dbgo:
            return
        t = P.dram("dbg_" + name, list(shape), dtype, kind="ExternalOutput")
        dbgo[name] = t
        P.dma("sp", t.v, view)

    cf = P.sb("cf", [128, 512], F32)
    ident4 = P.sb("ident4", [128, 512], BF16)
    identb = P.sb("identb", [128, 128], BF16)
    biasT = P.sb("biasT", [128, 2, 4, 128], BF16)
    kT_all = P.sb("kT_all", [128, 4, S], BF16)
    v_all = P.sb("v_all", [128, NBLK, 4, 130], BF16)
    ikT_all = P.sb("ikT_all", [64, S], BF16)
    xg = P.sb("xg", [128, 4, D], F32)
    hT = P.sb("hT", [128, 8, 512], BF16)
    ysT = P.sb("ysT", [128, 3, 4, 512], BF16)
    WB = [P.sb(f"wb{i}", [128, 8, 512], BF16) for i in range(3)]
    wsm = P.sb("wsm", [128, 8, WSM_N], BF16)
    prm_sb = P.sb("prm_sb", [128, NPRM], F32)
    modT = P.sb("modT", [128, 48], F32)
    sT = P.sb("sT", [128, 16], F32)
    WsTm = P.sb("WsTm", [128, 4, 128], BF16)
    Cg = P.sb("Cg", [128, 4, 128], F32)
    w2_sb = P.sb("w2_sb", [16, 256], BF16)
    bg_sb = P.sb("bg_sb", [1, 256], BF16)
    ones1 = P.sb("ones1", [1, 128], BF16)
    wr_sb = P.sb("wr_sb", [128, 8, 20], F32)
    rbias = P.sb("rbias", [128, 20], F32)
    cactT = P.sb("cactT", [128, 8], BF16)
    Sf = P.sb("Sf", [64, 4, 128], F32)
    Sb = P.sb("Sb", [64, 4, 128], BF16)
    stat = P.sb("stat", [128, 64], F32)
    combb = P.sb("combb", [128, 4, 16], BF16)
    fv = P.sb("fv", [128, NBIS], F32)
    offs = P.sb("offs", [128, NBIS], F32)
    PSR = [P.ps(f"psr{i}") for i in range(6)]
    psA = P.ps("psA")
    psB = P.ps("psB")
    P.psum_rot = PSR
    ARENA = max(46 * 1024, 23552 + 8 * S)
    P.make_arena(ARENA)

    identf = cf[:, C_ID:C_ID + 128]
    trif = cf[:, C_TRI:C_TRI + 128]
    onesf = cf[:, C_ONES:C_ONES + 128]
    negtri = cf[:, C_NEGTRI:C_NEGTRI + 128]

    def wload(dst, src):
        P.dma("pool", dst, src)

    class WStream:
        def __init__(self, bufs):
            self.free = list(bufs)
            self.sched = []
            self.issued = []
            self.nissued = 0

        def push(self, fn):
            self.sched.append(fn)

        def _pump(self):
            while self.free and self.nissued < len(self.sched):
                b = self.free.pop(0)
                self.sched[self.nissued](b)
                self.nissued += 1
                self.issued.append(b)

        def get(self):
            self._pump()
            assert self.issued, "weight schedule underflow / no free buffer"
            return self.issued.pop(0)

        def release(self, b):
            self.free.append(b)
            self._pump()

    ws_main = WStream(WB)

    def sched_kxn(src2d, c0, n):
        ws_main.push(lambda wb: wload(wb[:, :, 0:n],
                                      src2d.rearrange("(k p) n -> p k n", p=128)[:, :, c0:c0 + n]))

    def sched_layer_mod(l):
        for i in range(12):
            sched_kxn(w_mod[l], i * 512, 512)

    def sched_group(l):
        for nm in ("u", "v", "gq", "gv", "gr", "dq", "dk", "dv"):
            sched_kxn(w_in[l], OFF[nm], 512)
        for n in range(3):
            ws_main.push(lambda wb, n=n: wload(
                wb.v.rearrange("p a b -> p (a b)").rearrange("p (e d) -> p e d", e=4),
                w_br[l][n].rearrange("(e p) d -> p e d", p=128)))
            for half in range(2):
                sched_kxn(w_in[l], OFF["gates"] + (n * 2 + half) * 512, 512)
        for half in range(2):
            sched_kxn(w_out[l], half * 512, 512)
        for e in range(16):
            def f(wb, e=e):
                wload(wb[:, :, 0:256], weg[l][e].rearrange("(k p) n -> p k n", p=128))
                wload(wb[:, :, 256:512], weu[l][e].rearrange("(k p) n -> p k n", p=128))
            ws_main.push(f)

    def next_wb():
        return ws_main.get()

    def rel_wb(b):
        ws_main.release(b)

    P.dma("sp", cf.v, cst[:, 0:512])
    bk = P.ar("bk", [128, 256], F32, 2048)
    P.dma("sp", bk.v, cst[:, 512:768])
    for i4 in range(4):
        P.dma("pool", ident4[:, i4 * 128:(i4 + 1) * 128], cst[:, C_ID:C_ID + 128])
    P.dma("pool", identb.v, cst[:, C_ID:C_ID + 128])
    P.memset(ones1.v, 1.0)
    for it in range(NBIS):
        P.memset(fv[:, it:it + 1], 0.5 ** (it + 1))
    P.memset(v_all.v, 1.0)
    rbb = P.ar("rbb", [128, 128], F32, 0)
    bacc = P.ar("bacc", [128, 128], F32, 512)
    btmp = P.ar("btmp", [128, 128], F32, 1024)
    P.dma("sp", rbb.v, rbd.v.partition_broadcast(128))
    for h in range(4):
        for dist, cb in ((0, 0), (1, 128)):
            for b in range(32):
                P.ts(btmp.v, bk[:, cb:cb + 128], float(b), None, ALU.is_equal)
                if b == 0:
                    P.ts(bacc.v, btmp.v, rbb[:, b * 4 + h:b * 4 + h + 1], None, ALU.mult)
                else:
                    P.stt(bacc.v, btmp.v, rbb[:, b * 4 + h:b * 4 + h + 1], bacc.v, ALU.mult, ALU.add)
            P.ts(biasT[:, dist, h, :], bacc.v, rbb[:, 31 * 4 + h:31 * 4 + h + 1], None, ALU.subtract)

    def small_rstd(dst, src, inv_n):
        P.ts(dst, src, inv_n, EPS, ALU.mult, ALU.add)
        P.act(dst, dst, AF.Sqrt)
        P.recip(dst, dst)

    def norm_to_hT(scol, shcol, fp32_router):
        junk = P.ar("junk", [128, D], BF16, 0)
        ssq = stat[:, 0:4]
        rstd = stat[:, 4:8]
        for bi in range(4):
            P.act(junk.v, xg[:, bi, :], AF.Square, accum=stat[:, bi:bi + 1])
        small_rstd(rstd, ssq, 1.0 / D)
        if not fp32_router:
            xns = [P.ar(f"xn{i}", [128, D], BF16, 2048 + 2048 * i) for i in range(2)]
            for bi in range(4):
                xn = xns[bi % 2]
                P.ts(xn.v, xg[:, bi, :], stat[:, 4 + bi:5 + bi], None, ALU.mult)
                ps = P.next_ps()
                psb = ps.v.bitcast(BF16)
                for k in range(8):
                    P.tr(psb[:, k * 128:(k + 1) * 128], xn[:, k * 128:(k + 1) * 128], identb.v)
                for k in range(8):
                    o = hT[:, k, bi * 128:(bi + 1) * 128]
                    if k % 2 == 0:
                        P.act(o, psb[:, k * 128:(k + 1) * 128], AF.Identity,
                              bias=shcol[:, k:k + 1], scale=scol[:, k:k + 1])
                    else:
                        P.ts(o, psb[:, k * 128:(k + 1) * 128], scol[:, k:k + 1], shcol[:, k:k + 1],
                             ALU.mult, ALU.add)
        else:
            xn32 = P.ar("xn32", [128, D], F32, 2048)
            h32 = P.ar("h32", [128, 8, 128], F32, 6144)
            lg_all = P.ar("lg_all", [128, 4, 20], F32, 10240)
            for bi in range(4):
                P.ts(xn32.v, xg[:, bi, :], stat[:, 4 + bi:5 + bi], None, ALU.mult)
                pss = [P.next_ps(), P.next_ps()]
                for k in range(8):
                    P.mm(pss[k // 4][:, (k % 4) * 128:(k % 4 + 1) * 128],
                         xn32[:, k * 128:(k + 1) * 128], identf)
                for k in range(8):
                    src = pss[k // 4][:, (k % 4) * 128:(k % 4 + 1) * 128]
                    if k % 2 == 0:
                        P.act(h32[:, k, :], src, AF.Identity, bias=shcol[:, k:k + 1], scale=scol[:, k:k + 1])
                    else:
                        P.ts(h32[:, k, :], src, scol[:, k:k + 1], shcol[:, k:k + 1], ALU.mult, ALU.add)
                P.copy(hT[:, :, bi * 128:(bi + 1) * 128], h32.v, eng="pool")
                psr = P.next_ps()
                for k in range(8):
                    P.mm(psr[:, 0:20], h32[:, k, :], wr_sb[:, k, :], start=(k == 0), stop=(k == 7))
                P.tt(lg_all[:, bi, :], psr[:, 0:20], rbias.v, ALU.add)
            router_all()

    def proj_tok(wb, c0, n, bi):
        ps = P.next_ps()
        for k in range(8):
            P.mm(ps[:, 0:n], hT[:, k, bi * 128:(bi + 1) * 128], wb[:, k, c0:c0 + n],
                 start=(k == 0), stop=(k == 7))
        return ps

    def proj_feat(wb, c0, m):
        ps = P.next_ps()
        for k in range(8):
            P.mm(ps[0:m, :], wb[:, k, c0:c0 + m], hT[:, k, :], start=(k == 0), stop=(k == 7))
        return ps

    def router_all():
        BIG = 1.0e30
        o = [10240 + 320]

        def fld(name, n):
            b_ = P.ar("r_" + name, [128, 4, n], F32, o[0])
            o[0] += ((4 * n * 4 + 31) // 32) * 32
            return b_

        lg_all = P.ar("lg_all", [128, 4, 20], F32, 10240)
        gmax, gsum, pg, m1, m2, dd, e21, w1, w1p, w2p = [fld(n_, 1) for n_ in
                                                         ("gmax", "gsum", "pg", "m1", "m2", "dd", "e21", "w1", "w1p", "w2p")]
        d4, ge, oh, negm = [fld(n_, 4) for n_ in ("d4", "ge", "oh", "negm")]
        elm, oh1, elm2, oh2, cmb, cmb2 = [fld(n_, 16) for n_ in ("elm", "oh1", "elm2", "oh2", "cmb", "cmb2")]
        f2 = lambda t: t.v.rearrange("p b o -> p (b o)")
        bc = lambda t, n: t.v.to_broadcast([128, 4, n])
        lgg = lg_all[:, :, 0:4]
        lge = lg_all[:, :, 4:20]
        P.reduce(f2(gmax), lgg, ALU.max)
        P.tt(d4.v, lgg, bc(gmax, 4), ALU.subtract)
        P.act(ge.v, d4.v, AF.Exp)
        P.reduce(f2(gsum), ge.v, ALU.add)
        P.recip(pg.v, gsum.v)
        P.tt(oh.v, lgg, bc(gmax, 4), ALU.is_equal)
        P.ts(negm.v, oh.v, 1.0, BIG, ALU.subtract, ALU.mult)
        P.tt(elm.v.rearrange("p b (g e) -> p b g e", g=4), lge.rearrange("p b (g e) -> p b g e", g=4),
             negm.v.rearrange("p b (g o) -> p b g o", o=1).to_broadcast([128, 4, 4, 4]), ALU.add)
        P.reduce(f2(m1), elm.v, ALU.max)
        P.tt(oh1.v, elm.v, bc(m1, 16), ALU.is_equal)
        P.stt(elm2.v, oh1.v, -BIG, elm.v, ALU.mult, ALU.add)
        P.reduce(f2(m2), elm2.v, ALU.max)
        P.tt(oh2.v, elm2.v, bc(m2, 16), ALU.is_equal)
        P.tt(dd.v, m2.v, m1.v, ALU.subtract)
        P.act(e21.v, dd.v, AF.Exp)
        P.ts(w1.v, e21.v, 1.0, None, ALU.add)
        P.recip(w1.v, w1.v)
        P.tt(w1p.v, w1.v, pg.v, ALU.mult)
        P.tt(w2p.v, w1p.v, e21.v, ALU.mult)
        P.tt(cmb.v, oh1.v, bc(w1p, 16), ALU.mult)
        P.tt(cmb2.v, oh2.v, bc(w2p, 16), ALU.mult)
        P.tt(combb.v, cmb.v, cmb2.v, ALU.add)

    for l in range(L):
        xsrc = x_in if l == 0 else out
        sched_layer_mod(l)
        for _g in range(NG):
            sched_group(l)
        P.dma("sp", prm_sb.v, prm[l])
        P.dma("pool", w2_sb.v, w2[l])
        P.dma("pool", bg_sb.v, prow[l][:, RW_BG:RW_BG + 256])
        P.dma("sp", wr_sb.v, wr[l].rearrange("(k p) n -> p k n", p=128))
        P.dma("sp", rbias.v, prow[l][:, RW_BR:RW_BR + 20].partition_broadcast(128))
        wl = w_in[l].rearrange("(k p) n -> p k n", p=128)
        wload(wsm[:, :, WSM_GA:WSM_GA + 16], wl[:, :, OFF["ga"]:OFF["ga"] + 16])
        wload(wsm[:, :, WSM_IQ:WSM_N], wl[:, :, OFF["diq"]:OFF["diq"] + 324])
        cc32 = P.ar("cc32", [128, 8], F32, 0)
        P.dma("sp", cc32.v, ccol.v)
        P.act(cactT.v, cc32.v, AF.Silu)
        psm = P.next_ps()
        for i in range(12):
            wb = next_wb()
            for jj in range(4):
                j = i * 4 + jj
                for k in range(8):
                    P.mm(psm[:, j:j + 1], wb[:, k, jj * 128:(jj + 1) * 128], cactT[:, k:k + 1],
                         start=(k == 0), stop=(k == 7))
            rel_wb(wb)
        P.tt(modT.v, psm[:, 0:48], prm_sb[:, PR_BMOD:PR_BMOD + 48], ALU.add)
        P.stt(sT[:, 0:8], modT[:, 8:16], 1.0, prm_sb[:, PR_G1:PR_G1 + 8], ALU.add, ALU.mult)
        P.stt(sT[:, 8:16], modT[:, 32:40], 1.0, prm_sb[:, PR_G2:PR_G2 + 8], ALU.add, ALU.mult)
        ws32 = P.ar("ws32", [128, 4, 128], F32, 2048)
        P.dma("sp", ws32.v, wsT[l])
        P.tt(ws32.v, ws32.v, trif.rearrange("p (o t) -> p o t", o=1).to_broadcast([128, 4, 128]), ALU.mult)
        P.copy(WsTm.v, ws32.v)
        bsrow = P.ar("bsrow", [1, 512], F32, 4096)
        P.dma("sp", bsrow.v, prow[l][:, RW_BS:RW_BS + 512])
        bsb = P.ar("bsb", [128, 4, 128], F32, 6144)
        for g in range(4):
            ps1 = P.next_ps()
            P.mm(ps1[:, 0:128], onesf[0:1, :], bsrow[:, g * 128:(g + 1) * 128])
            P.copy(bsb[:, g, :], ps1[:, 0:128], eng="act")
            P.mm(ps1[:, 128:256], onesf, ws32[:, g, :])
            P.stt(Cg[:, g, :], ps1[:, 128:256], prm_sb[:, PR_LNB + g:PR_LNB + g + 1], bsb[:, g, :],
                  ALU.mult, ALU.add)
        P.ts(stat[:, 32:33], prm_sb[:, PR_GQ:PR_GQ + 1], float(128 ** -0.5), None, ALU.mult)
        gq_col = stat[:, 32:33]
        gk_col = prm_sb[:, PR_GK:PR_GK + 1]
        gng_col = prm_sb[:, PR_GNG:PR_GNG + 1]
        P.memset(Sf.v, 0.0)
        P.memset(Sb.v, 0.0)

        def gt_bcast(col0, o_gtb, o_dg):
            gtb = P.ar("gtb", [128, D], F32, o_gtb)
            dg = P.ar("dg", [128, 128], F32, o_dg)
            pss = [P.next_ps(), P.next_ps()]
            for k in range(8):
                P.ts(dg.v, identf, modT[:, col0 + k:col0 + k + 1], None, ALU.mult)
                P.mm(pss[k // 4][:, (k % 4) * 128:(k % 4 + 1) * 128], onesf, dg.v)
            P.copy(gtb[:, 0:512], pss[0].v, eng="act")
            P.copy(gtb[:, 512:1024], pss[1].v, eng="act")
            return gtb

        for g in range(NG):
            for bi in range(4):
                gb = g * 4 + bi
                P.dma("sp", xg[:, bi, :], (xsrc if l == 0 else out.sub(gb))[gb * 128:(gb + 1) * 128, :])
            P.mark(f'g{g}_start')
            norm_to_hT(sT[:, 0:8], modT[:, 0:8], False)
            P.mark(f'g{g}_norm1')
            dump("modT", modT.v, [128, 48], F32)
            dump("hT", hT.v, [128, 8, 512], BF16)

            uT = P.ar("uT", [128, 4, 512], BF16, 8192)
            vg = P.ar("vg", [128, 4, 512], F32, 12288)
            vn = P.ar("vn", [128, 4, 512], BF16, 20480)
            gtmp = [P.ar(f"gtmp{i}", [128, 128], F32, 24576 + 512 * i) for i in range(2)]
            gjunk = P.ar("gjunk", [128, 512], BF16, 25600)
            wu = next_wb()
            for c in range(4):
                ps = proj_feat(wu, c * 128, 128)
                P.act(uT[:, c, :], ps.v, AF.Gelu_apprx_tanh)
            rel_wb(wu)
            wv = next_wb()
            for bi in range(4):
                ps = proj_tok(wv, 0, 512, bi)
                P.act(vg[:, bi, :], ps.v, AF.Gelu_apprx_tanh, accum=stat[:, 8 + bi:9 + bi])
                P.act(gjunk.v, vg[:, bi, :], AF.Square, accum=stat[:, 12 + bi:13 + bi])
            rel_wb(wv)
            mean = stat[:, 16:20]
            P.ts(mean, stat[:, 8:12], 1.0 / 512, None, ALU.mult)
            msq = stat[:, 20:24]
            P.tt(msq, mean, mean, ALU.mult)
            var = stat[:, 24:28]
            P.stt(var, stat[:, 12:16], 1.0 / 512, msq, ALU.mult, ALU.subtract)
            small_rstd(var, var, 1.0)
            for bi in range(4):
                P.ts(vn[:, bi, :], vg[:, bi, :], stat[:, 16 + bi:17 + bi], stat[:, 24 + bi:25 + bi],
                     ALU.subtract, ALU.mult)
            for bi in range(4):
                ps = P.next_ps()
                for gg in range(4):
                    P.mm(ps[:, gg * 128:(gg + 1) * 128], vn[:, bi, gg * 128:(gg + 1) * 128], WsTm[:, gg, :])
                for gg in range(4):
                    tmp = gtmp[gg % 2]
                    P.stt(tmp.v, ps[:, gg * 128:(gg + 1) * 128], prm_sb[:, PR_LNG + gg:PR_LNG + gg + 1],
                          Cg[:, gg, :], ALU.mult, ALU.add)
                    P.tt(ysT[:, 0, gg, bi * 128:(bi + 1) * 128], tmp.v, uT[:, gg, bi * 128:(bi + 1) * 128],
                         ALU.mult)

            P.mark(f'g{g}_gmlp')
            gaT = P.ar("gaT", [16, 512], BF16, 8192)
            ps = proj_feat(wsm, WSM_GA, 16)
            P.copy(gaT.v, ps[0:16, :])
            qk_all = P.ar("qk_all", [128, 4, 512], F32, 9216)
            v_allg = P.ar("v_allg", [128, 4, 512], BF16, 17408)
            r_all = P.ar("r_all", [128, 4, 512], BF16, 21504)
            l_sb = P.ar("l_sb", [128, 256], F32, 25600)
            e_sb = P.ar("e_sb", [128, 256], F32, 26624)
            eb = P.ar("eb", [128, 256], F32, 27648)
            ebp = P.ar("ebp", [128, 256], F32, 28672)
            ebl = P.ar("ebl", [128, 256], F32, 29696)
            tmpk = P.ar("tmpk", [128, 256], F32, 30720)
            qe = P.ar("qe", [128, 256], BF16, 31744)
            ke = P.ar("ke", [128, 256], BF16, 32256)
            kd = P.ar("kd", [128, 256], BF16, 32768)
            qkT = P.ar("qkT", [64, 8, 128], BF16, 33280)
            ATm = P.ar("ATm", [128, 4, 128], BF16, 35328)
            yb = P.ar("yb", [128, 512], BF16, 36352)
            Dcol = P.ar("Dcol", [64, 4], F32, 37376)
            oj = P.ar("oj", [128, 128], BF16, 37888)
            wqk = next_wb()
            for bi in range(4):
                ps = proj_tok(wqk, 0, 512, bi)
                P.copy(qk_all[:, bi, :], ps.v, eng="act")
            rel_wb(wqk)
            wgv = next_wb()
            for bi in range(4):
                ps = proj_tok(wgv, 0, 512, bi)
                P.copy(v_allg[:, bi, :], ps.v)
            rel_wb(wgv)
            wgr = next_wb()
            for bi in range(4):
                ps = proj_tok(wgr, 0, 512, bi)
                P.act(r_all[:, bi, :], ps.v, AF.Silu)
            rel_wb(wgr)
            for bi in range(4):
                bs = slice(bi * 128, (bi + 1) * 128)
                qk_sb = qk_all[:, bi, :]
                v_sb = v_allg[:, bi, :]
                r_sb = r_all[:, bi, :]
                psz = P.next_ps()
                P.mm(psz[:, 0:256], gaT[:, bs], w2_sb.v, start=True, stop=False)
                P.mm(psz[:, 0:256], ones1.v, bg_sb.v, start=False, stop=True)
                P.act(e_sb.v, psz[:, 0:256], AF.Exp, scale=-1.0)
                P.act(l_sb.v, e_sb.v, AF.Ln, bias=1.0)
                psc = P.next_ps()
                P.mm(psc[:, 0:256], trif, l_sb.v)
                P.mm(psc[:, 256:512], onesf, l_sb.v)
                psd = P.next_ps()
                for h in range(4):
                    P.mm(psd[0:64, h:h + 1], l_sb[:, h * 64:(h + 1) * 64], onesf[:, 0:1])
                P.act(eb.v, psc[:, 0:256], AF.Exp, scale=-1.0 / 16)
                P.act(ebp.v, psc[:, 0:256], AF.Exp, scale=1.0 / 16)
                P.act(ebl.v, psc[:, 256:512], AF.Exp, scale=-1.0 / 16)
                P.act(Dcol.v, psd[0:64, 0:4], AF.Exp, scale=-1.0 / 16)
                P.stt(qe.v, qk_sb[:, 0:256], 0.125, eb.v, ALU.mult, ALU.mult)
                P.tt(ke.v, qk_sb[:, 256:512], ebp.v, ALU.mult)
                P.tt(tmpk.v, ebp.v, ebl.v, ALU.mult)
                P.tt(kd.v, qk_sb[:, 256:512], tmpk.v, ALU.mult)
                pst = P.next_ps()
                pstb = pst.v.bitcast(BF16)
                for h in range(4):
                    P.tr(pstb[0:64, h * 128:(h + 1) * 128], qe[:, h * 64:(h + 1) * 64], identb.v)
                    P.tr(pstb[0:64, (4 + h) * 128:(5 + h) * 128], ke[:, h * 64:(h + 1) * 64], identb.v)
                P.copy(qkT.v.rearrange("p a b -> p (a b)"), pstb[0:64, :], eng="act")
                psa = P.next_ps()
                for h in range(4):
                    P.mm(psa[:, h * 128:(h + 1) * 128], qkT[:, 4 + h, :], qkT[:, h, :])
                P.tt(ATm.v, psa.v.rearrange("p (h t) -> p h t", h=4),
                     trif.rearrange("p (o t) -> p o t", o=1).to_broadcast([128, 4, 128]), ALU.mult)
                pso = P.next_ps()
                for h in range(4):
                    hs = slice(h * 128, (h + 1) * 128)
                    P.mm(pso[:, hs], ATm[:, h, :], v_sb[:, hs], start=True, stop=False)
                    P.mm(pso[:, hs], qkT[:, h, :], Sb[:, h, :], start=False, stop=True)
                psu = P.next_ps()
                for h in range(4):
                    P.mm(psu[0:64, h * 128:(h + 1) * 128], kd[:, h * 64:(h + 1) * 64], v_sb[:, h * 128:(h + 1) * 128])
                for h in range(4):
                    P.stt(Sf[:, h, :], Sf[:, h, :], Dcol[:, h:h + 1], psu[0:64, h * 128:(h + 1) * 128],
                          ALU.mult, ALU.add)
                P.copy(Sb.v, Sf.v, eng="pool")
                for h in range(4):
                    P.act(oj.v, pso[:, h * 128:(h + 1) * 128], AF.Square, accum=stat[:, 36 + h:37 + h])
                small_rstd(stat[:, 36:40], stat[:, 36:40], 1.0 / 128)
                for h in range(4):
                    hs = slice(h * 128, (h + 1) * 128)
                    P.stt(yb[:, hs], pso[:, hs], stat[:, 36 + h:37 + h], r_sb[:, hs], ALU.mult, ALU.mult)
                pst2 = P.next_ps()
                pst2b = pst2.v.bitcast(BF16)
                for h in range(4):
                    P.tr(pst2b[:, h * 128:(h + 1) * 128], yb[:, h * 128:(h + 1) * 128], identb.v)
                P.act(ysT[:, 1, :, bs], pst2b[:, 0:512].rearrange("p (h t) -> p h t", h=4), AF.Identity,
                      scale=gng_col)

            P.mark(f'g{g}_gla')
            kn = P.ar("kn", [128, 512], BF16, 8192)
            qT = P.ar("qT", [128, 4, 512], BF16, 9216)
            iqT = P.ar("iqT", [64, 4, 512], BF16, 13312)
            iw_sb = P.ar("iw_sb", [128, 4, 4], F32, 17408)
            relu_t = [P.ar("relu0", [128, 512], F32, 17920)]
            PT = [P.ar(f"PT{i}", [128, 512], BF16, 19968 + 1024 * i) for i in range(2)]
            yc = P.ar("yc", [128, 512], BF16, 22016)
            bjunk = P.ar("bjunk", [128, 128], BF16, 23040)
            score = P.ar("score", [128, S], F32, 23552)
            maskbuf = [P.ar(f"maskbuf{i}", [128, S], BF16, 23552 + 4 * S + 2 * S * i) for i in range(2)]

            def qk_norm_T(ps, gcol, dst):
                for h in range(4):
                    P.act(bjunk.v, ps[:, h * 128:(h + 1) * 128], AF.Square, accum=stat[:, 40 + h:41 + h])
                small_rstd(stat[:, 40:44], stat[:, 40:44], 1.0 / 128)
                for h in range(4):
                    P.ts(kn[:, h * 128:(h + 1) * 128], ps[:, h * 128:(h + 1) * 128], stat[:, 40 + h:41 + h],
                         None, ALU.mult)
                pt = P.next_ps()
                ptb = pt.v.bitcast(BF16)
                for h in range(4):
                    P.tr(ptb[:, h * 128:(h + 1) * 128], kn[:, h * 128:(h + 1) * 128], identb.v)
                P.act(dst, ptb[:, 0:512].rearrange("p (h t) -> p h t", h=4), AF.Identity, scale=gcol)

            for bi in range(4):
                gb = g * 4 + bi
                bs = slice(bi * 128, (bi + 1) * 128)
                psw = P.next_ps()
                for k in range(8):
                    P.mm(psw[:, 0:4], hT[:, k, bs], wsm[:, k, WSM_IW:WSM_IW + 4], start=(k == 0), stop=(k == 7))
                P.copy(iw_sb[:, bi, :], psw[:, 0:4])
            ps = proj_feat(wsm, WSM_IK, 64)
            P.copy(ikT_all[:, g * 512:(g + 1) * 512], ps[0:64, :], eng="act")
            for h in range(4):
                ps = proj_feat(wsm, WSM_IQ + h * 64, 64)
                P.copy(iqT[:, h, :], ps[0:64, :], eng="act")


            def IB_gen(bi):
                gb = g * 4 + bi
                bs = slice(bi * 128, (bi + 1) * 128)
                nk = (gb + 1) * 128
                nch = (nk + 511) // 512
                mk = maskbuf[bi % 2]
                for c in range(nch):
                    c0, c1 = c * 512, min((c + 1) * 512, nk)
                    w = c1 - c0
                    for h in range(4):
                        psi = P.next_ps()
                        P.mm(psi[:, 0:w], iqT[:, h, bs], ikT_all[:, c0:c1])
                        rl = relu_t[0]
                        P.act(rl[:, 0:w], psi[:, 0:w], AF.Relu)
                        if h == 0:
                            P.ts(score[:, c0:c1], rl[:, 0:w], iw_sb[:, bi, 0:1], None, ALU.mult)
                        else:
                            P.stt(score[:, c0:c1], rl[:, 0:w], iw_sb[:, bi, h:h + 1], score[:, c0:c1],
                                  ALU.mult, ALU.add)
                    yield
                P.tt(score[:, gb * 128:nk], score[:, gb * 128:nk], negtri, ALU.add)
                tau = stat[:, 44:45]
                if gb < 2:
                    P.memset(tau, -1.0e29)
                else:
                    rng = stat[:, 45:46]
                    mid = stat[:, 46:47]
                    cnt = stat[:, 47:48]
                    u = stat[:, 48:49]
                    hi = stat[:, 49:50]
                    lo = stat[:, 50:51]
                    P.ts(mk[:, 0:nk], score[:, 0:nk], 0.0, -3.0e38, ALU.add, ALU.max, accum=hi)
                    P.ts(mk[:, 0:gb * 128], score[:, 0:gb * 128], 0.0, 3.0e38, ALU.add, ALU.min, accum=lo)
                    P.tt(rng, hi, lo, ALU.subtract)
                    P.ts(offs.v, fv.v, rng, None, ALU.mult)
                    P.tt(mid, lo, offs[:, 0:1], ALU.add)
                    for it in range(NBIS):
                        P.ts(mk[:, 0:nk], score[:, 0:nk], mid, 0.0, ALU.is_ge, ALU.add, accum=cnt)
                        last = (it == NBIS - 1)
                        P.ts(u, cnt, 256.0, 1.0 if last else 0.5, ALU.is_ge, ALU.subtract)
                        P.stt(tau if last else mid, u, offs[:, it:it + 1], mid, ALU.mult, ALU.add)
                        yield
                P.ts(mk[:, 0:nk], score[:, 0:nk], tau, NEG, ALU.is_lt, ALU.mult)
                yield

            def drain(gen):
                for _ in gen:
                    pass

            def interleave(ga, gb_):
                a_done = b_done = False
                while not (a_done and b_done):
                    if not a_done:
                        try:
                            next(ga)
                        except StopIteration:
                            a_done = True
                    if not b_done:
                        try:
                            next(gb_)
                        except StopIteration:
                            b_done = True

            def ATT_gen(bi):
                gb = g * 4 + bi
                bs = slice(bi * 128, (bi + 1) * 128)
                mk = maskbuf[bi % 2]
                P.memset(psA.v, 0.0)
                P.memset(psB.v, 0.0)
                def LG(j):
                    psl = P.next_ps()
                    dist = gb - j
                    P.mm(psl.v, mk[:, j * 128:(j + 1) * 128], ident4.v, start=True, stop=False)
                    if dist <= 1:
                        P.mm(psl.v, identb.v, biasT[:, dist, :, :].rearrange("p h t -> p (h t)"),
                             start=False, stop=False)
                    for h in range(4):
                        hs = slice(h * 128, (h + 1) * 128)
                        P.mm(psl[:, hs], kT_all[:, h, j * 128:(j + 1) * 128], qT[:, h, bs],
                             start=False, stop=(h == 3))
                    return psl

                psl = LG(0)
                P.act(PT[0].v, psl.v, AF.Exp, bias=-6.0)
                for j in range(gb + 1):
                    pt = PT[j % 2]
                    if j + 1 <= gb:
                        psl_next = LG(j + 1)
                    yield
                    for h in range(4):
                        acc = psA if h < 2 else psB
                        a0 = (h % 2) * 130
                        P.mm(acc[:, a0:a0 + 129], pt[:, h * 128:(h + 1) * 128], v_all[:, j, h, 0:129],
                             start=False, stop=False, skip=True)
                    if j + 1 <= gb:
                        P.act(PT[(j + 1) % 2].v, psl_next.v, AF.Exp, bias=-6.0)
                for h in range(4):
                    acc = psA if h < 2 else psB
                    a0 = (h % 2) * 130
                    P.recip(stat[:, 52 + h:53 + h], acc[:, a0 + 128:a0 + 129])
                    P.ts(yc[:, h * 128:(h + 1) * 128], acc[:, a0:a0 + 128], stat[:, 52 + h:53 + h], None, ALU.mult)
                pt2 = P.next_ps()
                pt2b = pt2.v.bitcast(BF16)
                for h in range(4):
                    P.tr(pt2b[:, h * 128:(h + 1) * 128], yc[:, h * 128:(h + 1) * 128], identb.v)
                P.copy(ysT[:, 2, :, bs], pt2b[:, 0:512].rearrange("p (h t) -> p h t", h=4), eng="act")

            def projA_gen():
                wdq = next_wb()
                for bi in range(4):
                    bs = slice(bi * 128, (bi + 1) * 128)
                    ps = proj_tok(wdq, 0, 512, bi)
                    qk_norm_T(ps, gq_col, qT[:, :, bs])
                    yield
                rel_wb(wdq)
                wdk = next_wb()
                for bi in range(4):
                    gb = g * 4 + bi
                    ps = proj_tok(wdk, 0, 512, bi)
                    qk_norm_T(ps, gk_col, kT_all[:, :, gb * 128:(gb + 1) * 128])
                    yield
                rel_wb(wdk)
                wdv = next_wb()
                for bi in range(4):
                    gb = g * 4 + bi
                    ps = proj_tok(wdv, 0, 512, bi)
                    P.copy(v_all[:, gb, :, 0:128], ps.v.rearrange("p (h e) -> p h e", h=4))
                    yield
                rel_wb(wdv)

            interleave(projA_gen(), IB_gen(0))
            P.mark(f'g{g}_dsaproj')
            for bi in range(4):
                if bi + 1 < 4:
                    interleave(ATT_gen(bi), IB_gen(bi + 1))
                else:
                    drain(ATT_gen(bi))

            P.mark(f'g{g}_dsa')
            dump("ysT", ysT.v, [128, 3, 4, 512], BF16)
            dump("kT", kT_all[:, :, 0:512], [128, 4, 512], BF16)
            mergedF = P.ar("mergedF", [128, 8, 512], F32, 8192)
            mergedT = P.ar("mergedT", [128, 8, 512], BF16, 24576)
            sgt = [P.ar(f"sgt{i}", [128, 512], BF16, 32768 + 1024 * i) for i in range(2)]
            mtmp = [P.ar(f"mtmp{i}", [128, 512], F32, 34816 + 2048 * i) for i in range(2)]
            ci = 0
            for n in range(3):
                wbr = next_wb()
                wbrv = wbr.v.rearrange("p a b -> p (a b)").rearrange("p (e d) -> p e d", e=4)
                for half in range(2):
                    wg = next_wb()
                    for cc in range(4):
                        dc = half * 4 + cc
                        psg = proj_feat(wg, cc * 128, 128)
                        sg = sgt[ci % 2]
                        P.act(sg.v, psg.v, AF.Sigmoid, bias=prm_sb[:, PR_BBG + n * 8 + dc:PR_BBG + n * 8 + dc + 1])
                        psu = P.next_ps()
                        for ec in range(4):
                            P.mm(psu.v, wbrv[:, ec, dc * 128:(dc + 1) * 128], ysT[:, n, ec, :],
                                 start=(ec == 0), stop=(ec == 3))
                        if n == 0:
                            P.tt(mergedF[:, dc, :], psu.v, sg.v, ALU.mult)
                        else:
                            mt = mtmp[ci % 2]
                            P.tt(mt.v, psu.v, sg.v, ALU.mult)
                            if n == 1:
                                P.tt(mergedF[:, dc, :], mergedF[:, dc, :], mt.v, ALU.add, eng="pool")
                            else:
                                P.tt(mergedT[:, dc, :], mergedF[:, dc, :], mt.v, ALU.add, eng="pool")
                        ci += 1
                    rel_wb(wg)
                rel_wb(wbr)
            P.mark(f'g{g}_merge')
            gtb = gt_bcast(16, 40960, 45056)
            for half in range(2):
                wo = next_wb()
                for bi in range(4):
                    ps = P.next_ps()
                    for k in range(8):
                        P.mm(ps.v, mergedT[:, k, bi * 128:(bi + 1) * 128], wo[:, k, :], start=(k == 0), stop=(k == 7))
                    mt = mtmp[bi % 2]
                    P.tt(mt.v, ps.v, gtb[:, half * 512:(half + 1) * 512], ALU.mult)
                    P.tt(xg[:, bi, half * 512:(half + 1) * 512], xg[:, bi, half * 512:(half + 1) * 512], mt.v,
                         ALU.add, eng="pool")
                rel_wb(wo)

            dump("mergedT", mergedT.v, [128, 8, 512], BF16)
            dump("x1", xg.v, [128, 4, D], F32)
            P.mark(f'g{g}_wout')
            norm_to_hT(sT[:, 8:16], modT[:, 24:32], True)
            dump("h2T", hT.v, [128, 8, 512], BF16)
            dump("combb", combb.v, [128, 4, 16], BF16)
            P.mark(f'g{g}_norm2')
            gtb = gt_bcast(40, 16384, 20480)
            cb_sb = P.ar("cb_sb", [128, 512], BF16, 0)
            t1 = [P.ar("t1", [128, 512], BF16, 1024)]
            sgm = [P.ar(f"sgm{i}", [128, 512], BF16, 2048 + 1024 * i) for i in range(2)]
            actT = P.ar("actT", [128, 4, 2, 512], BF16, 4096)
            mtmp = [P.ar(f"mtmpb{i}", [128, 512], F32, 12288 + 2048 * i) for i in range(2)]
            WD = [P.ar(f"wd{i}", [128, 2, D], BF16, 21504 + 4096 * i) for i in range(6)]
            wdi = 0
            for rnd in range(4):
                wds = []
                for ee in range(4):
                    e = rnd * 4 + ee
                    wgu = next_wb()
                    wd = WD[wdi % 6]
                    wdi += 1
                    wload(wd.v, wed[l][e].rearrange("(f p) d -> p f d", p=128))
                    wds.append(wd)
                    psc = P.next_ps()
                    for bi in range(4):
                        P.mm(psc[:, bi * 128:(bi + 1) * 128], combb[:, bi, e:e + 1].to_broadcast([128, 128]),
                             identb.v)
                    P.copy(cb_sb.v, psc.v, eng="act")
                    for fc in range(2):
                        psg = proj_feat(wgu, fc * 128, 128)
                        psu = proj_feat(wgu, 256 + fc * 128, 128)
                        sg = sgm[fc]
                        P.act(sg.v, psg.v, AF.Silu)
                        P.tt(t1[0].v, psu.v, sg.v, ALU.mult)
                        P.tt(actT[:, ee, fc, :], t1[0].v, cb_sb.v, ALU.mult, eng="pool")
                    rel_wb(wgu)
                for bi in range(4):
                    for half in range(2):
                        ps = P.next_ps()
                        n = 0
                        for ee in range(4):
                            for fc in range(2):
                                P.mm(ps.v, actT[:, ee, fc, bi * 128:(bi + 1) * 128],
                                     wds[ee][:, fc, half * 512:(half + 1) * 512], start=(n == 0), stop=(n == 7))
                                n += 1
                        mt = mtmp[(bi * 2 + half) % 2]
                        P.tt(mt.v, ps.v, gtb[:, half * 512:(half + 1) * 512], ALU.mult)
                        P.tt(xg[:, bi, half * 512:(half + 1) * 512], xg[:, bi, half * 512:(half + 1) * 512], mt.v,
                             ALU.add, eng="pool")
            P.mark(f'g{g}_moe')
            for bi in range(4):
                gb = g * 4 + bi
                P.dma("sp", out.sub(gb)[gb * 128:(gb + 1) * 128, :], xg[:, bi, :])
    P.fence("sp", [out] + list(dbgo.values()))
    stats = P.emit()
    stats['marks'] = getattr(P, 'marks', [])
    return nc, stats


def _bucket_np(rel):
    import math
    n = np.maximum(rel, 0)
    max_exact = 16
    large = max_exact + (np.log(np.maximum(n, max_exact).astype(np.float32) / max_exact)
                         / math.log(128 / max_exact) * (32 - max_exact)).astype(np.int32)
    large = np.minimum(large, 31)
    return np.where(n < max_exact, n, large)


def host_consts():
    cst = np.zeros((128, NCST), np.float32)
    i = np.arange(128)
    cst[:, C_ID:C_ID + 128] = np.eye(128, dtype=np.float32)
    cst[:, C_TRI:C_TRI + 128] = (i[:, None] <= i[None, :]).astype(np.float32)
    cst[:, C_ONES:C_ONES + 128] = 1.0
    cst[:, C_NEGTRI:C_NEGTRI + 128] = np.where(i[None, :] <= i[:, None], 0.0, BIGNEG)
    cst[:, C_BKD:C_BKD + 128] = _bucket_np(i[None, :] - i[:, None]).astype(np.float32)
    cst[:, C_BKN:C_BKN + 128] = _bucket_np(128 + i[None, :] - i[:, None]).astype(np.float32)
    return cst


def host_layout(inputs, L, b, S):
    f = lambda a: np.ascontiguousarray(np.asarray(a, dtype=np.float32))
    col = lambda v: np.asarray(v, np.float32).reshape(-1, 128).T
    m = {}
    m["x"] = f(inputs["x"][b, :S])
    m["ccol"] = f(col(inputs["c"][b]))
    m["cst"] = host_consts()
    m["rbrow"] = f(np.asarray(inputs["rel_bias"]).reshape(1, 128))
    prm = np.zeros((L, 128, NPRM), np.float32)
    prow = np.zeros((L, 1, NROW), np.float32)
    for l in range(L):
        prm[l, :, PR_G1:PR_G1 + 8] = col(inputs["g_norm1"][l])
        prm[l, :, PR_G2:PR_G2 + 8] = col(inputs["g_norm2"][l])
        prm[l, :, PR_BMOD:PR_BMOD + 48] = col(inputs["b_mod"][l])
        prm[l, :, PR_LNG:PR_LNG + 4] = col(inputs["gmlp_ln_g"][l])
        prm[l, :, PR_LNB:PR_LNB + 4] = col(inputs["gmlp_ln_b"][l])
        prm[l, :, PR_GNG] = np.asarray(inputs["gla_norm_g"][l])
        prm[l, :, PR_GQ] = np.asarray(inputs["dsa_qnorm_g"][l])
        prm[l, :, PR_GK] = np.asarray(inputs["dsa_knorm_g"][l])
        prm[l, :, PR_BBG:PR_BBG + 24] = col(np.asarray(inputs["b_branch_gate"][l]).reshape(-1))
        prow[l, 0, RW_BS:RW_BS + 512] = np.asarray(inputs["gmlp_b_s"][l]).reshape(-1)
        prow[l, 0, RW_BG:RW_BG + 256] = np.asarray(inputs["gla_b_gate"][l])
        prow[l, 0, RW_BR:RW_BR + 4] = np.asarray(inputs["b_group"][l])
        prow[l, 0, RW_BR + 4:RW_BR + 20] = np.asarray(inputs["b_router"][l])
    m["prm"] = prm
    m["prow"] = prow
    m["w_mod"] = f(inputs["w_mod"][:L])
    m["w_in"] = f(inputs["w_in"][:L])
    m["wsT"] = f(np.asarray(inputs["gmlp_w_s"][:L]).transpose(0, 3, 1, 2))
    m["w2"] = f(inputs["gla_w_gate2"][:L])
    m["w_branch"] = f(inputs["w_branch"][:L])
    m["w_out"] = f(inputs["w_out"][:L])
    m["wr"] = f(np.concatenate([np.asarray(inputs["w_group"][:L]), np.asarray(inputs["w_router"][:L])], axis=2))
    m["w_exp_gate"] = f(inputs["w_exp_gate"][:L])
    m["w_exp_up"] = f(inputs["w_exp_up"][:L])
    m["w_exp_down"] = f(inputs["w_exp_down"][:L])
    return m


_PROG_CACHE = {}


def run_model(inputs, L, S, batches, n_cores, want_all=False, active=None):
    key = (L, S)
    if key not in _PROG_CACHE:
        _PROG_CACHE[key] = build_program(L, S)
    nc, stats = _PROG_CACHE[key]
    if active is None:
        active = list(range(min(n_cores, len(batches))))
    shared = host_layout(inputs, L, batches[0], S)
    zeros = None
    in_maps = []
    for ci in range(n_cores):
        if ci in active:
            b = batches[active.index(ci)]
            m = dict(shared)
            m["x"] = np.ascontiguousarray(np.asarray(inputs["x"][b, :S], dtype=np.float32))
            m["ccol"] = np.ascontiguousarray(np.asarray(inputs["c"][b], np.float32).reshape(-1, 128).T)
        else:
            if zeros is None:
                zeros = {k: np.zeros_like(v) for k, v in shared.items()}
            m = dict(zeros)
        in_maps.append(m)
    res = run_bass_kernel_spmd(nc, in_maps, core_ids=list(range(n_cores)))
    if want_all:
        return res.results
    return [res.results[ci]["out"] for ci in active]


def kernel(**inputs):
    L, B, S = 4, 4, 4096
    outs = run_model(inputs, L, S, list(range(B)), 8, active=[0, 1, 4, 5])
    return np.stack(outs, axis=0).astype(np.float32)
```

```python
import numpy as np
import concourse.bass as bass
import concourse.mybir as mybir

F32 = mybir.dt.float32
BF16 = mybir.dt.bfloat16
AF = mybir.ActivationFunctionType
ALU = mybir.AluOpType
AX = mybir.AxisListType

GRAN = 512
SEM_CAP = 30000
STRICT_SAME_ENGINE = True


class Region:
    __slots__ = ("lw", "rd")

    def __init__(self):
        self.lw = None
        self.rd = {}


class View:
    __slots__ = ("buf", "ap")

    def __init__(self, buf, ap):
        self.buf = buf
        self.ap = ap

    def __getitem__(self, idx):
        return View(self.buf, self.ap[idx])

    def rearrange(self, s, **kw):
        return View(self.buf, self.ap.rearrange(s, **kw))

    def bitcast(self, dt):
        return View(self.buf, self.ap.bitcast(dt))

    def to_broadcast(self, shape):
        return View(self.buf, self.ap.to_broadcast(shape))

    def partition_broadcast(self, n):
        return View(self.buf, self.ap.partition_broadcast(n))


class Buf:
    def __init__(self, name, h, regions, is_dram=False):
        self.name = name
        self.h = h
        self.regions = regions
        self.is_dram = is_dram

    def __getitem__(self, idx):
        base = self.h.ap() if self.is_dram else self.h
        return View(self, base[idx])

    @property
    def v(self):
        base = self.h.ap() if self.is_dram else self.h[:]
        return View(self, base)

    def sub(self, i, n=1):
        return Buf(self.name, self.h, self.regions[i:i + n], self.is_dram)


class Op:
    __slots__ = ("eng", "fn", "kind", "deps", "lidx", "flag", "waits", "clock",
                 "fnum", "dma_sem", "dma_val", "dq_n")


class Prog:
    ENGS = ("pe", "act", "dve", "pool", "sp")

    def __init__(self, nc, n_dma_sems=None):
        self.nc = nc
        self.h = {"pe": nc.tensor, "act": nc.scalar, "dve": nc.vector,
                  "pool": nc.gpsimd, "sp": nc.sync}
        self.ops = []
        self.n_dma_sems = n_dma_sems or {"sp": 16, "pool": 16, "act": 4}
        self.dma_count = {e: 0 for e in self.ENGS}
        self.dma_ids = {e: [] for e in self.ENGS}
        self.arena_base = None
        self.arena_regions = None
        self.psum_rot = []
        self.psum_i = 0

    def sb(self, name, shape, dtype):
        h = self.nc.alloc_sbuf_tensor(name, list(shape), dtype)
        return Buf(name, h, [Region()])

    def make_arena(self, nbytes):
        r = self.nc.bump_sbuf(nbytes)
        self.arena_base = r[0]
        self.arena_size = nbytes
        self.arena_regions = [Region() for _ in range((nbytes + GRAN - 1) // GRAN)]

    def ar(self, name, shape, dtype, off):
        key = (name, tuple(shape), str(dtype), off)
        if not hasattr(self, "_arc"):
            self._arc = {}
        if key in self._arc:
            return self._arc[key]
        b = self._ar(f"{name}_{len(self._arc)}", shape, dtype, off)
        self._arc[key] = b
        return b

    def _ar(self, name, shape, dtype, off):
        esz = {F32: 4, BF16: 2}[dtype]
        n = int(np.prod(shape[1:])) * esz
        assert off % 32 == 0 and off + n <= self.arena_size, (name, off, n, self.arena_size)
        h = self.nc.alloc_sbuf_tensor_at(name, list(shape), dtype, offset=self.arena_base + off)
        regs = self.arena_regions[off // GRAN:(off + n + GRAN - 1) // GRAN]
        return Buf(name, h, regs)

    def ps(self, name, shape=(128, 512), dtype=F32):
        h = self.nc.alloc_psum_tensor(name, list(shape), dtype)
        return Buf(name, h, [Region()])

    def dram(self, name, shape, dtype, kind="Internal", nreg=1):
        h = self.nc.dram_tensor(name, list(shape), dtype, kind=kind)
        return Buf(name, h, [Region() for _ in range(nreg)], is_dram=True)

    def fence(self, eng, bufs):
        h = self.h[eng]
        self.add(eng, lambda: h.nop(), bufs, [])

    def mark(self, name):
        if not getattr(self, "marks_on", False):
            return
        last = {}
        for oid in range(len(self.ops) - 1, -1, -1):
            o = self.ops[oid]
            if o.kind == "c" and o.eng != "sp" and o.eng not in last:
                last[o.eng] = oid
            if len(last) == 4:
                break
        h = self.h["sp"]
        oid = self.add("sp", lambda: h.nop(), [], [])
        for e, d in last.items():
            self.ops[oid].deps[d] = True
        if not hasattr(self, "marks"):
            self.marks = []
        self.marks.append(name)

    def next_ps(self):
        b = self.psum_rot[self.psum_i % len(self.psum_rot)]
        self.psum_i += 1
        return b

    def add(self, eng, fn, reads, writes, kind="c"):
        op = Op()
        op.eng = eng
        op.fn = fn
        op.kind = kind
        op.flag = False
        op.deps = {}
        oid = len(self.ops)
        rregs = []
        for b in reads:
            if b is None:
                continue
            b = b.buf if isinstance(b, View) else b
            rregs.extend(b.regions)
        wregs = []
        for b in writes:
            b = b.buf if isinstance(b, View) else b
            wregs.extend(b.regions)
        for r in rregs:
            if r.lw is not None:
                op.deps[r.lw] = True
        for r in wregs:
            if r.lw is not None:
                op.deps.setdefault(r.lw, False)
            for rid in r.rd.values():
                op.deps.setdefault(rid, False)
        op.deps.pop(oid, None)
        for r in rregs:
            key = eng if kind == "c" else ("dma", oid)
            r.rd[key] = oid
        for r in wregs:
            r.lw = oid
            r.rd = {}
        if kind == "dma":
            n = self.dma_count[eng]
            self.dma_count[eng] += 1
            op.dq_n = n
            self.dma_ids[eng].append(oid)
        self.ops.append(op)
        return oid

    @staticmethod
    def _a(x):
        return x.ap if isinstance(x, View) else x

    def mm(self, out, lhsT, rhs, start=True, stop=True, skip=False):
        nc = self.nc
        if skip:
            self.add("pe", lambda: nc.tensor.matmul(out.ap, lhsT.ap, rhs.ap, start=start, stop=stop,
                                                    skip_group_check=True), [lhsT, rhs], [out])
        else:
            self.add("pe", lambda: nc.tensor.matmul(out.ap, lhsT.ap, rhs.ap, start=start, stop=stop),
                     [lhsT, rhs], [out])

    def tr(self, out, in_, ident):
        nc = self.nc
        self.add("pe", lambda: nc.tensor.transpose(out.ap, in_.ap, ident.ap), [in_, ident], [out])

    def act(self, out, in_, func, bias=None, scale=1.0, accum=None):
        nc = self.nc
        a = self._a
        kw = {}
        if bias is not None:
            kw["bias"] = a(bias)
        if accum is not None:
            kw["accum_out"] = a(accum)
        rd = [in_] + [x for x in (bias, scale) if isinstance(x, View)]
        wr = [out] + ([accum] if accum is not None else [])
        self.add("act", lambda: nc.scalar.activation(out=out.ap, in_=in_.ap, func=func,
                                                      scale=a(scale), **kw), rd, wr)

    def _veng(self, eng):
        return self.nc.vector if eng == "dve" else self.nc.gpsimd

    def tt(self, out, in0, in1, op, eng="dve"):
        e = self._veng(eng)
        self.add(eng, lambda: e.tensor_tensor(out=out.ap, in0=in0.ap, in1=in1.ap, op=op),
                 [in0, in1], [out])

    def ts(self, out, in0, s1, s2, op0, op1=None, accum=None, eng="dve"):
        e = self._veng(eng)
        a = self._a
        kw = {}
        if op1 is not None:
            kw["op1"] = op1
        if accum is not None:
            kw["accum_out"] = a(accum)
        rd = [in0] + [x for x in (s1, s2) if isinstance(x, View)]
        wr = [out] + ([accum] if accum is not None else [])
        self.add(eng, lambda: e.tensor_scalar(out=out.ap, in0=in0.ap, scalar1=a(s1), scalar2=a(s2),
                                              op0=op0, **kw), rd, wr)

    def stt(self, out, in0, scalar, in1, op0, op1, eng="dve"):
        e = self._veng(eng)
        a = self._a
        rd = [in0, in1] + ([scalar] if isinstance(scalar, View) else [])
        self.add(eng, lambda: e.scalar_tensor_tensor(out=out.ap, in0=in0.ap, scalar=a(scalar),
                                                     in1=in1.ap, op0=op0, op1=op1), rd, [out])

    def copy(self, out, in_, eng="dve"):
        if eng == "act":
            nc = self.nc
            self.add("act", lambda: nc.scalar.copy(out=out.ap, in_=in_.ap), [in_], [out])
        else:
            e = self._veng(eng)
            self.add(eng, lambda: e.tensor_copy(out=out.ap, in_=in_.ap), [in_], [out])

    def memset(self, out, val, eng="dve"):
        e = self._veng(eng)
        self.add(eng, lambda: e.memset(out.ap, val), [], [out])

    def reduce(self, out, in_, op, eng="dve"):
        e = self._veng(eng)
        self.add(eng, lambda: e.tensor_reduce(out=out.ap, in_=in_.ap, axis=AX.X, op=op), [in_], [out])

    def recip(self, out, in_):
        nc = self.nc
        self.add("dve", lambda: nc.vector.reciprocal(out=out.ap, in_=in_.ap), [in_], [out])

    def dma(self, q, out, in_, **kw):
        h = self.h[q]
        self.add(q, lambda: h.dma_start(out=out.ap, in_=in_.ap, **kw), [in_], [out], kind="dma")

    def emit(self):
        nc = self.nc
        ops = self.ops
        known = {e: {} for e in self.ENGS}
        dma_known = {e: set() for e in self.ENGS}
        count = {e: 0 for e in self.ENGS}
        for oid, op in enumerate(ops):
            E = op.eng
            waits = []
            kn = known[E]
            if op.kind == "dma":
                ns = self.n_dma_sems[E]
                if op.dq_n >= ns:
                    prev = self.dma_ids[E][op.dq_n - ns]
                    if prev not in dma_known[E]:
                        waits.append(prev)
                        dma_known[E].add(prev)
            for d, raw in op.deps.items():
                p = ops[d]
                if p.kind == "dma":
                    if d in dma_known[E]:
                        continue
                    waits.append(d)
                    dma_known[E].add(d)
                else:
                    if p.eng == E and (E == "pe" or (not raw and not STRICT_SAME_ENGINE)):
                        continue
                    if kn.get(p.eng, 0) >= p.lidx:
                        continue
                    waits.append(d)
                    p.flag = True
                    for e2, v in p.clock.items():
                        if kn.get(e2, 0) < v:
                            kn[e2] = v
                    if kn.get(p.eng, 0) < p.lidx:
                        kn[p.eng] = p.lidx
            op.waits = waits
            count[E] += 1
            op.lidx = count[E]
            op.clock = dict(kn) if op.kind == "c" else None
        fcount = {e: 0 for e in self.ENGS}
        for op in ops:
            if op.kind == "c" and op.flag:
                fcount[op.eng] += 1
                op.fnum = fcount[op.eng]
        sems = {}
        for e in self.ENGS:
            n = (fcount[e] + SEM_CAP - 1) // SEM_CAP
            sems[e] = [nc.alloc_semaphore(f"s_{e}_{i}") for i in range(n)]
        dsems = {}
        for e in self.ENGS:
            if self.dma_count[e]:
                dsems[e] = [nc.alloc_semaphore(f"d_{e}_{i}")
                            for i in range(min(self.n_dma_sems[e], self.dma_count[e]))]
        for op in ops:
            if op.kind == "dma":
                ns = self.n_dma_sems[op.eng]
                op.dma_sem = dsems[op.eng][op.dq_n % ns]
                op.dma_val = 16 * (op.dq_n // ns + 1)
        nwaits = 0
        for op in ops:
            h = self.h[op.eng]
            for d in op.waits:
                p = ops[d]
                if p.kind == "dma":
                    h.wait_ge(p.dma_sem, p.dma_val)
                else:
                    n = p.fnum - 1
                    h.wait_ge(sems[p.eng][n // SEM_CAP], n % SEM_CAP + 1)
                nwaits += 1
            ins = op.fn()
            if op.kind == "dma":
                ins.then_inc(op.dma_sem, 16)
            elif op.flag:
                n = op.fnum - 1
                ins.then_inc(sems[op.eng][n // SEM_CAP], 1)
        self.stats = dict(n_ops=len(ops), n_waits=nwaits, flagged=dict(fcount),
                          per_eng=dict(count))
        return self.stats

    def final_wait(self, eng, opids):
        pass


from concourse.bass_utils import run_bass_kernel_spmd

D = 1024
EPS = 1e-6
N_IN = 7508
OFF = dict(u=0, v=512, gq=1024, gk=1280, gv=1536, gr=2048, ga=2560, dq=2576, dk=3088,
           dv=3600, diq=4112, dik=4368, diw=4432, gates=4436)
NEG = -30000.0
BIGNEG = -1.0e30
NBIS = 12
C_ID, C_TRI, C_ONES, C_NEGTRI, C_BKD, C_BKN, NCST = 0, 128, 256, 384, 512, 640, 768
PR_G1, PR_G2, PR_BMOD, PR_LNG, PR_LNB, PR_GNG, PR_GQ, PR_GK, PR_BBG, NPRM = 0, 8, 16, 64, 68, 72, 73, 74, 75, 99
RW_BS, RW_BG, RW_BR, NROW = 0, 512, 768, 788
WSM_GA, WSM_IQ, WSM_IK, WSM_IW, WSM_N = 0, 16, 272, 336, 340


def build_program(L, S, dbg=False, marks=False):
    NBLK = S // 128
    NG = S // 512
    nc = bass.Bass("TRN2", target_bir_lowering=False)
    P = Prog(nc)
    P.marks_on = marks
    EI = "ExternalInput"
    x_in = P.dram("x", [S, D], F32, kind=EI)
    ccol = P.dram("ccol", [128, 8], F32, kind=EI)
    cst = P.dram("cst", [128, NCST], F32, kind=EI)
    rbd = P.dram("rbrow", [1, 128], F32, kind=EI)
    w_mod = P.dram("w_mod", [L, D, 6 * D], F32, kind=EI)
    prm = P.dram("prm", [L, 128, NPRM], F32, kind=EI)
    prow = P.dram("prow", [L, 1, NROW], F32, kind=EI)
    w_in = P.dram("w_in", [L, D, N_IN], F32, kind=EI)
    wsT = P.dram("wsT", [L, 128, 4, 128], F32, kind=EI)
    w2 = P.dram("w2", [L, 16, 256], F32, kind=EI)
    w_br = P.dram("w_branch", [L, 3, 512, D], F32, kind=EI)
    w_out = P.dram("w_out", [L, D, D], F32, kind=EI)
    wr = P.dram("wr", [L, D, 20], F32, kind=EI)
    weg = P.dram("w_exp_gate", [L, 16, D, 256], F32, kind=EI)
    weu = P.dram("w_exp_up", [L, 16, D, 256], F32, kind=EI)
    wed = P.dram("w_exp_down", [L, 16, 256, D], F32, kind=EI)
    out = P.dram("out", [S, D], F32, kind="ExternalOutput", nreg=NBLK)
    dbgo = {}

    def dump(name, view, shape, dtype):
        if not dbg or name in dbgo:
            return
        t = P.dram("dbg_" + name, list(shape), dtype, kind="ExternalOutput")
        dbgo[name] = t
        P.dma("sp", t.v, view)

    cf = P.sb("cf", [128, 512], F32)
    ident4 = P.sb("ident4", [128, 512], BF16)
    identb = P.sb("identb", [128, 128], BF16)
    biasT = P.sb("biasT", [128, 2, 4, 128], BF16)
    kT_all = P.sb("kT_all", [128, 4, S], BF16)
    v_all = P.sb("v_all", [128, NBLK, 4, 130], BF16)
    ikT_all = P.sb("ikT_all", [64, S], BF16)
    xg = P.sb("xg", [128, 4, D], F32)
    hT = P.sb("hT", [128, 8, 512], BF16)
    ysT = P.sb("ysT", [128, 3, 4, 512], BF16)
    WB = [P.sb(f"wb{i}", [128, 8, 512], BF16) for i in range(3)]
    wsm = P.sb("wsm", [128, 8, WSM_N], BF16)
    prm_sb = P.sb("prm_sb", [128, NPRM], F32)
    modT = P.sb("modT", [128, 48], F32)
    sT = P.sb("sT", [128, 16], F32)
    WsTm = P.sb("WsTm", [128, 4, 128], BF16)
    Cg = P.sb("Cg", [128, 4, 128], F32)
    w2_sb = P.sb("w2_sb", [16, 256], BF16)
    bg_sb = P.sb("bg_sb", [1, 256], BF16)
    ones1 = P.sb("ones1", [1, 128], BF16)
    wr_sb = P.sb("wr_sb", [128, 8, 20], F32)
    rbias = P.sb("rbias", [128, 20], F32)
    cactT = P.sb("cactT", [128, 8], BF16)
    Sf = P.sb("Sf", [64, 4, 128], F32)
    Sb = P.sb("Sb", [64, 4, 128], BF16)
    stat = P.sb("stat", [128, 64], F32)
    combb = P.sb("combb", [128, 4, 16], BF16)
    fv = P.sb("fv", [128, NBIS], F32)
    offs = P.sb("offs", [128, NBIS], F32)
    PSR = [P.ps(f"psr{i}") for i in range(6)]
    psA = P.ps("psA")
    psB = P.ps("psB")
    P.psum_rot = PSR
    ARENA = max(46 * 1024, 23552 + 8 * S)
    P.make_arena(ARENA)

    identf = cf[:, C_ID:C_ID + 128]
    trif = cf[:, C_TRI:C_TRI + 128]
    onesf = cf[:, C_ONES:C_ONES + 128]
    negtri = cf[:, C_NEGTRI:C_NEGTRI + 128]

    def wload(dst, src):
        P.dma("pool", dst, src)

    class WStream:
        def __init__(self, bufs):
            self.free = list(bufs)
            self.sched = []
            self.issued = []
            self.nissued = 0

        def push(self, fn):
            self.sched.append(fn)

        def _pump(self):
            while self.free and self.nissued < len(self.sched):
                b = self.free.pop(0)
                self.sched[self.nissued](b)
                self.nissued += 1
                self.issued.append(b)

        def get(self):
            self._pump()
            assert self.issued, "weight schedule underflow / no free buffer"
            return self.issued.pop(0)

        def release(self, b):
            self.free.append(b)
            self._pump()

    ws_main = WStream(WB)

    def sched_kxn(src2d, c0, n):
        ws_main.push(lambda wb: wload(wb[:, :, 0:n],
                                      src2d.rearrange("(k p) n -> p k n", p=128)[:, :, c0:c0 + n]))

    def sched_layer_mod(l):
        for i in range(12):
            sched_kxn(w_mod[l], i * 512, 512)

    def sched_group(l):
        for nm in ("u", "v", "gq", "gv", "gr", "dq", "dk", "dv"):
            sched_kxn(w_in[l], OFF[nm], 512)
        for n in range(3):
            ws_main.push(lambda wb, n=n: wload(
                wb.v.rearrange("p a b -> p (a b)").rearrange("p (e d) -> p e d", e=4),
                w_br[l][n].rearrange("(e p) d -> p e d", p=128)))
            for half in range(2):
                sched_kxn(w_in[l], OFF["gates"] + (n * 2 + half) * 512, 512)
        for half in range(2):
            sched_kxn(w_out[l], half * 512, 512)
        for e in range(16):
            def f(wb, e=e):
                wload(wb[:, :, 0:256], weg[l][e].rearrange("(k p) n -> p k n", p=128))
                wload(wb[:, :, 256:512], weu[l][e].rearrange("(k p) n -> p k n", p=128))
            ws_main.push(f)

    def next_wb():
        return ws_main.get()

    def rel_wb(b):
        ws_main.release(b)

    P.dma("sp", cf.v, cst[:, 0:512])
    bk = P.ar("bk", [128, 256], F32, 2048)
    P.dma("sp", bk.v, cst[:, 512:768])
    for i4 in range(4):
        P.dma("pool", ident4[:, i4 * 128:(i4 + 1) * 128], cst[:, C_ID:C_ID + 128])
    P.dma("pool", identb.v, cst[:, C_ID:C_ID + 128])
    P.memset(ones1.v, 1.0)
    for it in range(NBIS):
        P.memset(fv[:, it:it + 1], 0.5 ** (it + 1))
    P.memset(v_all.v, 1.0)
    rbb = P.ar("rbb", [128, 128], F32, 0)
    bacc = P.ar("bacc", [128, 128], F32, 512)
    btmp = P.ar("btmp", [128, 128], F32, 1024)
    P.dma("sp", rbb.v, rbd.v.partition_broadcast(128))
    for h in range(4):
        for dist, cb in ((0, 0), (1, 128)):
            for b in range(32):
                P.ts(btmp.v, bk[:, cb:cb + 128], float(b), None, ALU.is_equal)
                if b == 0:
                    P.ts(bacc.v, btmp.v, rbb[:, b * 4 + h:b * 4 + h + 1], None, ALU.mult)
                else:
                    P.stt(bacc.v, btmp.v, rbb[:, b * 4 + h:b * 4 + h + 1], bacc.v, ALU.mult, ALU.add)
            P.ts(biasT[:, dist, h, :], bacc.v, rbb[:, 31 * 4 + h:31 * 4 + h + 1], None, ALU.subtract)

    def small_rstd(dst, src, inv_n):
        P.ts(dst, src, inv_n, EPS, ALU.mult, ALU.add)
        P.act(dst, dst, AF.Sqrt)
        P.recip(dst, dst)

    def norm_to_hT(scol, shcol, fp32_router):
        junk = P.ar("junk", [128, D], BF16, 0)
        ssq = stat[:, 0:4]
        rstd = stat[:, 4:8]
        for bi in range(4):
            P.act(junk.v, xg[:, bi, :], AF.Square, accum=stat[:, bi:bi + 1])
        small_rstd(rstd, ssq, 1.0 / D)
        if not fp32_router:
            xns = [P.ar(f"xn{i}", [128, D], BF16, 2048 + 2048 * i) for i in range(2)]
            for bi in range(4):
                xn = xns[bi % 2]
                P.ts(xn.v, xg[:, bi, :], stat[:, 4 + bi:5 + bi], None, ALU.mult)
                ps = P.next_ps()
                psb = ps.v.bitcast(BF16)
                for k in range(8):
                    P.tr(psb[:, k * 128:(k + 1) * 128], xn[:, k * 128:(k + 1) * 128], identb.v)
                for k in range(8):
                    o = hT[:, k, bi * 128:(bi + 1) * 128]
                    if k % 2 == 0:
                        P.act(o, psb[:, k * 128:(k + 1) * 128], AF.Identity,
                              bias=shcol[:, k:k + 1], scale=scol[:, k:k + 1])
                    else:
                        P.ts(o, psb[:, k * 128:(k + 1) * 128], scol[:, k:k + 1], shcol[:, k:k + 1],
                             ALU.mult, ALU.add)
        else:
            xn32 = P.ar("xn32", [128, D], F32, 2048)
            h32 = P.ar("h32", [128, 8, 128], F32, 6144)
            lg_all = P.ar("lg_all", [128, 4, 20], F32, 10240)
            for bi in range(4):
                P.ts(xn32.v, xg[:, bi, :], stat[:, 4 + bi:5 + bi], None, ALU.mult)
                pss = [P.next_ps(), P.next_ps()]
                for k in range(8):
                    P.mm(pss[k // 4][:, (k % 4) * 128:(k % 4 + 1) * 128],
                         xn32[:, k * 128:(k + 1) * 128], identf)
                for k in range(8):
                    src = pss[k // 4][:, (k % 4) * 128:(k % 4 + 1) * 128]
                    if k % 2 == 0:
                        P.act(h32[:, k, :], src, AF.Identity, bias=shcol[:, k:k + 1], scale=scol[:, k:k + 1])
                    else:
                        P.ts(h32[:, k, :], src, scol[:, k:k + 1], shcol[:, k:k + 1], ALU.mult, ALU.add)
                P.copy(hT[:, :, bi * 128:(bi + 1) * 128], h32.v, eng="pool")
                psr = P.next_ps()
                for k in range(8):
                    P.mm(psr[:, 0:20], h32[:, k, :], wr_sb[:, k, :], start=(k == 0), stop=(k == 7))
                P.tt(lg_all[:, bi, :], psr[:, 0:20], rbias.v, ALU.add)
            router_all()

    def proj_tok(wb, c0, n, bi):
        ps = P.next_ps()
        for k in range(8):
            P.mm(ps[:, 0:n], hT[:, k, bi * 128:(bi + 1) * 128], wb[:, k, c0:c0 + n],
                 start=(k == 0), stop=(k == 7))
        return ps

    def proj_feat(wb, c0, m):
        ps = P.next_ps()
        for k in range(8):
            P.mm(ps[0:m, :], wb[:, k, c0:c0 + m], hT[:, k, :], start=(k == 0), stop=(k == 7))
        return ps

    def router_all():
        BIG = 1.0e30
        o = [10240 + 320]

        def fld(name, n):
            b_ = P.ar("r_" + name, [128, 4, n], F32, o[0])
            o[0] += ((4 * n * 4 + 31) // 32) * 32
            return b_

        lg_all = P.ar("lg_all", [128, 4, 20], F32, 10240)
        gmax, gsum, pg, m1, m2, dd, e21, w1, w1p, w2p = [fld(n_, 1) for n_ in
                                                         ("gmax", "gsum", "pg", "m1", "m2", "dd", "e21", "w1", "w1p", "w2p")]
        d4, ge, oh, negm = [fld(n_, 4) for n_ in ("d4", "ge", "oh", "negm")]
        elm, oh1, elm2, oh2, cmb, cmb2 = [fld(n_, 16) for n_ in ("elm", "oh1", "elm2", "oh2", "cmb", "cmb2")]
        f2 = lambda t: t.v.rearrange("p b o -> p (b o)")
        bc = lambda t, n: t.v.to_broadcast([128, 4, n])
        lgg = lg_all[:, :, 0:4]
        lge = lg_all[:, :, 4:20]
        P.reduce(f2(gmax), lgg, ALU.max)
        P.tt(d4.v, lgg, bc(gmax, 4), ALU.subtract)
        P.act(ge.v, d4.v, AF.Exp)
        P.reduce(f2(gsum), ge.v, ALU.add)
        P.recip(pg.v, gsum.v)
        P.tt(oh.v, lgg, bc(gmax, 4), ALU.is_equal)
        P.ts(negm.v, oh.v, 1.0, BIG, ALU.subtract, ALU.mult)
        P.tt(elm.v.rearrange("p b (g e) -> p b g e", g=4), lge.rearrange("p b (g e) -> p b g e", g=4),
             negm.v.rearrange("p b (g o) -> p b g o", o=1).to_broadcast([128, 4, 4, 4]), ALU.add)
        P.reduce(f2(m1), elm.v, ALU.max)
        P.tt(oh1.v, elm.v, bc(m1, 16), ALU.is_equal)
        P.stt(elm2.v, oh1.v, -BIG, elm.v, ALU.mult, ALU.add)
        P.reduce(f2(m2), elm2.v, ALU.max)
        P.tt(oh2.v, elm2.v, bc(m2, 16), ALU.is_equal)
        P.tt(dd.v, m2.v, m1.v, ALU.subtract)
        P.act(e21.v, dd.v, AF.Exp)
        P.ts(w1.v, e21.v, 1.0, None, ALU.add)
        P.recip(w1.v, w1.v)
        P.tt(w1p.v, w1.v, pg.v, ALU.mult)
        P.tt(w2p.v, w1p.v, e21.v, ALU.mult)
        P.tt(cmb.v, oh1.v, bc(w1p, 16), ALU.mult)
        P.tt(cmb2.v, oh2.v, bc(w2p, 16), ALU.mult)
        P.tt(combb.v, cmb.v, cmb2.v, ALU.add)

    for l in range(L):
        xsrc = x_in if l == 0 else out
        sched_layer_mod(l)
        for _g in range(NG):
            sched_group(l)
        P.dma("sp", prm_sb.v, prm[l])
        P.dma("pool", w2_sb.v, w2[l])
        P.dma("pool", bg_sb.v, prow[l][:, RW_BG:RW_BG + 256])
        P.dma("sp", wr_sb.v, wr[l].rearrange("(k p) n -> p k n", p=128))
        P.dma("sp", rbias.v, prow[l][:, RW_BR:RW_BR + 20].partition_broadcast(128))
        wl = w_in[l].rearrange("(k p) n -> p k n", p=128)
        wload(wsm[:, :, WSM_GA:WSM_GA + 16], wl[:, :, OFF["ga"]:OFF["ga"] + 16])
        wload(wsm[:, :, WSM_IQ:WSM_N], wl[:, :, OFF["diq"]:OFF["diq"] + 324])
        cc32 = P.ar("cc32", [128, 8], F32, 0)
        P.dma("sp", cc32.v, ccol.v)
        P.act(cactT.v, cc32.v, AF.Silu)
        psm = P.next_ps()
        for i in range(12):
            wb = next_wb()
            for jj in range(4):
                j = i * 4 + jj
                for k in range(8):
                    P.mm(psm[:, j:j + 1], wb[:, k, jj * 128:(jj + 1) * 128], cactT[:, k:k + 1],
                         start=(k == 0), stop=(k == 7))
            rel_wb(wb)
        P.tt(modT.v, psm[:, 0:48], prm_sb[:, PR_BMOD:PR_BMOD + 48], ALU.add)
        P.stt(sT[:, 0:8], modT[:, 8:16], 1.0, prm_sb[:, PR_G1:PR_G1 + 8], ALU.add, ALU.mult)
        P.stt(sT[:, 8:16], modT[:, 32:40], 1.0, prm_sb[:, PR_G2:PR_G2 + 8], ALU.add, ALU.mult)
        ws32 = P.ar("ws32", [128, 4, 128], F32, 2048)
        P.dma("sp", ws32.v, wsT[l])
        P.tt(ws32.v, ws32.v, trif.rearrange("p (o t) -> p o t", o=1).to_broadcast([128, 4, 128]), ALU.mult)
        P.copy(WsTm.v, ws32.v)
        bsrow = P.ar("bsrow", [1, 512], F32, 4096)
        P.dma("sp", bsrow.v, prow[l][:, RW_BS:RW_BS + 512])
        bsb = P.ar("bsb", [128, 4, 128], F32, 6144)
        for g in range(4):
            ps1 = P.next_ps()
            P.mm(ps1[:, 0:128], onesf[0:1, :], bsrow[:, g * 128:(g + 1) * 128])
            P.copy(bsb[:, g, :], ps1[:, 0:128], eng="act")
            P.mm(ps1[:, 128:256], onesf, ws32[:, g, :])
            P.stt(Cg[:, g, :], ps1[:, 128:256], prm_sb[:, PR_LNB + g:PR_LNB + g + 1], bsb[:, g, :],
                  ALU.mult, ALU.add)
        P.ts(stat[:, 32:33], prm_sb[:, PR_GQ:PR_GQ + 1], float(128 ** -0.5), None, ALU.mult)
        gq_col = stat[:, 32:33]
        gk_col = prm_sb[:, PR_GK:PR_GK + 1]
        gng_col = prm_sb[:, PR_GNG:PR_GNG + 1]
        P.memset(Sf.v, 0.0)
        P.memset(Sb.v, 0.0)

        def gt_bcast(col0, o_gtb, o_dg):
            gtb = P.ar("gtb", [128, D], F32, o_gtb)
            dg = P.ar("dg", [128, 128], F32, o_dg)
            pss = [P.next_ps(), P.next_ps()]
            for k in range(8):
                P.ts(dg.v, identf, modT[:, col0 + k:col0 + k + 1], None, ALU.mult)
                P.mm(pss[k // 4][:, (k % 4) * 128:(k % 4 + 1) * 128], onesf, dg.v)
            P.copy(gtb[:, 0:512], pss[0].v, eng="act")
            P.copy(gtb[:, 512:1024], pss[1].v, eng="act")
            return gtb

        for g in range(NG):
            for bi in range(4):
                gb = g * 4 + bi
                P.dma("sp", xg[:, bi, :], (xsrc if l == 0 else out.sub(gb))[gb * 128:(gb + 1) * 128, :])
            P.mark(f'g{g}_start')
            norm_to_hT(sT[:, 0:8], modT[:, 0:8], False)
            P.mark(f'g{g}_norm1')
            dump("modT", modT.v, [128, 48], F32)
            dump("hT", hT.v, [128, 8, 512], BF16)

            uT = P.ar("uT", [128, 4, 512], BF16, 8192)
            vg = P.ar("vg", [128, 4, 512], F32, 12288)
            vn = P.ar("vn", [128, 4, 512], BF16, 20480)
            gtmp = [P.ar(f"gtmp{i}", [128, 128], F32, 24576 + 512 * i) for i in range(2)]
            gjunk = P.ar("gjunk", [128, 512], BF16, 25600)
            wu = next_wb()
            for c in range(4):
                ps = proj_feat(wu, c * 128, 128)
                P.act(uT[:, c, :], ps.v, AF.Gelu_apprx_tanh)
            rel_wb(wu)
            wv = next_wb()
            for bi in range(4):
                ps = proj_tok(wv, 0, 512, bi)
                P.act(vg[:, bi, :], ps.v, AF.Gelu_apprx_tanh, accum=stat[:, 8 + bi:9 + bi])
                P.act(gjunk.v, vg[:, bi, :], AF.Square, accum=stat[:, 12 + bi:13 + bi])
            rel_wb(wv)
            mean = stat[:, 16:20]
            P.ts(mean, stat[:, 8:12], 1.0 / 512, None, ALU.mult)
            msq = stat[:, 20:24]
            P.tt(msq, mean, mean, ALU.mult)
            var = stat[:, 24:28]
            P.stt(var, stat[:, 12:16], 1.0 / 512, msq, ALU.mult, ALU.subtract)
            small_rstd(var, var, 1.0)
            for bi in range(4):
                P.ts(vn[:, bi, :], vg[:, bi, :], stat[:, 16 + bi:17 + bi], stat[:, 24 + bi:25 + bi],
                     ALU.subtract, ALU.mult)
            for bi in range(4):
                ps = P.next_ps()
                for gg in range(4):
                    P.mm(ps[:, gg * 128:(gg + 1) * 128], vn[:, bi, gg * 128:(gg + 1) * 128], WsTm[:, gg, :])
                for gg in range(4):
                    tmp = gtmp[gg % 2]
                    P.stt(tmp.v, ps[:, gg * 128:(gg + 1) * 128], prm_sb[:, PR_LNG + gg:PR_LNG + gg + 1],
                          Cg[:, gg, :], ALU.mult, ALU.add)
                    P.tt(ysT[:, 0, gg, bi * 128:(bi + 1) * 128], tmp.v, uT[:, gg, bi * 128:(bi + 1) * 128],
                         ALU.mult)

            P.mark(f'g{g}_gmlp')
            gaT = P.ar("gaT", [16, 512], BF16, 8192)
            ps = proj_feat(wsm, WSM_GA, 16)
            P.copy(gaT.v, ps[0:16, :])
            qk_all = P.ar("qk_all", [128, 4, 512], F32, 9216)
            v_allg = P.ar("v_allg", [128, 4, 512], BF16, 17408)
            r_all = P.ar("r_all", [128, 4, 512], BF16, 21504)
            l_sb = P.ar("l_sb", [128, 256], F32, 25600)
            e_sb = P.ar("e_sb", [128, 256], F32, 26624)
            eb = P.ar("eb", [128, 256], F32, 27648)
            ebp = P.ar("ebp", [128, 256], F32, 28672)
            ebl = P.ar("ebl", [128, 256], F32, 29696)
            tmpk = P.ar("tmpk", [128, 256], F32, 30720)
            qe = P.ar("qe", [128, 256], BF16, 31744)
            ke = P.ar("ke", [128, 256], BF16, 32256)
            kd = P.ar("kd", [128, 256], BF16, 32768)
            qkT = P.ar("qkT", [64, 8, 128], BF16, 33280)
            ATm = P.ar("ATm", [128, 4, 128], BF16, 35328)
            yb = P.ar("yb", [128, 512], BF16, 36352)
            Dcol = P.ar("Dcol", [64, 4], F32, 37376)
            oj = P.ar("oj", [128, 128], BF16, 37888)
            wqk = next_wb()
            for bi in range(4):
                ps = proj_tok(wqk, 0, 512, bi)
                P.copy(qk_all[:, bi, :], ps.v, eng="act")
            rel_wb(wqk)
            wgv = next_wb()
            for bi in range(4):
                ps = proj_tok(wgv, 0, 512, bi)
                P.copy(v_allg[:, bi, :], ps.v)
            rel_wb(wgv)
            wgr = next_wb()
            for bi in range(4):
                ps = proj_tok(wgr, 0, 512, bi)
                P.act(r_all[:, bi, :], ps.v, AF.Silu)
            rel_wb(wgr)
            for bi in range(4):
                bs = slice(bi * 128, (bi + 1) * 128)
                qk_sb = qk_all[:, bi, :]
                v_sb = v_allg[:, bi, :]
                r_sb = r_all[:, bi, :]
                psz = P.next_ps()
                P.mm(psz[:, 0:256], gaT[:, bs], w2_sb.v, start=True, stop=False)
                P.mm(psz[:, 0:256], ones1.v, bg_sb.v, start=False, stop=True)
                P.act(e_sb.v, psz[:, 0:256], AF.Exp, scale=-1.0)
                P.act(l_sb.v, e_sb.v, AF.Ln, bias=1.0)
                psc = P.next_ps()
                P.mm(psc[:, 0:256], trif, l_sb.v)
                P.mm(psc[:, 256:512], onesf, l_sb.v)
                psd = P.next_ps()
                for h in range(4):
                    P.mm(psd[0:64, h:h + 1], l_sb[:, h * 64:(h + 1) * 64], onesf[:, 0:1])
                P.act(eb.v, psc[:, 0:256], AF.Exp, scale=-1.0 / 16)
                P.act(ebp.v, psc[:, 0:256], AF.Exp, scale=1.0 / 16)
                P.act(ebl.v, psc[:, 256:512], AF.Exp, scale=-1.0 / 16)
                P.act(Dcol.v, psd[0:64, 0:4], AF.Exp, scale=-1.0 / 16)
                P.stt(qe.v, qk_sb[:, 0:256], 0.125, eb.v, ALU.mult, ALU.mult)
                P.tt(ke.v, qk_sb[:, 256:512], ebp.v, ALU.mult)
                P.tt(tmpk.v, ebp.v, ebl.v, ALU.mult)
                P.tt(kd.v, qk_sb[:, 256:512], tmpk.v, ALU.mult)
                pst = P.next_ps()
                pstb = pst.v.bitcast(BF16)
                for h in range(4):
                    P.tr(pstb[0:64, h * 128:(h + 1) * 128], qe[:, h * 64:(h + 1) * 64], identb.v)
                    P.tr(pstb[0:64, (4 + h) * 128:(5 + h) * 128], ke[:, h * 64:(h + 1) * 64], identb.v)
                P.copy(qkT.v.rearrange("p a b -> p (a b)"), pstb[0:64, :], eng="act")
                psa = P.next_ps()
                for h in range(4):
                    P.mm(psa[:, h * 128:(h + 1) * 128], qkT[:, 4 + h, :], qkT[:, h, :])
                P.tt(ATm.v, psa.v.rearrange("p (h t) -> p h t", h=4),
                     trif.rearrange("p (o t) -> p o t", o=1).to_broadcast([128, 4, 128]), ALU.mult)
                pso = P.next_ps()
                for h in range(4):
                    hs = slice(h * 128, (h + 1) * 128)
                    P.mm(pso[:, hs], ATm[:, h, :], v_sb[:, hs], start=True, stop=False)
                    P.mm(pso[:, hs], qkT[:, h, :], Sb[:, h, :], start=False, stop=True)
                psu = P.next_ps()
                for h in range(4):
                    P.mm(psu[0:64, h * 128:(h + 1) * 128], kd[:, h * 64:(h + 1) * 64], v_sb[:, h * 128:(h + 1) * 128])
                for h in range(4):
                    P.stt(Sf[:, h, :], Sf[:, h, :], Dcol[:, h:h + 1], psu[0:64, h * 128:(h + 1) * 128],
                          ALU.mult, ALU.add)
                P.copy(Sb.v, Sf.v, eng="pool")
                for h in range(4):
                    P.act(oj.v, pso[:, h * 128:(h + 1) * 128], AF.Square, accum=stat[:, 36 + h:37 + h])
                small_rstd(stat[:, 36:40], stat[:, 36:40], 1.0 / 128)
                for h in range(4):
                    hs = slice(h * 128, (h + 1) * 128)
                    P.stt(yb[:, hs], pso[:, hs], stat[:, 36 + h:37 + h], r_sb[:, hs], ALU.mult, ALU.mult)
                pst2 = P.next_ps()
                pst2b = pst2.v.bitcast(BF16)
                for h in range(4):
                    P.tr(pst2b[:, h * 128:(h + 1) * 128], yb[:, h * 128:(h + 1) * 128], identb.v)
                P.act(ysT[:, 1, :, bs], pst2b[:, 0:512].rearrange("p (h t) -> p h t", h=4), AF.Identity,
                      scale=gng_col)

            P.mark(f'g{g}_gla')
            kn = P.ar("kn", [128, 512], BF16, 8192)
            qT = P.ar("qT", [128, 4, 512], BF16, 9216)
            iqT = P.ar("iqT", [64, 4, 512], BF16, 13312)
            iw_sb = P.ar("iw_sb", [128, 4, 4], F32, 17408)
            relu_t = [P.ar("relu0", [128, 512], F32, 17920)]
            PT = [P.ar(f"PT{i}", [128, 512], BF16, 19968 + 1024 * i) for i in range(2)]
            yc = P.ar("yc", [128, 512], BF16, 22016)
            bjunk = P.ar("bjunk", [128, 128], BF16, 23040)
            score = P.ar("score", [128, S], F32, 23552)
            maskbuf = [P.ar(f"maskbuf{i}", [128, S], BF16, 23552 + 4 * S + 2 * S * i) for i in range(2)]
            cD = max(max(1, n_ * 15 // 32) for n_ in range(1, NBLK + 1)) * 128
            cA = max(n_ - max(1, n_ * 15 // 32) for n_ in range(1, NBLK + 1)) * 128
            assert cD + cA <= S
            jDb = [P.ar(f"jD{i}", [128, cD], BF16, 23552 + 4 * S + 2 * S * i) for i in range(2)]
            jAb = [P.ar(f"jA{i}", [128, cA], BF16, 23552 + 4 * S + 2 * S * i + 2 * cD) for i in range(2)]

            def qk_norm_T(ps, gcol, dst):
                for h in range(4):
                    P.act(bjunk.v, ps[:, h * 128:(h + 1) * 128], AF.Square, accum=stat[:, 40 + h:41 + h])
                small_rstd(stat[:, 40:44], stat[:, 40:44], 1.0 / 128)
                for h in range(4):
                    P.ts(kn[:, h * 128:(h + 1) * 128], ps[:, h * 128:(h + 1) * 128], stat[:, 40 + h:41 + h],
                         None, ALU.mult)
                pt = P.next_ps()
                ptb = pt.v.bitcast(BF16)
                for h in range(4):
                    P.tr(ptb[:, h * 128:(h + 1) * 128], kn[:, h * 128:(h + 1) * 128], identb.v)
                P.act(dst, ptb[:, 0:512].rearrange("p (h t) -> p h t", h=4), AF.Identity, scale=gcol)

            for bi in range(4):
                gb = g * 4 + bi
                bs = slice(bi * 128, (bi + 1) * 128)
                psw = P.next_ps()
                for k in range(8):
                    P.mm(psw[:, 0:4], hT[:, k, bs], wsm[:, k, WSM_IW:WSM_IW + 4], start=(k == 0), stop=(k == 7))
                P.copy(iw_sb[:, bi, :], psw[:, 0:4])
            ps = proj_feat(wsm, WSM_IK, 64)
            P.copy(ikT_all[:, g * 512:(g + 1) * 512], ps[0:64, :], eng="act")
            for h in range(4):
                ps = proj_feat(wsm, WSM_IQ + h * 64, 64)
                P.copy(iqT[:, h, :], ps[0:64, :], eng="act")


            def IB_gen(bi):
                gb = g * 4 + bi
                bs = slice(bi * 128, (bi + 1) * 128)
                nk = (gb + 1) * 128
                nch = (nk + 511) // 512
                mk = maskbuf[bi % 2]
                for c in range(nch):
                    c0, c1 = c * 512, min((c + 1) * 512, nk)
                    w = c1 - c0
                    for h in range(4):
                        psi = P.next_ps()
                        P.mm(psi[:, 0:w], iqT[:, h, bs], ikT_all[:, c0:c1])
                        rl = relu_t[0]
                        P.act(rl[:, 0:w], psi[:, 0:w], AF.Relu)
                        if h == 0:
                            P.ts(score[:, c0:c1], rl[:, 0:w], iw_sb[:, bi, 0:1], None, ALU.mult)
                        else:
                            P.stt(score[:, c0:c1], rl[:, 0:w], iw_sb[:, bi, h:h + 1], score[:, c0:c1],
                                  ALU.mult, ALU.add)
                    yield
                P.tt(score[:, gb * 128:nk], score[:, gb * 128:nk], negtri, ALU.add)
                tau = stat[:, 44:45]
                if gb < 2:
                    P.memset(tau, -1.0e29)
                else:
                    rng = stat[:, 45:46]
                    mid = stat[:, 46:47]
                    cnt = stat[:, 47:48]
                    u = stat[:, 48:49]
                    hi = stat[:, 49:50]
                    lo = stat[:, 50:51]
                    P.ts(mk[:, 0:nk], score[:, 0:nk], 0.0, -3.0e38, ALU.add, ALU.max, accum=hi)
                    P.ts(mk[:, 0:256], score[:, 0:256], 0.0, 3.0e38, ALU.add, ALU.min, accum=lo)
                    P.tt(rng, hi, lo, ALU.subtract)
                    P.ts(offs.v, fv.v, rng, None, ALU.mult)
                    P.tt(mid, lo, offs[:, 0:1], ALU.add)
                    for it in range(NBIS):
                        P.ts(mk[:, 0:nk], score[:, 0:nk], mid, 0.0, ALU.is_ge, ALU.add, accum=cnt)
                        last = (it == NBIS - 1)
                        P.ts(u, cnt, 256.0, 1.0 if last else 0.5, ALU.is_ge, ALU.subtract)
                        P.stt(tau if last else mid, u, offs[:, it:it + 1], mid, ALU.mult, ALU.add)
                        yield
                P.ts(mk[:, 0:nk], score[:, 0:nk], tau, NEG, ALU.is_lt, ALU.mult)
                yield

            def drain(gen):
                for _ in gen:
                    pass

            def interleave(ga, gb_):
                a_done = b_done = False
                while not (a_done and b_done):
                    if not a_done:
                        try:
                            next(ga)
                        except StopIteration:
                            a_done = True
                    if not b_done:
                        try:
                            next(gb_)
                        except StopIteration:
                            b_done = True

            def ATT_gen(bi):
                gb = g * 4 + bi
                bs = slice(bi * 128, (bi + 1) * 128)
                mk = maskbuf[bi % 2]
                P.memset(psA.v, 0.0)
                P.memset(psB.v, 0.0)
                def LG(j):
                    psl = P.next_ps()
                    dist = gb - j
                    P.mm(psl.v, mk[:, j * 128:(j + 1) * 128], ident4.v, start=True, stop=False)
                    if dist <= 1:
                        P.mm(psl.v, identb.v, biasT[:, dist, :, :].rearrange("p h t -> p (h t)"),
                             start=False, stop=False)
                    for h in range(4):
                        hs = slice(h * 128, (h + 1) * 128)
                        P.mm(psl[:, hs], kT_all[:, h, j * 128:(j + 1) * 128], qT[:, h, bs],
                             start=False, stop=(h == 3))
                    return psl

                psl = LG(0)
                P.act(PT[0].v, psl.v, AF.Exp, bias=-6.0)
                for j in range(gb + 1):
                    pt = PT[j % 2]
                    if j + 1 <= gb:
                        psl_next = LG(j + 1)
                    yield
                    for h in range(4):
                        acc = psA if h < 2 else psB
                        a0 = (h % 2) * 130
                        P.mm(acc[:, a0:a0 + 129], pt[:, h * 128:(h + 1) * 128], v_all[:, j, h, 0:129],
                             start=False, stop=False, skip=True)
                    if j + 1 <= gb:
                        P.act(PT[(j + 1) % 2].v, psl_next.v, AF.Exp, bias=-6.0)
                for h in range(4):
                    acc = psA if h < 2 else psB
                    a0 = (h % 2) * 130
                    P.recip(stat[:, 52 + h:53 + h], acc[:, a0 + 128:a0 + 129])
                    P.ts(yc[:, h * 128:(h + 1) * 128], acc[:, a0:a0 + 128], stat[:, 52 + h:53 + h], None, ALU.mult)
                pt2 = P.next_ps()
                pt2b = pt2.v.bitcast(BF16)
                for h in range(4):
                    P.tr(pt2b[:, h * 128:(h + 1) * 128], yc[:, h * 128:(h + 1) * 128], identb.v)
                P.copy(ysT[:, 2, :, bs], pt2b[:, 0:512].rearrange("p (h t) -> p h t", h=4), eng="act")

            def projA_gen():
                wdq = next_wb()
                for bi in range(4):
                    bs = slice(bi * 128, (bi + 1) * 128)
                    ps = proj_tok(wdq, 0, 512, bi)
                    qk_norm_T(ps, gq_col, qT[:, :, bs])
                    yield
                rel_wb(wdq)
                wdk = next_wb()
                for bi in range(4):
                    gb = g * 4 + bi
                    ps = proj_tok(wdk, 0, 512, bi)
                    qk_norm_T(ps, gk_col, kT_all[:, :, gb * 128:(gb + 1) * 128])
                    yield
                rel_wb(wdk)
                wdv = next_wb()
                for bi in range(4):
                    gb = g * 4 + bi
                    ps = proj_tok(wdv, 0, 512, bi)
                    P.copy(v_all[:, gb, :, 0:128], ps.v.rearrange("p (h e) -> p h e", h=4))
                    yield
                rel_wb(wdv)

            interleave(projA_gen(), IB_gen(0))
            P.mark(f'g{g}_dsaproj')
            for bi in range(4):
                if bi + 1 < 4:
                    interleave(ATT_gen(bi), IB_gen(bi + 1))
                else:
                    drain(ATT_gen(bi))

            P.mark(f'g{g}_dsa')
            dump("ysT", ysT.v, [128, 3, 4, 512], BF16)
            dump("kT", kT_all[:, :, 0:512], [128, 4, 512], BF16)
            mergedF = P.ar("mergedF", [128, 8, 512], F32, 8192)
            mergedT = P.ar("mergedT", [128, 8, 512], BF16, 24576)
            sgt = [P.ar(f"sgt{i}", [128, 512], BF16, 32768 + 1024 * i) for i in range(2)]
            mtmp = [P.ar(f"mtmp{i}", [128, 512], F32, 34816 + 2048 * i) for i in range(2)]
            ci = 0
            for n in range(3):
                wbr = next_wb()
                wbrv = wbr.v.rearrange("p a b -> p (a b)").rearrange("p (e d) -> p e d", e=4)
                for half in range(2):
                    wg = next_wb()
                    for cc in range(4):
                        dc = half * 4 + cc
                        psg = proj_feat(wg, cc * 128, 128)
                        sg = sgt[ci % 2]
                        P.act(sg.v, psg.v, AF.Sigmoid, bias=prm_sb[:, PR_BBG + n * 8 + dc:PR_BBG + n * 8 + dc + 1])
                        psu = P.next_ps()
                        for ec in range(4):
                            P.mm(psu.v, wbrv[:, ec, dc * 128:(dc + 1) * 128], ysT[:, n, ec, :],
                                 start=(ec == 0), stop=(ec == 3))
                        if n == 0:
                            P.tt(mergedF[:, dc, :], psu.v, sg.v, ALU.mult)
                        else:
                            mt = mtmp[ci % 2]
                            P.tt(mt.v, psu.v, sg.v, ALU.mult)
                            if n == 1:
                                P.tt(mergedF[:, dc, :], mergedF[:, dc, :], mt.v, ALU.add, eng="pool")
                            else:
                                P.tt(mergedT[:, dc, :], mergedF[:, dc, :], mt.v, ALU.add, eng="pool")
                        ci += 1
                    rel_wb(wg)
                rel_wb(wbr)
            P.mark(f'g{g}_merge')
            gtb = gt_bcast(16, 40960, 45056)
            for half in range(2):
                wo = next_wb()
                for bi in range(4):
                    ps = P.next_ps()
                    for k in range(8):
                        P.mm(ps.v, mergedT[:, k, bi * 128:(bi + 1) * 128], wo[:, k, :], start=(k == 0), stop=(k == 7))
                    mt = mtmp[bi % 2]
                    P.tt(mt.v, ps.v, gtb[:, half * 512:(half + 1) * 512], ALU.mult)
                    P.tt(xg[:, bi, half * 512:(half + 1) * 512], xg[:, bi, half * 512:(half + 1) * 512], mt.v,
                         ALU.add, eng="pool")
                rel_wb(wo)

            dump("mergedT", mergedT.v, [128, 8, 512], BF16)
            dump("x1", xg.v, [128, 4, D], F32)
            P.mark(f'g{g}_wout')
            norm_to_hT(sT[:, 8:16], modT[:, 24:32], True)
            dump("h2T", hT.v, [128, 8, 512], BF16)
            dump("combb", combb.v, [128, 4, 16], BF16)
            P.mark(f'g{g}_norm2')
            gtb = gt_bcast(40, 16384, 20480)
            cb_sb = P.ar("cb_sb", [128, 512], BF16, 0)
            t1 = [P.ar("t1", [128, 512], BF16, 1024)]
            sgm = [P.ar(f"sgm{i}", [128, 512], BF16, 2048 + 1024 * i) for i in range(2)]
            actT = P.ar("actT", [128, 4, 2, 512], BF16, 4096)
            mtmp = [P.ar(f"mtmpb{i}", [128, 512], F32, 12288 + 2048 * i) for i in range(2)]
            WD = [P.ar(f"wd{i}", [128, 2, D], BF16, 21504 + 4096 * i) for i in range(6)]
            wdi = 0
            for rnd in range(4):
                wds = []
                for ee in range(4):
                    e = rnd * 4 + ee
                    wgu = next_wb()
                    wd = WD[wdi % 6]
                    wdi += 1
                    wload(wd.v, wed[l][e].rearrange("(f p) d -> p f d", p=128))
                    wds.append(wd)
                    psc = P.next_ps()
                    for bi in range(4):
                        P.mm(psc[:, bi * 128:(bi + 1) * 128], combb[:, bi, e:e + 1].to_broadcast([128, 128]),
                             identb.v)
                    P.copy(cb_sb.v, psc.v, eng="act")
                    for fc in range(2):
                        psg = proj_feat(wgu, fc * 128, 128)
                        psu = proj_feat(wgu, 256 + fc * 128, 128)
                        sg = sgm[fc]
                        P.act(sg.v, psg.v, AF.Silu)
                        P.tt(t1[0].v, psu.v, sg.v, ALU.mult)
                        P.tt(actT[:, ee, fc, :], t1[0].v, cb_sb.v, ALU.mult, eng="pool")
                    rel_wb(wgu)
                for bi in range(4):
                    for half in range(2):
                        ps = P.next_ps()
                        n = 0
                        for ee in range(4):
                            for fc in range(2):
                                P.mm(ps.v, actT[:, ee, fc, bi * 128:(bi + 1) * 128],
                                     wds[ee][:, fc, half * 512:(half + 1) * 512], start=(n == 0), stop=(n == 7))
                                n += 1
                        mt = mtmp[(bi * 2 + half) % 2]
                        P.tt(mt.v, ps.v, gtb[:, half * 512:(half + 1) * 512], ALU.mult)
                        P.tt(xg[:, bi, half * 512:(half + 1) * 512], xg[:, bi, half * 512:(half + 1) * 512], mt.v,
                             ALU.add, eng="pool")
            P.mark(f'g{g}_moe')
            for bi in range(4):
                gb = g * 4 + bi
                P.dma("sp", out.sub(gb)[gb * 128:(gb + 1) * 128, :], xg[:, bi, :])
    P.fence("sp", [out] + list(dbgo.values()))
    stats = P.emit()
    stats['marks'] = getattr(P, 'marks', [])
    return nc, stats


def _bucket_np(rel):
    import math
    n = np.maximum(rel, 0)
    max_exact = 16
    large = max_exact + (np.log(np.maximum(n, max_exact).astype(np.float32) / max_exact)
                         / math.log(128 / max_exact) * (32 - max_exact)).astype(np.int32)
    large = np.minimum(large, 31)
    return np.where(n < max_exact, n, large)


def host_consts():
    cst = np.zeros((128, NCST), np.float32)
    i = np.arange(128)
    cst[:, C_ID:C_ID + 128] = np.eye(128, dtype=np.float32)
    cst[:, C_TRI:C_TRI + 128] = (i[:, None] <= i[None, :]).astype(np.float32)
    cst[:, C_ONES:C_ONES + 128] = 1.0
    cst[:, C_NEGTRI:C_NEGTRI + 128] = np.where(i[None, :] <= i[:, None], 0.0, BIGNEG)
    cst[:, C_BKD:C_BKD + 128] = _bucket_np(i[None, :] - i[:, None]).astype(np.float32)
    cst[:, C_BKN:C_BKN + 128] = _bucket_np(128 + i[None, :] - i[:, None]).astype(np.float32)
    return cst


def host_layout(inputs, L, b, S):
    f = lambda a: np.ascontiguousarray(np.asarray(a, dtype=np.float32))
    col = lambda v: np.asarray(v, np.float32).reshape(-1, 128).T
    m = {}
    m["x"] = f(inputs["x"][b, :S])
    m["ccol"] = f(col(inputs["c"][b]))
    m["cst"] = host_consts()
    m["rbrow"] = f(np.asarray(inputs["rel_bias"]).reshape(1, 128))
    prm = np.zeros((L, 128, NPRM), np.float32)
    prow = np.zeros((L, 1, NROW), np.float32)
    for l in range(L):
        prm[l, :, PR_G1:PR_G1 + 8] = col(inputs["g_norm1"][l])
        prm[l, :, PR_G2:PR_G2 + 8] = col(inputs["g_norm2"][l])
        prm[l, :, PR_BMOD:PR_BMOD + 48] = col(inputs["b_mod"][l])
        prm[l, :, PR_LNG:PR_LNG + 4] = col(inputs["gmlp_ln_g"][l])
        prm[l, :, PR_LNB:PR_LNB + 4] = col(inputs["gmlp_ln_b"][l])
        prm[l, :, PR_GNG] = np.asarray(inputs["gla_norm_g"][l])
        prm[l, :, PR_GQ] = np.asarray(inputs["dsa_qnorm_g"][l])
        prm[l, :, PR_GK] = np.asarray(inputs["dsa_knorm_g"][l])
        prm[l, :, PR_BBG:PR_BBG + 24] = col(np.asarray(inputs["b_branch_gate"][l]).reshape(-1))
        prow[l, 0, RW_BS:RW_BS + 512] = np.asarray(inputs["gmlp_b_s"][l]).reshape(-1)
        prow[l, 0, RW_BG:RW_BG + 256] = np.asarray(inputs["gla_b_gate"][l])
        prow[l, 0, RW_BR:RW_BR + 4] = np.asarray(inputs["b_group"][l])
        prow[l, 0, RW_BR + 4:RW_BR + 20] = np.asarray(inputs["b_router"][l])
    m["prm"] = prm
    m["prow"] = prow
    m["w_mod"] = f(inputs["w_mod"][:L])
    m["w_in"] = f(inputs["w_in"][:L])
    m["wsT"] = f(np.asarray(inputs["gmlp_w_s"][:L]).transpose(0, 3, 1, 2))
    m["w2"] = f(inputs["gla_w_gate2"][:L])
    m["w_branch"] = f(inputs["w_branch"][:L])
    m["w_out"] = f(inputs["w_out"][:L])
    m["wr"] = f(np.concatenate([np.asarray(inputs["w_group"][:L]), np.asarray(inputs["w_router"][:L])], axis=2))
    m["w_exp_gate"] = f(inputs["w_exp_gate"][:L])
    m["w_exp_up"] = f(inputs["w_exp_up"][:L])
    m["w_exp_down"] = f(inputs["w_exp_down"][:L])
    return m


_PROG_CACHE = {}


def run_model(inputs, L, S, batches, n_cores, want_all=False, active=None):
    key = (L, S)
    if key not in _PROG_CACHE:
        _PROG_CACHE[key] = build_program(L, S)
    nc, stats = _PROG_CACHE[key]
    if active is None:
        active = list(range(min(n_cores, len(batches))))
    shared = host_layout(inputs, L, batches[0], S)
    zeros = None
    in_maps = []
    for ci in range(n_cores):
        if ci in active:
            b = batches[active.index(ci)]
            m = dict(shared)
            m["x"] = np.ascontiguousarray(np.asarray(inputs["x"][b, :S], dtype=np.float32))
            m["ccol"] = np.ascontiguousarray(np.asarray(inputs["c"][b], np.float32).reshape(-1, 128).T)
        else:
            if zeros is None:
                zeros = {k: np.zeros_like(v) for k, v in shared.items()}
            m = dict(zeros)
        in_maps.append(m)
    res = run_bass_kernel_spmd(nc, in_maps, core_ids=list(range(n_cores)))
    if want_all:
        return res.results
    return [res.results[ci]["out"] for ci in active]


def kernel(**inputs):
    L, B, S = 4, 4, 4096
    outs = run_model(inputs, L, S, list(range(B)), 8, active=[0, 1, 4, 5])
    return np.stack(outs, axis=0).astype(np.float32)
```

```python
import numpy as np
import concourse.bass as bass
import concourse.mybir as mybir

F32 = mybir.dt.float32
BF16 = mybir.dt.bfloat16
AF = mybir.ActivationFunctionType
ALU = mybir.AluOpType
AX = mybir.AxisListType

GRAN = 512
SEM_CAP = 30000
STRICT_SAME_ENGINE = True


class Region:
    __slots__ = ("lw", "rd")

    def __init__(self):
        self.lw = None
        self.rd = {}


class View:
    __slots__ = ("buf", "ap")

    def __init__(self, buf, ap):
        self.buf = buf
        self.ap = ap

    def __getitem__(self, idx):
        return View(self.buf, self.ap[idx])

    def rearrange(self, s, **kw):
        return View(self.buf, self.ap.rearrange(s, **kw))

    def bitcast(self, dt):
        return View(self.buf, self.ap.bitcast(dt))

    def to_broadcast(self, shape):
        return View(self.buf, self.ap.to_broadcast(shape))

    def partition_broadcast(self, n):
        return View(self.buf, self.ap.partition_broadcast(n))


class Buf:
    def __init__(self, name, h, regions, is_dram=False):
        self.name = name
        self.h = h
        self.regions = regions
        self.is_dram = is_dram

    def __getitem__(self, idx):
        base = self.h.ap() if self.is_dram else self.h
        return View(self, base[idx])

    @property
    def v(self):
        base = self.h.ap() if self.is_dram else self.h[:]
        return View(self, base)

    def sub(self, i, n=1):
        return Buf(self.name, self.h, self.regions[i:i + n], self.is_dram)


class Op:
    __slots__ = ("eng", "fn", "kind", "deps", "lidx", "flag", "waits", "clock",
                 "fnum", "dma_sem", "dma_val", "dq_n")


class Prog:
    ENGS = ("pe", "act", "dve", "pool", "sp")

    def __init__(self, nc, n_dma_sems=None):
        self.nc = nc
        self.h = {"pe": nc.tensor, "act": nc.scalar, "dve": nc.vector,
                  "pool": nc.gpsimd, "sp": nc.sync}
        self.ops = []
        self.n_dma_sems = n_dma_sems or {"sp": 16, "pool": 16, "act": 4}
        self.dma_count = {e: 0 for e in self.ENGS}
        self.dma_ids = {e: [] for e in self.ENGS}
        self.arena_base = None
        self.arena_regions = None
        self.psum_rot = []
        self.psum_i = 0

    def sb(self, name, shape, dtype):
        h = self.nc.alloc_sbuf_tensor(name, list(shape), dtype)
        return Buf(name, h, [Region()])

    def make_arena(self, nbytes):
        r = self.nc.bump_sbuf(nbytes)
        self.arena_base = r[0]
        self.arena_size = nbytes
        self.arena_regions = [Region() for _ in range((nbytes + GRAN - 1) // GRAN)]

    def ar(self, name, shape, dtype, off):
        key = (name, tuple(shape), str(dtype), off)
        if not hasattr(self, "_arc"):
            self._arc = {}
        if key in self._arc:
            return self._arc[key]
        b = self._ar(f"{name}_{len(self._arc)}", shape, dtype, off)
        self._arc[key] = b
        return b

    def _ar(self, name, shape, dtype, off):
        esz = {F32: 4, BF16: 2}[dtype]
        n = int(np.prod(shape[1:])) * esz
        assert off % 32 == 0 and off + n <= self.arena_size, (name, off, n, self.arena_size)
        h = self.nc.alloc_sbuf_tensor_at(name, list(shape), dtype, offset=self.arena_base + off)
        regs = self.arena_regions[off // GRAN:(off + n + GRAN - 1) // GRAN]
        return Buf(name, h, regs)

    def ps(self, name, shape=(128, 512), dtype=F32):
        h = self.nc.alloc_psum_tensor(name, list(shape), dtype)
        return Buf(name, h, [Region()])

    def dram(self, name, shape, dtype, kind="Internal", nreg=1):
        h = self.nc.dram_tensor(name, list(shape), dtype, kind=kind)
        return Buf(name, h, [Region() for _ in range(nreg)], is_dram=True)

    def fence(self, eng, bufs):
        h = self.h[eng]
        self.add(eng, lambda: h.nop(), bufs, [])

    def mark(self, name):
        if not getattr(self, "marks_on", False):
            return
        last = {}
        for oid in range(len(self.ops) - 1, -1, -1):
            o = self.ops[oid]
            if o.kind == "c" and o.eng != "sp" and o.eng not in last:
                last[o.eng] = oid
            if len(last) == 4:
                break
        h = self.h["sp"]
        oid = self.add("sp", lambda: h.nop(), [], [])
        for e, d in last.items():
            self.ops[oid].deps[d] = True
        if not hasattr(self, "marks"):
            self.marks = []
        self.marks.append(name)

    def next_ps(self):
        b = self.psum_rot[self.psum_i % len(self.psum_rot)]
        self.psum_i += 1
        return b

    def add(self, eng, fn, reads, writes, kind="c"):
        op = Op()
        op.eng = eng
        op.fn = fn
        op.kind = kind
        op.flag = False
        op.deps = {}
        oid = len(self.ops)
        rregs = []
        for b in reads:
            if b is None:
                continue
            b = b.buf if isinstance(b, View) else b
            rregs.extend(b.regions)
        wregs = []
        for b in writes:
            b = b.buf if isinstance(b, View) else b
            wregs.extend(b.regions)
        for r in rregs:
            if r.lw is not None:
                op.deps[r.lw] = True
        for r in wregs:
            if r.lw is not None:
                op.deps.setdefault(r.lw, False)
            for rid in r.rd.values():
                op.deps.setdefault(rid, False)
        op.deps.pop(oid, None)
        for r in rregs:
            key = eng if kind == "c" else ("dma", oid)
            r.rd[key] = oid
        for r in wregs:
            r.lw = oid
            r.rd = {}
        if kind == "dma":
            n = self.dma_count[eng]
            self.dma_count[eng] += 1
            op.dq_n = n
            self.dma_ids[eng].append(oid)
        self.ops.append(op)
        return oid

    @staticmethod
    def _a(x):
        return x.ap if isinstance(x, View) else x

    def mm(self, out, lhsT, rhs, start=True, stop=True, skip=False):
        nc = self.nc
        if skip:
            self.add("pe", lambda: nc.tensor.matmul(out.ap, lhsT.ap, rhs.ap, start=start, stop=stop,
                                                    skip_group_check=True), [lhsT, rhs], [out])
        else:
            self.add("pe", lambda: nc.tensor.matmul(out.ap, lhsT.ap, rhs.ap, start=start, stop=stop),
                     [lhsT, rhs], [out])

    def tr(self, out, in_, ident):
        nc = self.nc
        self.add("pe", lambda: nc.tensor.transpose(out.ap, in_.ap, ident.ap), [in_, ident], [out])

    def act(self, out, in_, func, bias=None, scale=1.0, accum=None):
        nc = self.nc
        a = self._a
        kw = {}
        if bias is not None:
            kw["bias"] = a(bias)
        if accum is not None:
            kw["accum_out"] = a(accum)
        rd = [in_] + [x for x in (bias, scale) if isinstance(x, View)]
        wr = [out] + ([accum] if accum is not None else [])
        self.add("act", lambda: nc.scalar.activation(out=out.ap, in_=in_.ap, func=func,
                                                      scale=a(scale), **kw), rd, wr)

    def _veng(self, eng):
        return self.nc.vector if eng == "dve" else self.nc.gpsimd

    def tt(self, out, in0, in1, op, eng="dve"):
        e = self._veng(eng)
        self.add(eng, lambda: e.tensor_tensor(out=out.ap, in0=in0.ap, in1=in1.ap, op=op),
                 [in0, in1], [out])

    def ts(self, out, in0, s1, s2, op0, op1=None, accum=None, eng="dve"):
        e = self._veng(eng)
        a = self._a
        kw = {}
        if op1 is not None:
            kw["op1"] = op1
        if accum is not None:
            kw["accum_out"] = a(accum)
        rd = [in0] + [x for x in (s1, s2) if isinstance(x, View)]
        wr = [out] + ([accum] if accum is not None else [])
        self.add(eng, lambda: e.tensor_scalar(out=out.ap, in0=in0.ap, scalar1=a(s1), scalar2=a(s2),
                                              op0=op0, **kw), rd, wr)

    def stt(self, out, in0, scalar, in1, op0, op1, eng="dve"):
        e = self._veng(eng)
        a = self._a
        rd = [in0, in1] + ([scalar] if isinstance(scalar, View) else [])
        self.add(eng, lambda: e.scalar_tensor_tensor(out=out.ap, in0=in0.ap, scalar=a(scalar),
                                                     in1=in1.ap, op0=op0, op1=op1), rd, [out])

    def copy(self, out, in_, eng="dve"):
        if eng == "act":
            nc = self.nc
            self.add("act", lambda: nc.scalar.copy(out=out.ap, in_=in_.ap), [in_], [out])
        else:
            e = self._veng(eng)
            self.add(eng, lambda: e.tensor_copy(out=out.ap, in_=in_.ap), [in_], [out])

    def memset(self, out, val, eng="dve"):
        e = self._veng(eng)
        self.add(eng, lambda: e.memset(out.ap, val), [], [out])

    def reduce(self, out, in_, op, eng="dve"):
        e = self._veng(eng)
        self.add(eng, lambda: e.tensor_reduce(out=out.ap, in_=in_.ap, axis=AX.X, op=op), [in_], [out])

    def recip(self, out, in_):
        nc = self.nc
        self.add("dve", lambda: nc.vector.reciprocal(out=out.ap, in_=in_.ap), [in_], [out])

    def dma(self, q, out, in_, **kw):
        h = self.h[q]
        self.add(q, lambda: h.dma_start(out=out.ap, in_=in_.ap, **kw), [in_], [out], kind="dma")

    def emit(self):
        nc = self.nc
        ops = self.ops
        known = {e: {} for e in self.ENGS}
        dma_known = {e: set() for e in self.ENGS}
        count = {e: 0 for e in self.ENGS}
        for oid, op in enumerate(ops):
            E = op.eng
            waits = []
            kn = known[E]
            if op.kind == "dma":
                ns = self.n_dma_sems[E]
                if op.dq_n >= ns:
                    prev = self.dma_ids[E][op.dq_n - ns]
                    if prev not in dma_known[E]:
                        waits.append(prev)
                        dma_known[E].add(prev)
            for d, raw in op.deps.items():
                p = ops[d]
                if p.kind == "dma":
                    if d in dma_known[E]:
                        continue
                    waits.append(d)
                    dma_known[E].add(d)
                else:
                    if p.eng == E and (E == "pe" or (not raw and not STRICT_SAME_ENGINE)):
                        continue
                    if kn.get(p.eng, 0) >= p.lidx:
                        continue
                    waits.append(d)
                    p.flag = True
                    for e2, v in p.clock.items():
                        if kn.get(e2, 0) < v:
                            kn[e2] = v
                    if kn.get(p.eng, 0) < p.lidx:
                        kn[p.eng] = p.lidx
            op.waits = waits
            count[E] += 1
            op.lidx = count[E]
            op.clock = dict(kn) if op.kind == "c" else None
        fcount = {e: 0 for e in self.ENGS}
        for op in ops:
            if op.kind == "c" and op.flag:
                fcount[op.eng] += 1
                op.fnum = fcount[op.eng]
        sems = {}
        for e in self.ENGS:
            n = (fcount[e] + SEM_CAP - 1) // SEM_CAP
            sems[e] = [nc.alloc_semaphore(f"s_{e}_{i}") for i in range(n)]
        dsems = {}
        for e in self.ENGS:
            if self.dma_count[e]:
                dsems[e] = [nc.alloc_semaphore(f"d_{e}_{i}")
                            for i in range(min(self.n_dma_sems[e], self.dma_count[e]))]
        for op in ops:
            if op.kind == "dma":
                ns = self.n_dma_sems[op.eng]
                op.dma_sem = dsems[op.eng][op.dq_n % ns]
                op.dma_val = 16 * (op.dq_n // ns + 1)
        nwaits = 0
        for op in ops:
            h = self.h[op.eng]
            for d in op.waits:
                p = ops[d]
                if p.kind == "dma":
                    h.wait_ge(p.dma_sem, p.dma_val)
                else:
                    n = p.fnum - 1
                    h.wait_ge(sems[p.eng][n // SEM_CAP], n % SEM_CAP + 1)
                nwaits += 1
            ins = op.fn()
            if op.kind == "dma":
                ins.then_inc(op.dma_sem, 16)
            elif op.flag:
                n = op.fnum - 1
                ins.then_inc(sems[op.eng][n // SEM_CAP], 1)
        self.stats = dict(n_ops=len(ops), n_waits=nwaits, flagged=dict(fcount),
                          per_eng=dict(count))
        return self.stats

    def final_wait(self, eng, opids):
        pass


from concourse.bass_utils import run_bass_kernel_spmd

D = 1024
EPS = 1e-6
N_IN = 7508
OFF = dict(u=0, v=512, gq=1024, gk=1280, gv=1536, gr=2048, ga=2560, dq=2576, dk=3088,
           dv=3600, diq=4112, dik=4368, diw=4432, gates=4436)
NEG = -30000.0
BIGNEG = -1.0e30
NBIS = 12
C_ID, C_TRI, C_ONES, C_NEGTRI, C_BKD, C_BKN, NCST = 0, 128, 256, 384, 512, 640, 768
PR_G1, PR_G2, PR_BMOD, PR_LNG, PR_LNB, PR_GNG, PR_GQ, PR_GK, PR_BBG, NPRM = 0, 8, 16, 64, 68, 72, 73, 74, 75, 99
RW_BS, RW_BG, RW_BR, NROW = 0, 512, 768, 788
WSM_GA, WSM_IQ, WSM_IK, WSM_IW, WSM_N = 0, 16, 272, 336, 340


def build_program(L, S, dbg=False, marks=False):
    NBLK = S // 128
    NG = S // 512
    nc = bass.Bass("TRN2", target_bir_lowering=False)
    P = Prog(nc)
    P.marks_on = marks
    EI = "ExternalInput"
    x_in = P.dram("x", [S, D], F32, kind=EI)
    ccol = P.dram("ccol", [128, 8], F32, kind=EI)
    cst = P.dram("cst", [128, NCST], F32, kind=EI)
    rbd = P.dram("rbrow", [1, 128], F32, kind=EI)
    w_mod = P.dram("w_mod", [L, D, 6 * D], F32, kind=EI)
    prm = P.dram("prm", [L, 128, NPRM], F32, kind=EI)
    prow = P.dram("prow", [L, 1, NROW], F32, kind=EI)
    w_in = P.dram("w_in", [L, D, N_IN], F32, kind=EI)
    wsT = P.dram("wsT", [L, 128, 4, 128], F32, kind=EI)
    w2 = P.dram("w2", [L, 16, 256], F32, kind=EI)
    w_br = P.dram("w_branch", [L, 3, 512, D], F32, kind=EI)
    w_out = P.dram("w_out", [L, D, D], F32, kind=EI)
    wr = P.dram("wr", [L, D, 20], F32, kind=EI)
    weg = P.dram("w_exp_gate", [L, 16, D, 256], F32, kind=EI)
    weu = P.dram("w_exp_up", [L, 16, D, 256], F32, kind=EI)
    wed = P.dram("w_exp_down", [L, 16, 256, D], F32, kind=EI)
    out = P.dram("out", [S, D], F32, kind="ExternalOutput", nreg=NBLK)
    gt_dram = P.dram("gt_bc", [2, 128, D], F32, nreg=2)
    dbgo = {}

    def dump(name, view, shape, dtype):
        if not dbg or name in dbgo:
            return
        t = P.dram("dbg_" + name, list(shape), dtype, kind="ExternalOutput")
        dbgo[name] = t
        P.dma("sp", t.v, view)

    cf = P.sb("cf", [128, 512], F32)
    ident4 = P.sb("ident4", [128, 512], BF16)
    identb = P.sb("identb", [128, 128], BF16)
    biasT = P.sb("biasT", [128, 2, 4, 128], BF16)
    kT_all = P.sb("kT_all", [128, 4, S], BF16)
    v_all = P.sb("v_all", [128, NBLK, 4, 130], BF16)
    ikT_all = P.sb("ikT_all", [64, S], BF16)
    xg = P.sb("xg", [128, 4, D], F32)
    hT = P.sb("hT", [128, 8, 512], BF16)
    ysT = P.sb("ysT", [128, 3, 4, 512], BF16)
    WB = [P.sb(f"wb{i}", [128, 8, 512], BF16) for i in range(3)]
    wsm = P.sb("wsm", [128, 8, WSM_N], BF16)
    prm_sb = P.sb("prm_sb", [128, NPRM], F32)
    modT = P.sb("modT", [128, 48], F32)
    sT = P.sb("sT", [128, 16], F32)
    WsTm = P.sb("WsTm", [128, 4, 128], BF16)
    Cg = P.sb("Cg", [128, 4, 128], F32)
    w2_sb = P.sb("w2_sb", [16, 256], BF16)
    bg_sb = P.sb("bg_sb", [1, 256], BF16)
    ones1 = P.sb("ones1", [1, 128], BF16)
    wr_sb = P.sb("wr_sb", [128, 8, 20], F32)
    rbias = P.sb("rbias", [128, 20], F32)
    cactT = P.sb("cactT", [128, 8], BF16)
    Sf = P.sb("Sf", [64, 4, 128], F32)
    Sb = P.sb("Sb", [64, 4, 128], BF16)
    stat = P.sb("stat", [128, 64], F32)
    combb = P.sb("combb", [128, 4, 16], BF16)
    fv = P.sb("fv", [128, NBIS], F32)
    offs = P.sb("offs", [128, NBIS], F32)
    PSR = [P.ps(f"psr{i}") for i in range(6)]
    psA = P.ps("psA")
    psB = P.ps("psB")
    P.psum_rot = PSR
    ARENA = max(46 * 1024, 23552 + 8 * S)
    P.make_arena(ARENA)

    identf = cf[:, C_ID:C_ID + 128]
    trif = cf[:, C_TRI:C_TRI + 128]
    onesf = cf[:, C_ONES:C_ONES + 128]
    negtri = cf[:, C_NEGTRI:C_NEGTRI + 128]

    def wload(dst, src):
        P.dma("pool", dst, src)

    class WStream:
        def __init__(self, bufs):
            self.free = list(bufs)
            self.sched = []
            self.issued = []
            self.nissued = 0

        def push(self, fn):
            self.sched.append(fn)

        def _pump(self):
            while self.free and self.nissued < len(self.sched):
                b = self.free.pop(0)
                self.sched[self.nissued](b)
                self.nissued += 1
                self.issued.append(b)

        def get(self):
            self._pump()
            assert self.issued, "weight schedule underflow / no free buffer"
            return self.issued.pop(0)

        def release(self, b):
            self.free.append(b)
            self._pump()

    ws_main = WStream(WB)

    def sched_kxn(src2d, c0, n):
        ws_main.push(lambda wb: wload(wb[:, :, 0:n],
                                      src2d.rearrange("(k p) n -> p k n", p=128)[:, :, c0:c0 + n]))

    def sched_layer_mod(l):
        for i in range(12):
            sched_kxn(w_mod[l], i * 512, 512)

    def sched_group(l):
        for nm in ("u", "v", "gq", "gv", "gr", "dq", "dk", "dv"):
            sched_kxn(w_in[l], OFF[nm], 512)
        for n in range(3):
            ws_main.push(lambda wb, n=n: wload(
                wb.v.rearrange("p a b -> p (a b)").rearrange("p (e d) -> p e d", e=4),
                w_br[l][n].rearrange("(e p) d -> p e d", p=128)))
            for half in range(2):
                sched_kxn(w_in[l], OFF["gates"] + (n * 2 + half) * 512, 512)
        for half in range(2):
            sched_kxn(w_out[l], half * 512, 512)
        for e in range(16):
            def f(wb, e=e):
                wload(wb[:, :, 0:256], weg[l][e].rearrange("(k p) n -> p k n", p=128))
                wload(wb[:, :, 256:512], weu[l][e].rearrange("(k p) n -> p k n", p=128))
            ws_main.push(f)

    def next_wb():
        return ws_main.get()

    def rel_wb(b):
        ws_main.release(b)

    P.dma("sp", cf.v, cst[:, 0:512])
    bk = P.ar("bk", [128, 256], F32, 2048)
    P.dma("sp", bk.v, cst[:, 512:768])
    for i4 in range(4):
        P.dma("pool", ident4[:, i4 * 128:(i4 + 1) * 128], cst[:, C_ID:C_ID + 128])
    P.dma("pool", identb.v, cst[:, C_ID:C_ID + 128])
    P.memset(ones1.v, 1.0)
    for it in range(NBIS):
        P.memset(fv[:, it:it + 1], 0.5 ** (it + 1))
    P.memset(v_all.v, 1.0)
    rbb = P.ar("rbb", [128, 128], F32, 0)
    bacc = P.ar("bacc", [128, 128], F32, 512)
    btmp = P.ar("btmp", [128, 128], F32, 1024)
    P.dma("sp", rbb.v, rbd.v.partition_broadcast(128))
    for h in range(4):
        for dist, cb in ((0, 0), (1, 128)):
            for b in range(32):
                P.ts(btmp.v, bk[:, cb:cb + 128], float(b), None, ALU.is_equal)
                if b == 0:
                    P.ts(bacc.v, btmp.v, rbb[:, b * 4 + h:b * 4 + h + 1], None, ALU.mult)
                else:
                    P.stt(bacc.v, btmp.v, rbb[:, b * 4 + h:b * 4 + h + 1], bacc.v, ALU.mult, ALU.add)
            P.ts(biasT[:, dist, h, :], bacc.v, rbb[:, 31 * 4 + h:31 * 4 + h + 1], None, ALU.subtract)

    def small_rstd(dst, src, inv_n):
        P.ts(dst, src, inv_n, EPS, ALU.mult, ALU.add)
        P.act(dst, dst, AF.Sqrt)
        P.recip(dst, dst)

    def norm_to_hT(scol, shcol, fp32_router):
        junk = P.ar("junk", [128, D], BF16, 0)
        ssq = stat[:, 0:4]
        rstd = stat[:, 4:8]
        for bi in range(4):
            P.act(junk.v, xg[:, bi, :], AF.Square, accum=stat[:, bi:bi + 1])
        small_rstd(rstd, ssq, 1.0 / D)
        if not fp32_router:
            xns = [P.ar(f"xn{i}", [128, D], BF16, 2048 + 2048 * i) for i in range(2)]
            for bi in range(4):
                xn = xns[bi % 2]
                P.ts(xn.v, xg[:, bi, :], stat[:, 4 + bi:5 + bi], None, ALU.mult)
                ps = P.next_ps()
                psb = ps.v.bitcast(BF16)
                for k in range(8):
                    P.tr(psb[:, k * 128:(k + 1) * 128], xn[:, k * 128:(k + 1) * 128], identb.v)
                for k in range(8):
                    o = hT[:, k, bi * 128:(bi + 1) * 128]
                    if k % 2 == 0:
                        P.act(o, psb[:, k * 128:(k + 1) * 128], AF.Identity,
                              bias=shcol[:, k:k + 1], scale=scol[:, k:k + 1])
                    else:
                        P.ts(o, psb[:, k * 128:(k + 1) * 128], scol[:, k:k + 1], shcol[:, k:k + 1],
                             ALU.mult, ALU.add)
        else:
            xn32 = P.ar("xn32", [128, D], F32, 2048)
            h32 = P.ar("h32", [128, 8, 128], F32, 6144)
            lg_all = P.ar("lg_all", [128, 4, 20], F32, 10240)
            for bi in range(4):
                P.ts(xn32.v, xg[:, bi, :], stat[:, 4 + bi:5 + bi], None, ALU.mult)
                pss = [P.next_ps(), P.next_ps()]
                for k in range(8):
                    P.mm(pss[k // 4][:, (k % 4) * 128:(k % 4 + 1) * 128],
                         xn32[:, k * 128:(k + 1) * 128], identf)
                for k in range(8):
                    src = pss[k // 4][:, (k % 4) * 128:(k % 4 + 1) * 128]
                    if k % 2 == 0:
                        P.act(h32[:, k, :], src, AF.Identity, bias=shcol[:, k:k + 1], scale=scol[:, k:k + 1])
                    else:
                        P.ts(h32[:, k, :], src, scol[:, k:k + 1], shcol[:, k:k + 1], ALU.mult, ALU.add)
                P.copy(hT[:, :, bi * 128:(bi + 1) * 128], h32.v)
                psr = P.next_ps()
                for k in range(8):
                    P.mm(psr[:, 0:20], h32[:, k, :], wr_sb[:, k, :], start=(k == 0), stop=(k == 7))
                P.tt(lg_all[:, bi, :], psr[:, 0:20], rbias.v, ALU.add)
            router_all()

    def proj_tok(wb, c0, n, bi):
        ps = P.next_ps()
        for k in range(8):
            P.mm(ps[:, 0:n], hT[:, k, bi * 128:(bi + 1) * 128], wb[:, k, c0:c0 + n],
                 start=(k == 0), stop=(k == 7))
        return ps

    def proj_feat(wb, c0, m):
        ps = P.next_ps()
        for k in range(8):
            P.mm(ps[0:m, :], wb[:, k, c0:c0 + m], hT[:, k, :], start=(k == 0), stop=(k == 7))
        return ps

    def router_all():
        BIG = 1.0e30
        o = [10240 + 320]

        def fld(name, n):
            b_ = P.ar("r_" + name, [128, 4, n], F32, o[0])
            o[0] += ((4 * n * 4 + 31) // 32) * 32
            return b_

        lg_all = P.ar("lg_all", [128, 4, 20], F32, 10240)
        gmax, gsum, pg, m1, m2, dd, e21, w1, w1p, w2p = [fld(n_, 1) for n_ in
                                                         ("gmax", "gsum", "pg", "m1", "m2", "dd", "e21", "w1", "w1p", "w2p")]
        d4, ge, oh, negm = [fld(n_, 4) for n_ in ("d4", "ge", "oh", "negm")]
        elm, oh1, elm2, oh2, cmb, cmb2 = [fld(n_, 16) for n_ in ("elm", "oh1", "elm2", "oh2", "cmb", "cmb2")]
        f2 = lambda t: t.v.rearrange("p b o -> p (b o)")
        bc = lambda t, n: t.v.to_broadcast([128, 4, n])
        lgg = lg_all[:, :, 0:4]
        lge = lg_all[:, :, 4:20]
        P.reduce(f2(gmax), lgg, ALU.max)
        P.tt(d4.v, lgg, bc(gmax, 4), ALU.subtract)
        P.act(ge.v, d4.v, AF.Exp)
        P.reduce(f2(gsum), ge.v, ALU.add)
        P.recip(pg.v, gsum.v)
        P.tt(oh.v, lgg, bc(gmax, 4), ALU.is_equal)
        P.ts(negm.v, oh.v, 1.0, BIG, ALU.subtract, ALU.mult)
        P.tt(elm.v.rearrange("p b (g e) -> p b g e", g=4), lge.rearrange("p b (g e) -> p b g e", g=4),
             negm.v.rearrange("p b (g o) -> p b g o", o=1).to_broadcast([128, 4, 4, 4]), ALU.add)
        P.reduce(f2(m1), elm.v, ALU.max)
        P.tt(oh1.v, elm.v, bc(m1, 16), ALU.is_equal)
        P.stt(elm2.v, oh1.v, -BIG, elm.v, ALU.mult, ALU.add)
        P.reduce(f2(m2), elm2.v, ALU.max)
        P.tt(oh2.v, elm2.v, bc(m2, 16), ALU.is_equal)
        P.tt(dd.v, m2.v, m1.v, ALU.subtract)
        P.act(e21.v, dd.v, AF.Exp)
        P.ts(w1.v, e21.v, 1.0, None, ALU.add)
        P.recip(w1.v, w1.v)
        P.tt(w1p.v, w1.v, pg.v, ALU.mult)
        P.tt(w2p.v, w1p.v, e21.v, ALU.mult)
        P.tt(cmb.v, oh1.v, bc(w1p, 16), ALU.mult)
        P.tt(cmb2.v, oh2.v, bc(w2p, 16), ALU.mult)
        P.tt(combb.v, cmb.v, cmb2.v, ALU.add)

    for l in range(L):
        xsrc = x_in if l == 0 else out
        sched_layer_mod(l)
        for _g in range(NG):
            sched_group(l)
        P.dma("sp", prm_sb.v, prm[l])
        P.dma("pool", w2_sb.v, w2[l])
        P.dma("pool", bg_sb.v, prow[l][:, RW_BG:RW_BG + 256])
        P.dma("sp", wr_sb.v, wr[l].rearrange("(k p) n -> p k n", p=128))
        P.dma("sp", rbias.v, prow[l][:, RW_BR:RW_BR + 20].partition_broadcast(128))
        wl = w_in[l].rearrange("(k p) n -> p k n", p=128)
        wload(wsm[:, :, WSM_GA:WSM_GA + 16], wl[:, :, OFF["ga"]:OFF["ga"] + 16])
        wload(wsm[:, :, WSM_IQ:WSM_N], wl[:, :, OFF["diq"]:OFF["diq"] + 324])
        cc32 = P.ar("cc32", [128, 8], F32, 0)
        P.dma("sp", cc32.v, ccol.v)
        P.act(cactT.v, cc32.v, AF.Silu)
        psm = P.next_ps()
        for i in range(12):
            wb = next_wb()
            for jj in range(4):
                j = i * 4 + jj
                for k in range(8):
                    P.mm(psm[:, j:j + 1], wb[:, k, jj * 128:(jj + 1) * 128], cactT[:, k:k + 1],
                         start=(k == 0), stop=(k == 7))
            rel_wb(wb)
        P.tt(modT.v, psm[:, 0:48], prm_sb[:, PR_BMOD:PR_BMOD + 48], ALU.add)
        P.stt(sT[:, 0:8], modT[:, 8:16], 1.0, prm_sb[:, PR_G1:PR_G1 + 8], ALU.add, ALU.mult)
        P.stt(sT[:, 8:16], modT[:, 32:40], 1.0, prm_sb[:, PR_G2:PR_G2 + 8], ALU.add, ALU.mult)
        ws32 = P.ar("ws32", [128, 4, 128], F32, 2048)
        P.dma("sp", ws32.v, wsT[l])
        P.tt(ws32.v, ws32.v, trif.rearrange("p (o t) -> p o t", o=1).to_broadcast([128, 4, 128]), ALU.mult)
        P.copy(WsTm.v, ws32.v)
        bsrow = P.ar("bsrow", [1, 512], F32, 4096)
        P.dma("sp", bsrow.v, prow[l][:, RW_BS:RW_BS + 512])
        bsb = P.ar("bsb", [128, 4, 128], F32, 6144)
        for g in range(4):
            ps1 = P.next_ps()
            P.mm(ps1[:, 0:128], onesf[0:1, :], bsrow[:, g * 128:(g + 1) * 128])
            P.copy(bsb[:, g, :], ps1[:, 0:128], eng="act")
            P.mm(ps1[:, 128:256], onesf, ws32[:, g, :])
            P.stt(Cg[:, g, :], ps1[:, 128:256], prm_sb[:, PR_LNB + g:PR_LNB + g + 1], bsb[:, g, :],
                  ALU.mult, ALU.add)
        P.ts(stat[:, 32:33], prm_sb[:, PR_GQ:PR_GQ + 1], float(128 ** -0.5), None, ALU.mult)
        gq_col = stat[:, 32:33]
        gk_col = prm_sb[:, PR_GK:PR_GK + 1]
        gng_col = prm_sb[:, PR_GNG:PR_GNG + 1]
        P.memset(Sf.v, 0.0)
        P.memset(Sb.v, 0.0)

        def gt_bcast(col0, o_gtb, o_dg):
            gtb = P.ar("gtb", [128, D], F32, o_gtb)
            dg = P.ar("dg", [128, 128], F32, o_dg)
            pss = [P.next_ps(), P.next_ps()]
            for k in range(8):
                P.ts(dg.v, identf, modT[:, col0 + k:col0 + k + 1], None, ALU.mult)
                P.mm(pss[k // 4][:, (k % 4) * 128:(k % 4 + 1) * 128], onesf, dg.v)
            P.copy(gtb[:, 0:512], pss[0].v, eng="act")
            P.copy(gtb[:, 512:1024], pss[1].v, eng="act")
            return gtb

        for gi, col0 in enumerate((16, 40)):
            gtb0 = gt_bcast(col0, 16384, 20480)
            P.dma("sp", gt_dram.sub(gi)[gi], gtb0.v)

        def gt_load(gi, o_gtb):
            gtb_ = P.ar("gtb", [128, D], F32, o_gtb)
            P.dma("sp", gtb_.v, gt_dram.sub(gi)[gi])
            return gtb_

        for g in range(NG):
            for bi in range(4):
                gb = g * 4 + bi
                P.dma("sp", xg[:, bi, :], (xsrc if l == 0 else out.sub(gb))[gb * 128:(gb + 1) * 128, :])
            P.mark(f'g{g}_start')
            norm_to_hT(sT[:, 0:8], modT[:, 0:8], False)
            P.mark(f'g{g}_norm1')
            dump("modT", modT.v, [128, 48], F32)
            dump("hT", hT.v, [128, 8, 512], BF16)

            uT = P.ar("uT", [128, 4, 512], BF16, 8192)
            vg = P.ar("vg", [128, 4, 512], F32, 12288)
            vn = P.ar("vn", [128, 4, 512], BF16, 20480)
            gtmp = [P.ar(f"gtmp{i}", [128, 128], F32, 24576 + 512 * i) for i in range(2)]
            gjunk = P.ar("gjunk", [128, 512], BF16, 25600)
            wu = next_wb()
            for c in range(4):
                ps = proj_feat(wu, c * 128, 128)
                P.act(uT[:, c, :], ps.v, AF.Gelu_apprx_tanh)
            rel_wb(wu)
            wv = next_wb()
            for bi in range(4):
                ps = proj_tok(wv, 0, 512, bi)
                P.act(vg[:, bi, :], ps.v, AF.Gelu_apprx_tanh, accum=stat[:, 8 + bi:9 + bi])
                P.act(gjunk.v, vg[:, bi, :], AF.Square, accum=stat[:, 12 + bi:13 + bi])
            rel_wb(wv)
            mean = stat[:, 16:20]
            P.ts(mean, stat[:, 8:12], 1.0 / 512, None, ALU.mult)
            msq = stat[:, 20:24]
            P.tt(msq, mean, mean, ALU.mult)
            var = stat[:, 24:28]
            P.stt(var, stat[:, 12:16], 1.0 / 512, msq, ALU.mult, ALU.subtract)
            small_rstd(var, var, 1.0)
            for bi in range(4):
                P.ts(vn[:, bi, :], vg[:, bi, :], stat[:, 16 + bi:17 + bi], stat[:, 24 + bi:25 + bi],
                     ALU.subtract, ALU.mult)
            for bi in range(4):
                ps = P.next_ps()
                for gg in range(4):
                    P.mm(ps[:, gg * 128:(gg + 1) * 128], vn[:, bi, gg * 128:(gg + 1) * 128], WsTm[:, gg, :])
                for gg in range(4):
                    tmp = gtmp[gg % 2]
                    P.stt(tmp.v, ps[:, gg * 128:(gg + 1) * 128], prm_sb[:, PR_LNG + gg:PR_LNG + gg + 1],
                          Cg[:, gg, :], ALU.mult, ALU.add)
                    P.tt(ysT[:, 0, gg, bi * 128:(bi + 1) * 128], tmp.v, uT[:, gg, bi * 128:(bi + 1) * 128],
                         ALU.mult)

            P.mark(f'g{g}_gmlp')
            gaT = P.ar("gaT", [16, 512], BF16, 8192)
            ps = proj_feat(wsm, WSM_GA, 16)
            P.copy(gaT.v, ps[0:16, :])
            qk_all = P.ar("qk_all", [128, 4, 512], F32, 9216)
            v_allg = P.ar("v_allg", [128, 4, 512], BF16, 17408)
            r_all = P.ar("r_all", [128, 4, 512], BF16, 21504)
            l_sb = P.ar("l_sb", [128, 256], F32, 25600)
            e_sb = P.ar("e_sb", [128, 256], F32, 26624)
            eb = P.ar("eb", [128, 256], F32, 27648)
            ebp = P.ar("ebp", [128, 256], F32, 28672)
            ebl = P.ar("ebl", [128, 256], F32, 29696)
            tmpk = P.ar("tmpk", [128, 256], F32, 30720)
            qe = P.ar("qe", [128, 256], BF16, 31744)
            ke = P.ar("ke", [128, 256], BF16, 32256)
            kd = P.ar("kd", [128, 256], BF16, 32768)
            qkT = P.ar("qkT", [64, 8, 128], BF16, 33280)
            ATm = P.ar("ATm", [128, 4, 128], BF16, 35328)
            yb = P.ar("yb", [128, 512], BF16, 36352)
            Dcol = P.ar("Dcol", [64, 4], F32, 37376)
            oj = P.ar("oj", [128, 128], BF16, 37888)
            wqk = next_wb()
            for bi in range(4):
                ps = proj_tok(wqk, 0, 512, bi)
                P.copy(qk_all[:, bi, :], ps.v, eng="act")
            rel_wb(wqk)
            wgv = next_wb()
            for bi in range(4):
                ps = proj_tok(wgv, 0, 512, bi)
                P.copy(v_allg[:, bi, :], ps.v)
            rel_wb(wgv)
            wgr = next_wb()
            for bi in range(4):
                ps = proj_tok(wgr, 0, 512, bi)
                P.act(r_all[:, bi, :], ps.v, AF.Silu)
            rel_wb(wgr)
            for bi in range(4):
                bs = slice(bi * 128, (bi + 1) * 128)
                qk_sb = qk_all[:, bi, :]
                v_sb = v_allg[:, bi, :]
                r_sb = r_all[:, bi, :]
                psz = P.next_ps()
                P.mm(psz[:, 0:256], gaT[:, bs], w2_sb.v, start=True, stop=False)
                P.mm(psz[:, 0:256], ones1.v, bg_sb.v, start=False, stop=True)
                P.act(e_sb.v, psz[:, 0:256], AF.Exp, scale=-1.0)
                P.act(l_sb.v, e_sb.v, AF.Ln, bias=1.0)
                psc = P.next_ps()
                P.mm(psc[:, 0:256], trif, l_sb.v)
                P.mm(psc[:, 256:512], onesf, l_sb.v)
                psd = P.next_ps()
                for h in range(4):
                    P.mm(psd[0:64, h:h + 1], l_sb[:, h * 64:(h + 1) * 64], onesf[:, 0:1])
                P.act(eb.v, psc[:, 0:256], AF.Exp, scale=-1.0 / 16)
                P.act(ebp.v, psc[:, 0:256], AF.Exp, scale=1.0 / 16)
                P.act(ebl.v, psc[:, 256:512], AF.Exp, scale=-1.0 / 16)
                P.act(Dcol.v, psd[0:64, 0:4], AF.Exp, scale=-1.0 / 16)
                P.stt(qe.v, qk_sb[:, 0:256], 0.125, eb.v, ALU.mult, ALU.mult)
                P.tt(ke.v, qk_sb[:, 256:512], ebp.v, ALU.mult)
                P.tt(tmpk.v, ebp.v, ebl.v, ALU.mult)
                P.tt(kd.v, qk_sb[:, 256:512], tmpk.v, ALU.mult)
                pst = P.next_ps()
                pstb = pst.v.bitcast(BF16)
                for h in range(4):
                    P.tr(pstb[0:64, h * 128:(h + 1) * 128], qe[:, h * 64:(h + 1) * 64], identb.v)
                    P.tr(pstb[0:64, (4 + h) * 128:(5 + h) * 128], ke[:, h * 64:(h + 1) * 64], identb.v)
                P.copy(qkT.v.rearrange("p a b -> p (a b)"), pstb[0:64, :], eng="act")
                psa = P.next_ps()
                for h in range(4):
                    P.mm(psa[:, h * 128:(h + 1) * 128], qkT[:, 4 + h, :], qkT[:, h, :])
                P.tt(ATm.v, psa.v.rearrange("p (h t) -> p h t", h=4),
                     trif.rearrange("p (o t) -> p o t", o=1).to_broadcast([128, 4, 128]), ALU.mult)
                pso = P.next_ps()
                for h in range(4):
                    hs = slice(h * 128, (h + 1) * 128)
                    P.mm(pso[:, hs], ATm[:, h, :], v_sb[:, hs], start=True, stop=False)
                    P.mm(pso[:, hs], qkT[:, h, :], Sb[:, h, :], start=False, stop=True)
                psu = P.next_ps()
                for h in range(4):
                    P.mm(psu[0:64, h * 128:(h + 1) * 128], kd[:, h * 64:(h + 1) * 64], v_sb[:, h * 128:(h + 1) * 128])
                for h in range(4):
                    P.stt(Sf[:, h, :], Sf[:, h, :], Dcol[:, h:h + 1], psu[0:64, h * 128:(h + 1) * 128],
                          ALU.mult, ALU.add)
                P.copy(Sb.v, Sf.v)
                for h in range(4):
                    P.act(oj.v, pso[:, h * 128:(h + 1) * 128], AF.Square, accum=stat[:, 36 + h:37 + h])
                small_rstd(stat[:, 36:40], stat[:, 36:40], 1.0 / 128)
                for h in range(4):
                    hs = slice(h * 128, (h + 1) * 128)
                    P.stt(yb[:, hs], pso[:, hs], stat[:, 36 + h:37 + h], r_sb[:, hs], ALU.mult, ALU.mult)
                pst2 = P.next_ps()
                pst2b = pst2.v.bitcast(BF16)
                for h in range(4):
                    P.tr(pst2b[:, h * 128:(h + 1) * 128], yb[:, h * 128:(h + 1) * 128], identb.v)
                P.act(ysT[:, 1, :, bs], pst2b[:, 0:512].rearrange("p (h t) -> p h t", h=4), AF.Identity,
                      scale=gng_col)

            P.mark(f'g{g}_gla')
            kn = P.ar("kn", [128, 512], BF16, 8192)
            qT = P.ar("qT", [128, 4, 512], BF16, 9216)
            iqT = P.ar("iqT", [64, 4, 512], BF16, 13312)
            iw_sb = P.ar("iw_sb", [128, 4, 4], F32, 17408)
            relu_t = [P.ar("relu0", [128, 512], F32, 17920)]
            PT = [P.ar(f"PT{i}", [128, 512], BF16, 19968 + 1024 * i) for i in range(2)]
            yc = P.ar("yc", [128, 512], BF16, 22016)
            bjunk = P.ar("bjunk", [128, 128], BF16, 23040)
            score = P.ar("score", [128, S], F32, 23552)
            maskbuf = [P.ar(f"maskbuf{i}", [128, S], BF16, 23552 + 4 * S + 2 * S * i) for i in range(2)]
            cD = max(max(1, n_ * 15 // 32) for n_ in range(1, NBLK + 1)) * 128
            cA = max(n_ - max(1, n_ * 15 // 32) for n_ in range(1, NBLK + 1)) * 128
            assert cD + cA <= S
            jDb = [P.ar(f"jD{i}", [128, cD], BF16, 23552 + 4 * S + 2 * S * i) for i in range(2)]
            jAb = [P.ar(f"jA{i}", [128, cA], BF16, 23552 + 4 * S + 2 * S * i + 2 * cD) for i in range(2)]

            def qk_norm_T(ps, gcol, dst):
                for h in range(4):
                    P.act(bjunk.v, ps[:, h * 128:(h + 1) * 128], AF.Square, accum=stat[:, 40 + h:41 + h])
                small_rstd(stat[:, 40:44], stat[:, 40:44], 1.0 / 128)
                for h in range(4):
                    P.ts(kn[:, h * 128:(h + 1) * 128], ps[:, h * 128:(h + 1) * 128], stat[:, 40 + h:41 + h],
                         None, ALU.mult)
                pt = P.next_ps()
                ptb = pt.v.bitcast(BF16)
                for h in range(4):
                    P.tr(ptb[:, h * 128:(h + 1) * 128], kn[:, h * 128:(h + 1) * 128], identb.v)
                P.act(dst, ptb[:, 0:512].rearrange("p (h t) -> p h t", h=4), AF.Identity, scale=gcol)

            for bi in range(4):
                gb = g * 4 + bi
                bs = slice(bi * 128, (bi + 1) * 128)
                psw = P.next_ps()
                for k in range(8):
                    P.mm(psw[:, 0:4], hT[:, k, bs], wsm[:, k, WSM_IW:WSM_IW + 4], start=(k == 0), stop=(k == 7))
                P.copy(iw_sb[:, bi, :], psw[:, 0:4])
            ps = proj_feat(wsm, WSM_IK, 64)
            P.copy(ikT_all[:, g * 512:(g + 1) * 512], ps[0:64, :], eng="act")
            for h in range(4):
                ps = proj_feat(wsm, WSM_IQ + h * 64, 64)
                P.copy(iqT[:, h, :], ps[0:64, :], eng="act")


            def IB_gen(bi):
                gb = g * 4 + bi
                bs = slice(bi * 128, (bi + 1) * 128)
                nk = (gb + 1) * 128
                nch = (nk + 511) // 512
                mk = maskbuf[bi % 2]
                for c in range(nch):
                    c0, c1 = c * 512, min((c + 1) * 512, nk)
                    w = c1 - c0
                    for h in range(4):
                        psi = P.next_ps()
                        P.mm(psi[:, 0:w], iqT[:, h, bs], ikT_all[:, c0:c1])
                        rl = relu_t[0]
                        P.act(rl[:, 0:w], psi[:, 0:w], AF.Relu)
                        if h == 0:
                            P.ts(score[:, c0:c1], rl[:, 0:w], iw_sb[:, bi, 0:1], None, ALU.mult)
                        else:
                            P.stt(score[:, c0:c1], rl[:, 0:w], iw_sb[:, bi, h:h + 1], score[:, c0:c1],
                                  ALU.mult, ALU.add)
                    yield
                P.tt(score[:, gb * 128:nk], score[:, gb * 128:nk], negtri, ALU.add)
                tau = stat[:, 44:45]
                if gb < 2:
                    P.memset(tau, -1.0e29)
                else:
                    rng = stat[:, 45:46]
                    mid = stat[:, 46:47]
                    cnt = stat[:, 47:48]
                    u = stat[:, 48:49]
                    hi = stat[:, 49:50]
                    lo = stat[:, 50:51]
                    P.ts(mk[:, 0:nk], score[:, 0:nk], 0.0, -3.0e38, ALU.add, ALU.max, accum=hi)
                    P.ts(mk[:, 0:256], score[:, 0:256], 0.0, 3.0e38, ALU.add, ALU.min, accum=lo)
                    P.tt(rng, hi, lo, ALU.subtract)
                    P.ts(offs.v, fv.v, rng, None, ALU.mult)
                    P.tt(mid, lo, offs[:, 0:1], ALU.add)
                    for it in range(NBIS):
                        P.ts(mk[:, 0:nk], score[:, 0:nk], mid, 0.0, ALU.is_ge, ALU.add, accum=cnt)
                        last = (it == NBIS - 1)
                        P.ts(u, cnt, 256.0, 1.0 if last else 0.5, ALU.is_ge, ALU.subtract)
                        P.stt(tau if last else mid, u, offs[:, it:it + 1], mid, ALU.mult, ALU.add)
                        yield
                P.ts(mk[:, 0:nk], score[:, 0:nk], tau, NEG, ALU.is_lt, ALU.mult)
                yield

            def drain(gen):
                for _ in gen:
                    pass

            def interleave(ga, gb_):
                a_done = b_done = False
                while not (a_done and b_done):
                    if not a_done:
                        try:
                            next(ga)
                        except StopIteration:
                            a_done = True
                    if not b_done:
                        try:
                            next(gb_)
                        except StopIteration:
                            b_done = True

            def ATT_gen(bi):
                gb = g * 4 + bi
                bs = slice(bi * 128, (bi + 1) * 128)
                mk = maskbuf[bi % 2]
                P.memset(psA.v, 0.0)
                P.memset(psB.v, 0.0)
                def LG(j):
                    psl = P.next_ps()
                    dist = gb - j
                    P.mm(psl.v, mk[:, j * 128:(j + 1) * 128], ident4.v, start=True, stop=False)
                    if dist <= 1:
                        P.mm(psl.v, identb.v, biasT[:, dist, :, :].rearrange("p h t -> p (h t)"),
                             start=False, stop=False)
                    for h in range(4):
                        hs = slice(h * 128, (h + 1) * 128)
                        P.mm(psl[:, hs], kT_all[:, h, j * 128:(j + 1) * 128], qT[:, h, bs],
                             start=False, stop=(h == 3))
                    return psl

                psl = LG(0)
                P.act(PT[0].v, psl.v, AF.Exp, bias=-6.0)
                for j in range(gb + 1):
                    pt = PT[j % 2]
                    if j + 1 <= gb:
                        psl_next = LG(j + 1)
                    yield
                    for h in range(4):
                        acc = psA if h < 2 else psB
                        a0 = (h % 2) * 130
                        P.mm(acc[:, a0:a0 + 129], pt[:, h * 128:(h + 1) * 128], v_all[:, j, h, 0:129],
                             start=False, stop=False, skip=True)
                    if j + 1 <= gb:
                        P.act(PT[(j + 1) % 2].v, psl_next.v, AF.Exp, bias=-6.0)
                for h in range(4):
                    acc = psA if h < 2 else psB
                    a0 = (h % 2) * 130
                    P.recip(stat[:, 52 + h:53 + h], acc[:, a0 + 128:a0 + 129])
                    P.ts(yc[:, h * 128:(h + 1) * 128], acc[:, a0:a0 + 128], stat[:, 52 + h:53 + h], None, ALU.mult)
                pt2 = P.next_ps()
                pt2b = pt2.v.bitcast(BF16)
                for h in range(4):
                    P.tr(pt2b[:, h * 128:(h + 1) * 128], yc[:, h * 128:(h + 1) * 128], identb.v)
                P.copy(ysT[:, 2, :, bs], pt2b[:, 0:512].rearrange("p (h t) -> p h t", h=4), eng="act")

            def projA_gen():
                wdq = next_wb()
                for bi in range(4):
                    bs = slice(bi * 128, (bi + 1) * 128)
                    ps = proj_tok(wdq, 0, 512, bi)
                    qk_norm_T(ps, gq_col, qT[:, :, bs])
                    yield
                rel_wb(wdq)
                wdk = next_wb()
                for bi in range(4):
                    gb = g * 4 + bi
                    ps = proj_tok(wdk, 0, 512, bi)
                    qk_norm_T(ps, gk_col, kT_all[:, :, gb * 128:(gb + 1) * 128])
                    yield
                rel_wb(wdk)
                wdv = next_wb()
                for bi in range(4):
                    gb = g * 4 + bi
                    ps = proj_tok(wdv, 0, 512, bi)
                    P.copy(v_all[:, gb, :, 0:128], ps.v.rearrange("p (h e) -> p h e", h=4))
                    yield
                rel_wb(wdv)

            interleave(projA_gen(), IB_gen(0))
            P.mark(f'g{g}_dsaproj')
            for bi in range(4):
                if bi + 1 < 4:
                    interleave(ATT_gen(bi), IB_gen(bi + 1))
                else:
                    drain(ATT_gen(bi))

            P.mark(f'g{g}_dsa')
            dump("ysT", ysT.v, [128, 3, 4, 512], BF16)
            dump("kT", kT_all[:, :, 0:512], [128, 4, 512], BF16)
            gtb = gt_load(0, 40960)
            mergedF = P.ar("mergedF", [128, 8, 512], F32, 8192)
            mergedT = P.ar("mergedT", [128, 8, 512], BF16, 24576)
            sgt = [P.ar(f"sgt{i}", [128, 512], BF16, 32768 + 1024 * i) for i in range(2)]
            mtmp = [P.ar(f"mtmp{i}", [128, 512], F32, 34816 + 2048 * i) for i in range(2)]
            ci = 0
            for n in range(3):
                wbr = next_wb()
                wbrv = wbr.v.rearrange("p a b -> p (a b)").rearrange("p (e d) -> p e d", e=4)
                for half in range(2):
                    wg = next_wb()
                    for cc in range(4):
                        dc = half * 4 + cc
                        psg = proj_feat(wg, cc * 128, 128)
                        sg = sgt[ci % 2]
                        P.act(sg.v, psg.v, AF.Sigmoid, bias=prm_sb[:, PR_BBG + n * 8 + dc:PR_BBG + n * 8 + dc + 1])
                        psu = P.next_ps()
                        for ec in range(4):
                            P.mm(psu.v, wbrv[:, ec, dc * 128:(dc + 1) * 128], ysT[:, n, ec, :],
                                 start=(ec == 0), stop=(ec == 3))
                        if n == 0:
                            P.tt(mergedF[:, dc, :], psu.v, sg.v, ALU.mult)
                        else:
                            mt = mtmp[ci % 2]
                            P.tt(mt.v, psu.v, sg.v, ALU.mult)
                            if n == 1:
                                P.tt(mergedF[:, dc, :], mergedF[:, dc, :], mt.v, ALU.add)
                            else:
                                P.tt(mergedT[:, dc, :], mergedF[:, dc, :], mt.v, ALU.add)
                        ci += 1
                    rel_wb(wg)
                rel_wb(wbr)
            P.mark(f'g{g}_merge')
            for half in range(2):
                wo = next_wb()
                for bi in range(4):
                    ps = P.next_ps()
                    for k in range(8):
                        P.mm(ps.v, mergedT[:, k, bi * 128:(bi + 1) * 128], wo[:, k, :], start=(k == 0), stop=(k == 7))
                    mt = mtmp[bi % 2]
                    P.tt(mt.v, ps.v, gtb[:, half * 512:(half + 1) * 512], ALU.mult)
                    P.tt(xg[:, bi, half * 512:(half + 1) * 512], xg[:, bi, half * 512:(half + 1) * 512], mt.v,
                         ALU.add)
                rel_wb(wo)

            dump("mergedT", mergedT.v, [128, 8, 512], BF16)
            dump("x1", xg.v, [128, 4, D], F32)
            P.mark(f'g{g}_wout')
            gtb = gt_load(1, 16384)
            norm_to_hT(sT[:, 8:16], modT[:, 24:32], True)
            dump("h2T", hT.v, [128, 8, 512], BF16)
            dump("combb", combb.v, [128, 4, 16], BF16)
            P.mark(f'g{g}_norm2')
            cb_sb = P.ar("cb_sb", [128, 512], BF16, 0)
            t1 = [P.ar("t1", [128, 512], BF16, 1024)]
            sgm = [P.ar(f"sgm{i}", [128, 512], BF16, 2048 + 1024 * i) for i in range(2)]
            actT = P.ar("actT", [128, 4, 2, 512], BF16, 4096)
            mtmp = [P.ar(f"mtmpb{i}", [128, 512], F32, 12288 + 2048 * i) for i in range(2)]
            WD = [P.ar(f"wd{i}", [128, 2, D], BF16, 21504 + 4096 * i) for i in range(6)]
            wdi = 0
            for rnd in range(4):
                wds = []
                for ee in range(4):
                    e = rnd * 4 + ee
                    wgu = next_wb()
                    wd = WD[wdi % 6]
                    wdi += 1
                    wload(wd.v, wed[l][e].rearrange("(f p) d -> p f d", p=128))
                    wds.append(wd)
                    psc = P.next_ps()
                    for bi in range(4):
                        P.mm(psc[:, bi * 128:(bi + 1) * 128], combb[:, bi, e:e + 1].to_broadcast([128, 128]),
                             identb.v)
                    P.copy(cb_sb.v, psc.v, eng="act")
                    for fc in range(2):
                        psg = proj_feat(wgu, fc * 128, 128)
                        psu = proj_feat(wgu, 256 + fc * 128, 128)
                        sg = sgm[fc]
                        P.act(sg.v, psg.v, AF.Silu)
                        P.tt(t1[0].v, psu.v, sg.v, ALU.mult)
                        P.tt(actT[:, ee, fc, :], t1[0].v, cb_sb.v, ALU.mult)
                    rel_wb(wgu)
                for bi in range(4):
                    for half in range(2):
                        ps = P.next_ps()
                        n = 0
                        for ee in range(4):
                            for fc in range(2):
                                P.mm(ps.v, actT[:, ee, fc, bi * 128:(bi + 1) * 128],
                                     wds[ee][:, fc, half * 512:(half + 1) * 512], start=(n == 0), stop=(n == 7))
                                n += 1
                        mt = mtmp[(bi * 2 + half) % 2]
                        P.tt(mt.v, ps.v, gtb[:, half * 512:(half + 1) * 512], ALU.mult)
                        P.tt(xg[:, bi, half * 512:(half + 1) * 512], xg[:, bi, half * 512:(half + 1) * 512], mt.v,
                             ALU.add)
            P.mark(f'g{g}_moe')
            for bi in range(4):
                gb = g * 4 + bi
                P.dma("sp", out.sub(gb)[gb * 128:(gb + 1) * 128, :], xg[:, bi, :])
    P.fence("sp", [out] + list(dbgo.values()))
    stats = P.emit()
    stats['marks'] = getattr(P, 'marks', [])
    return nc, stats


def _bucket_np(rel):
    import math
    n = np.maximum(rel, 0)
    max_exact = 16
    large = max_exact + (np.log(np.maximum(n, max_exact).astype(np.float32) / max_exact)
                         / math.log(128 / max_exact) * (32 - max_exact)).astype(np.int32)
    large = np.minimum(large, 31)
    return np.where(n < max_exact, n, large)


def host_consts():
    cst = np.zeros((128, NCST), np.float32)
    i = np.arange(128)
    cst[:, C_ID:C_ID + 128] = np.eye(128, dtype=np.float32)
    cst[:, C_TRI:C_TRI + 128] = (i[:, None] <= i[None, :]).astype(np.float32)
    cst[:, C_ONES:C_ONES + 128] = 1.0
    cst[:, C_NEGTRI:C_NEGTRI + 128] = np.where(i[None, :] <= i[:, None], 0.0, BIGNEG)
    cst[:, C_BKD:C_BKD + 128] = _bucket_np(i[None, :] - i[:, None]).astype(np.float32)
    cst[:, C_BKN:C_BKN + 128] = _bucket_np(128 + i[None, :] - i[:, None]).astype(np.float32)
    return cst


def host_layout(inputs, L, b, S):
    f = lambda a: np.ascontiguousarray(np.asarray(a, dtype=np.float32))
    col = lambda v: np.asarray(v, np.float32).reshape(-1, 128).T
    m = {}
    m["x"] = f(inputs["x"][b, :S])
    m["ccol"] = f(col(inputs["c"][b]))
    m["cst"] = host_consts()
    m["rbrow"] = f(np.asarray(inputs["rel_bias"]).reshape(1, 128))
    prm = np.zeros((L, 128, NPRM), np.float32)
    prow = np.zeros((L, 1, NROW), np.float32)
    for l in range(L):
        prm[l, :, PR_G1:PR_G1 + 8] = col(inputs["g_norm1"][l])
        prm[l, :, PR_G2:PR_G2 + 8] = col(inputs["g_norm2"][l])
        prm[l, :, PR_BMOD:PR_BMOD + 48] = col(inputs["b_mod"][l])
        prm[l, :, PR_LNG:PR_LNG + 4] = col(inputs["gmlp_ln_g"][l])
        prm[l, :, PR_LNB:PR_LNB + 4] = col(inputs["gmlp_ln_b"][l])
        prm[l, :, PR_GNG] = np.asarray(inputs["gla_norm_g"][l])
        prm[l, :, PR_GQ] = np.asarray(inputs["dsa_qnorm_g"][l])
        prm[l, :, PR_GK] = np.asarray(inputs["dsa_knorm_g"][l])
        prm[l, :, PR_BBG:PR_BBG + 24] = col(np.asarray(inputs["b_branch_gate"][l]).reshape(-1))
        prow[l, 0, RW_BS:RW_BS + 512] = np.asarray(inputs["gmlp_b_s"][l]).reshape(-1)
        prow[l, 0, RW_BG:RW_BG + 256] = np.asarray(inputs["gla_b_gate"][l])
        prow[l, 0, RW_BR:RW_BR + 4] = np.asarray(inputs["b_group"][l])
        prow[l, 0, RW_BR + 4:RW_BR + 20] = np.asarray(inputs["b_router"][l])
    m["prm"] = prm
    m["prow"] = prow
    m["w_mod"] = f(inputs["w_mod"][:L])
    m["w_in"] = f(inputs["w_in"][:L])
    m["wsT"] = f(np.asarray(inputs["gmlp_w_s"][:L]).transpose(0, 3, 1, 2))
    m["w2"] = f(inputs["gla_w_gate2"][:L])
    m["w_branch"] = f(inputs["w_branch"][:L])
    m["w_out"] = f(inputs["w_out"][:L])
    m["wr"] = f(np.concatenate([np.asarray(inputs["w_group"][:L]), np.asarray(inputs["w_router"][:L])], axis=2))
    m["w_exp_gate"] = f(inputs["w_exp_gate"][:L])
    m["w_exp_up"] = f(inputs["w_exp_up"][:L])
    m["w_exp_down"] = f(inputs["w_exp_down"][:L])
    return m


_PROG_CACHE = {}


def run_model(inputs, L, S, batches, n_cores, want_all=False, active=None):
    key = (L, S)
    if key not in _PROG_CACHE:
        _PROG_CACHE[key] = build_program(L, S)
    nc, stats = _PROG_CACHE[key]
    if active is None:
        active = list(range(min(n_cores, len(batches))))
    shared = host_layout(inputs, L, batches[0], S)
    zeros = None
    in_maps = []
    for ci in range(n_cores):
        if ci in active:
            b = batches[active.index(ci)]
            m = dict(shared)
            m["x"] = np.ascontiguousarray(np.asarray(inputs["x"][b, :S], dtype=np.float32))
            m["ccol"] = np.ascontiguousarray(np.asarray(inputs["c"][b], np.float32).reshape(-1, 128).T)
        else:
            if zeros is None:
                zeros = {k: np.zeros_like(v) for k, v in shared.items()}
            m = dict(zeros)
        in_maps.append(m)
    res = run_bass_kernel_spmd(nc, in_maps, core_ids=list(range(n_cores)))
    if want_all:
        return res.results
    return [res.results[ci]["out"] for ci in active]


def kernel(**inputs):
    L, B, S = 4, 4, 4096
    outs = run_model(inputs, L, S, list(range(B)), 8, active=[0, 1, 4, 5])
    return np.stack(outs, axis=0).astype(np.float32)
```

```python
import numpy as np
import concourse.bass as bass
import concourse.mybir as mybir

F32 = mybir.dt.float32
BF16 = mybir.dt.bfloat16
AF = mybir.ActivationFunctionType
ALU = mybir.AluOpType
AX = mybir.AxisListType

GRAN = 512
SEM_CAP = 30000
STRICT_SAME_ENGINE = True


class Region:
    __slots__ = ("lw", "rd")

    def __init__(self):
        self.lw = None
        self.rd = {}


class View:
    __slots__ = ("buf", "ap")

    def __init__(self, buf, ap):
        self.buf = buf
        self.ap = ap

    def __getitem__(self, idx):
        return View(self.buf, self.ap[idx])

    def rearrange(self, s, **kw):
        return View(self.buf, self.ap.rearrange(s, **kw))

    def bitcast(self, dt):
        return View(self.buf, self.ap.bitcast(dt))

    def to_broadcast(self, shape):
        return View(self.buf, self.ap.to_broadcast(shape))

    def partition_broadcast(self, n):
        return View(self.buf, self.ap.partition_broadcast(n))


class Buf:
    def __init__(self, name, h, regions, is_dram=False):
        self.name = name
        self.h = h
        self.regions = regions
        self.is_dram = is_dram

    def __getitem__(self, idx):
        base = self.h.ap() if self.is_dram else self.h
        return View(self, base[idx])

    @property
    def v(self):
        base = self.h.ap() if self.is_dram else self.h[:]
        return View(self, base)

    def sub(self, i, n=1):
        return Buf(self.name, self.h, self.regions[i:i + n], self.is_dram)


class Op:
    __slots__ = ("eng", "fn", "kind", "deps", "lidx", "flag", "waits", "clock",
                 "fnum", "dma_sem", "dma_val", "dq_n")


class Prog:
    ENGS = ("pe", "act", "dve", "pool", "sp")

    def __init__(self, nc, n_dma_sems=None):
        self.nc = nc
        self.h = {"pe": nc.tensor, "act": nc.scalar, "dve": nc.vector,
                  "pool": nc.gpsimd, "sp": nc.sync}
        self.ops = []
        self.n_dma_sems = n_dma_sems or {"sp": 16, "pool": 16, "act": 4}
        self.dma_count = {e: 0 for e in self.ENGS}
        self.dma_ids = {e: [] for e in self.ENGS}
        self.arena_base = None
        self.arena_regions = None
        self.psum_rot = []
        self.psum_i = 0

    def sb(self, name, shape, dtype):
        h = self.nc.alloc_sbuf_tensor(name, list(shape), dtype)
        return Buf(name, h, [Region()])

    def make_arena(self, nbytes):
        r = self.nc.bump_sbuf(nbytes)
        self.arena_base = r[0]
        self.arena_size = nbytes
        self.arena_regions = [Region() for _ in range((nbytes + GRAN - 1) // GRAN)]

    def ar(self, name, shape, dtype, off):
        key = (name, tuple(shape), str(dtype), off)
        if not hasattr(self, "_arc"):
            self._arc = {}
        if key in self._arc:
            return self._arc[key]
        b = self._ar(f"{name}_{len(self._arc)}", shape, dtype, off)
        self._arc[key] = b
        return b

    def _ar(self, name, shape, dtype, off):
        esz = {F32: 4, BF16: 2}[dtype]
        n = int(np.prod(shape[1:])) * esz
        assert off % 32 == 0 and off + n <= self.arena_size, (name, off, n, self.arena_size)
        h = self.nc.alloc_sbuf_tensor_at(name, list(shape), dtype, offset=self.arena_base + off)
        regs = self.arena_regions[off // GRAN:(off + n + GRAN - 1) // GRAN]
        return Buf(name, h, regs)

    def ps(self, name, shape=(128, 512), dtype=F32):
        h = self.nc.alloc_psum_tensor(name, list(shape), dtype)
        return Buf(name, h, [Region()])

    def dram(self, name, shape, dtype, kind="Internal", nreg=1):
        h = self.nc.dram_tensor(name, list(shape), dtype, kind=kind)
        return Buf(name, h, [Region() for _ in range(nreg)], is_dram=True)

    def fence(self, eng, bufs):
        h = self.h[eng]
        self.add(eng, lambda: h.nop(), bufs, [])

    def mark(self, name):
        if not getattr(self, "marks_on", False):
            return
        last = {}
        for oid in range(len(self.ops) - 1, -1, -1):
            o = self.ops[oid]
            if o.kind == "c" and o.eng != "sp" and o.eng not in last:
                last[o.eng] = oid
            if len(last) == 4:
                break
        h = self.h["sp"]
        oid = self.add("sp", lambda: h.nop(), [], [])
        for e, d in last.items():
            self.ops[oid].deps[d] = True
        if not hasattr(self, "marks"):
            self.marks = []
        self.marks.append(name)

    def next_ps(self):
        b = self.psum_rot[self.psum_i % len(self.psum_rot)]
        self.psum_i += 1
        return b

    def add(self, eng, fn, reads, writes, kind="c"):
        op = Op()
        op.eng = eng
        op.fn = fn
        op.kind = kind
        op.flag = False
        op.deps = {}
        oid = len(self.ops)
        rregs = []
        for b in reads:
            if b is None:
                continue
            b = b.buf if isinstance(b, View) else b
            rregs.extend(b.regions)
        wregs = []
        for b in writes:
            b = b.buf if isinstance(b, View) else b
            wregs.extend(b.regions)
        for r in rregs:
            if r.lw is not None:
                op.deps[r.lw] = True
        for r in wregs:
            if r.lw is not None:
                op.deps.setdefault(r.lw, False)
            for rid in r.rd.values():
                op.deps.setdefault(rid, False)
        op.deps.pop(oid, None)
        for r in rregs:
            key = eng if kind == "c" else ("dma", oid)
            r.rd[key] = oid
        for r in wregs:
            r.lw = oid
            r.rd = {}
        if kind == "dma":
            n = self.dma_count[eng]
            self.dma_count[eng] += 1
            op.dq_n = n
            self.dma_ids[eng].append(oid)
        self.ops.append(op)
        return oid

    @staticmethod
    def _a(x):
        return x.ap if isinstance(x, View) else x

    def mm(self, out, lhsT, rhs, start=True, stop=True, skip=False):
        nc = self.nc
        if skip:
            self.add("pe", lambda: nc.tensor.matmul(out.ap, lhsT.ap, rhs.ap, start=start, stop=stop,
                                                    skip_group_check=True), [lhsT, rhs], [out])
        else:
            self.add("pe", lambda: nc.tensor.matmul(out.ap, lhsT.ap, rhs.ap, start=start, stop=stop),
                     [lhsT, rhs], [out])

    def tr(self, out, in_, ident):
        nc = self.nc
        self.add("pe", lambda: nc.tensor.transpose(out.ap, in_.ap, ident.ap), [in_, ident], [out])

    def act(self, out, in_, func, bias=None, scale=1.0, accum=None):
        nc = self.nc
        a = self._a
        kw = {}
        if bias is not None:
            kw["bias"] = a(bias)
        if accum is not None:
            kw["accum_out"] = a(accum)
        rd = [in_] + [x for x in (bias, scale) if isinstance(x, View)]
        wr = [out] + ([accum] if accum is not None else [])
        self.add("act", lambda: nc.scalar.activation(out=out.ap, in_=in_.ap, func=func,
                                                      scale=a(scale), **kw), rd, wr)

    def _veng(self, eng):
        return self.nc.vector if eng == "dve" else self.nc.gpsimd

    def tt(self, out, in0, in1, op, eng="dve"):
        e = self._veng(eng)
        self.add(eng, lambda: e.tensor_tensor(out=out.ap, in0=in0.ap, in1=in1.ap, op=op),
                 [in0, in1], [out])

    def ts(self, out, in0, s1, s2, op0, op1=None, accum=None, eng="dve"):
        e = self._veng(eng)
        a = self._a
        kw = {}
        if op1 is not None:
            kw["op1"] = op1
        if accum is not None:
            kw["accum_out"] = a(accum)
        rd = [in0] + [x for x in (s1, s2) if isinstance(x, View)]
        wr = [out] + ([accum] if accum is not None else [])
        self.add(eng, lambda: e.tensor_scalar(out=out.ap, in0=in0.ap, scalar1=a(s1), scalar2=a(s2),
                                              op0=op0, **kw), rd, wr)

    def stt(self, out, in0, scalar, in1, op0, op1, eng="dve"):
        e = self._veng(eng)
        a = self._a
        rd = [in0, in1] + ([scalar] if isinstance(scalar, View) else [])
        self.add(eng, lambda: e.scalar_tensor_tensor(out=out.ap, in0=in0.ap, scalar=a(scalar),
                                                     in1=in1.ap, op0=op0, op1=op1), rd, [out])

    def copy(self, out, in_, eng="dve"):
        if eng == "act":
            nc = self.nc
            self.add("act", lambda: nc.scalar.copy(out=out.ap, in_=in_.ap), [in_], [out])
        else:
            e = self._veng(eng)
            self.add(eng, lambda: e.tensor_copy(out=out.ap, in_=in_.ap), [in_], [out])

    def memset(self, out, val, eng="dve"):
        e = self._veng(eng)
        self.add(eng, lambda: e.memset(out.ap, val), [], [out])

    def reduce(self, out, in_, op, eng="dve"):
        e = self._veng(eng)
        self.add(eng, lambda: e.tensor_reduce(out=out.ap, in_=in_.ap, axis=AX.X, op=op), [in_], [out])

    def recip(self, out, in_):
        nc = self.nc
        self.add("dve", lambda: nc.vector.reciprocal(out=out.ap, in_=in_.ap), [in_], [out])

    def dma(self, q, out, in_, **kw):
        h = self.h[q]
        self.add(q, lambda: h.dma_start(out=out.ap, in_=in_.ap, **kw), [in_], [out], kind="dma")

    def emit(self):
        nc = self.nc
        ops = self.ops
        known = {e: {} for e in self.ENGS}
        dma_known = {e: set() for e in self.ENGS}
        count = {e: 0 for e in self.ENGS}
        for oid, op in enumerate(ops):
            E = op.eng
            waits = []
            kn = known[E]
            if op.kind == "dma":
                ns = self.n_dma_sems[E]
                if op.dq_n >= ns:
                    prev = self.dma_ids[E][op.dq_n - ns]
                    if prev not in dma_known[E]:
                        waits.append(prev)
                        dma_known[E].add(prev)
            for d, raw in op.deps.items():
                p = ops[d]
                if p.kind == "dma":
                    if d in dma_known[E]:
                        continue
                    waits.append(d)
                    dma_known[E].add(d)
                else:
                    if p.eng == E and (E == "pe" or (not raw and not STRICT_SAME_ENGINE)):
                        continue
                    if kn.get(p.eng, 0) >= p.lidx:
                        continue
                    waits.append(d)
                    p.flag = True
                    for e2, v in p.clock.items():
                        if kn.get(e2, 0) < v:
                            kn[e2] = v
                    if kn.get(p.eng, 0) < p.lidx:
                        kn[p.eng] = p.lidx
            op.waits = waits
            count[E] += 1
            op.lidx = count[E]
            op.clock = dict(kn) if op.kind == "c" else None
        fcount = {e: 0 for e in self.ENGS}
        for op in ops:
            if op.kind == "c" and op.flag:
                fcount[op.eng] += 1
                op.fnum = fcount[op.eng]
        sems = {}
        for e in self.ENGS:
            n = (fcount[e] + SEM_CAP - 1) // SEM_CAP
            sems[e] = [nc.alloc_semaphore(f"s_{e}_{i}") for i in range(n)]
        dsems = {}
        for e in self.ENGS:
            if self.dma_count[e]:
                dsems[e] = [nc.alloc_semaphore(f"d_{e}_{i}")
                            for i in range(min(self.n_dma_sems[e], self.dma_count[e]))]
        for op in ops:
            if op.kind == "dma":
                ns = self.n_dma_sems[op.eng]
                op.dma_sem = dsems[op.eng][op.dq_n % ns]
                op.dma_val = 16 * (op.dq_n // ns + 1)
        nwaits = 0
        for op in ops:
            h = self.h[op.eng]
            for d in op.waits:
                p = ops[d]
                if p.kind == "dma":
                    h.wait_ge(p.dma_sem, p.dma_val)
                else:
                    n = p.fnum - 1
                    h.wait_ge(sems[p.eng][n // SEM_CAP], n % SEM_CAP + 1)
                nwaits += 1
            ins = op.fn()
            if op.kind == "dma":
                ins.then_inc(op.dma_sem, 16)
            elif op.flag:
                n = op.fnum - 1
                ins.then_inc(sems[op.eng][n // SEM_CAP], 1)
        self.stats = dict(n_ops=len(ops), n_waits=nwaits, flagged=dict(fcount),
                          per_eng=dict(count))
        return self.stats

    def final_wait(self, eng, opids):
        pass


from concourse.bass_utils import run_bass_kernel_spmd

D = 1024
EPS = 1e-6
N_IN = 7508
OFF = dict(u=0, v=512, gq=1024, gk=1280, gv=1536, gr=2048, ga=2560, dq=2576, dk=3088,
           dv=3600, diq=4112, dik=4368, diw=4432, gates=4436)
NEG = -30000.0
BIGNEG = -1.0e30
NBIS = 12
C_ID, C_TRI, C_ONES, C_NEGTRI, C_BKD, C_BKN, NCST = 0, 128, 256, 384, 512, 640, 768
PR_G1, PR_G2, PR_BMOD, PR_LNG, PR_LNB, PR_GNG, PR_GQ, PR_GK, PR_BBG, NPRM = 0, 8, 16, 64, 68, 72, 73, 74, 75, 99
RW_BS, RW_BG, RW_BR, NROW = 0, 512, 768, 788
WSM_GA, WSM_IQ, WSM_IK, WSM_IW, WSM_N = 0, 16, 272, 336, 340


def build_program(L, S, dbg=False, marks=False):
    NBLK = S // 128
    NG = S // 512
    nc = bass.Bass("TRN2", target_bir_lowering=False)
    P = Prog(nc)
    P.marks_on = marks
    EI = "ExternalInput"
    x_in = P.dram("x", [S, D], F32, kind=EI)
    ccol = P.dram("ccol", [128, 8], F32, kind=EI)
    cst = P.dram("cst", [128, NCST], F32, kind=EI)
    rbd = P.dram("rbrow", [1, 128], F32, kind=EI)
    w_mod = P.dram("w_mod", [L, D, 6 * D], F32, kind=EI)
    prm = P.dram("prm", [L, 128, NPRM], F32, kind=EI)
    prow = P.dram("prow", [L, 1, NROW], F32, kind=EI)
    w_in = P.dram("w_in", [L, D, N_IN], F32, kind=EI)
    wsT = P.dram("wsT", [L, 128, 4, 128], F32, kind=EI)
    w2 = P.dram("w2", [L, 16, 256], F32, kind=EI)
    w_br = P.dram("w_branch", [L, 3, 512, D], F32, kind=EI)
    w_out = P.dram("w_out", [L, D, D], F32, kind=EI)
    wr = P.dram("wr", [L, D, 20], F32, kind=EI)
    weg = P.dram("w_exp_gate", [L, 16, D, 256], F32, kind=EI)
    weu = P.dram("w_exp_up", [L, 16, D, 256], F32, kind=EI)
    wed = P.dram("w_exp_down", [L, 16, 256, D], F32, kind=EI)
    out = P.dram("out", [S, D], F32, kind="ExternalOutput", nreg=NBLK)
    gt_dram = P.dram("gt_bc", [2, 128, D], F32, nreg=2)
    w_in_b = P.dram("w_in_b", [2, D, N_IN], BF16, nreg=2)
    w_br_b = P.dram("w_br_b", [2, 3, 512, D], BF16, nreg=2)
    w_out_b = P.dram("w_out_b", [2, D, D], BF16, nreg=2)
    weg_b = P.dram("weg_b", [2, 16, D, 256], BF16, nreg=2)
    weu_b = P.dram("weu_b", [2, 16, D, 256], BF16, nreg=2)
    wed_b = P.dram("wed_b", [2, 16, 256, D], BF16, nreg=2)

    def wsrc(l):
        if l == 0:
            return ("pool", w_in[0], w_br[0], w_out[0], weg[0], weu[0], wed[0])
        p_ = l % 2
        return ("sp", w_in_b.sub(p_)[p_], w_br_b.sub(p_)[p_], w_out_b.sub(p_)[p_],
                weg_b.sub(p_)[p_], weu_b.sub(p_)[p_], wed_b.sub(p_)[p_])

    def cast_chunks(l):
        _, di, db, do, dg, du, dd = wsrc(l)
        ch = []
        for k in range(8):
            ch.append((di[k * 128:(k + 1) * 128, :], w_in[l][k * 128:(k + 1) * 128, :]))
        for n in range(3):
            for e in range(4):
                ch.append((db[n][e * 128:(e + 1) * 128, :], w_br[l][n][e * 128:(e + 1) * 128, :]))
        for k in range(8):
            ch.append((do[k * 128:(k + 1) * 128, :], w_out[l][k * 128:(k + 1) * 128, :]))
        for e in range(16):
            ch.append((dg[e].rearrange("(k p) n -> p k n", p=128), weg[l][e].rearrange("(k p) n -> p k n", p=128)))
            ch.append((du[e].rearrange("(k p) n -> p k n", p=128), weu[l][e].rearrange("(k p) n -> p k n", p=128)))
            ch.append((dd[e].rearrange("(f p) d -> p f d", p=128), wed[l][e].rearrange("(f p) d -> p f d", p=128)))
        return ch
    dbgo = {}

    def dump(name, view, shape, dtype):
        if not dbg or name in dbgo:
            return
        t = P.dram("dbg_" + name, list(shape), dtype, kind="ExternalOutput")
        dbgo[name] = t
        P.dma("sp", t.v, view)

    cf = P.sb("cf", [128, 512], F32)
    ident4 = P.sb("ident4", [128, 512], BF16)
    identb = P.sb("identb", [128, 128], BF16)
    biasT = P.sb("biasT", [128, 2, 4, 128], BF16)
    kT_all = P.sb("kT_all", [128, 4, S], BF16)
    v_all = P.sb("v_all", [128, NBLK, 4, 130], BF16)
    ikT_all = P.sb("ikT_all", [64, S], BF16)
    xg = P.sb("xg", [128, 4, D], F32)
    hT = P.sb("hT", [128, 8, 512], BF16)
    ysT = P.sb("ysT", [128, 3, 4, 512], BF16)
    WB = [P.sb(f"wb{i}", [128, 8, 512], BF16) for i in range(3)]
    wsm = P.sb("wsm", [128, 8, WSM_N], BF16)
    prm_sb = P.sb("prm_sb", [128, NPRM], F32)
    modT = P.sb("modT", [128, 48], F32)
    sT = P.sb("sT", [128, 16], F32)
    WsTm = P.sb("WsTm", [128, 4, 128], BF16)
    Cg = P.sb("Cg", [128, 4, 128], F32)
    w2_sb = P.sb("w2_sb", [16, 256], BF16)
    bg_sb = P.sb("bg_sb", [1, 256], BF16)
    ones1 = P.sb("ones1", [1, 128], BF16)
    wr_sb = P.sb("wr_sb", [128, 8, 20], F32)
    rbias = P.sb("rbias", [128, 20], F32)
    cactT = P.sb("cactT", [128, 8], BF16)
    Sf = P.sb("Sf", [64, 4, 128], F32)
    Sb = P.sb("Sb", [64, 4, 128], BF16)
    stat = P.sb("stat", [128, 64], F32)
    combb = P.sb("combb", [128, 4, 16], BF16)
    fv = P.sb("fv", [128, NBIS], F32)
    offs = P.sb("offs", [128, NBIS], F32)
    PSR = [P.ps(f"psr{i}") for i in range(6)]
    psA = P.ps("psA")
    psB = P.ps("psB")
    P.psum_rot = PSR
    ARENA = max(46 * 1024, 23552 + 8 * S)
    P.make_arena(ARENA)

    identf = cf[:, C_ID:C_ID + 128]
    trif = cf[:, C_TRI:C_TRI + 128]
    onesf = cf[:, C_ONES:C_ONES + 128]
    negtri = cf[:, C_NEGTRI:C_NEGTRI + 128]

    def wload(dst, src):
        P.dma("pool", dst, src)

    class WStream:
        def __init__(self, bufs):
            self.free = list(bufs)
            self.sched = []
            self.issued = []
            self.nissued = 0

        def push(self, fn):
            self.sched.append(fn)

        def _pump(self):
            while self.free and self.nissued < len(self.sched):
                b = self.free.pop(0)
                self.sched[self.nissued](b)
                self.nissued += 1
                self.issued.append(b)

        def get(self):
            self._pump()
            assert self.issued, "weight schedule underflow / no free buffer"
            return self.issued.pop(0)

        def release(self, b):
            self.free.append(b)
            self._pump()

    ws_main = WStream(WB)

    def sched_kxn(src2d, c0, n, q="pool"):
        ws_main.push(lambda wb: P.dma(q, wb[:, :, 0:n],
                                      src2d.rearrange("(k p) n -> p k n", p=128)[:, :, c0:c0 + n]))

    def sched_layer_mod(l):
        for i in range(12):
            sched_kxn(w_mod[l], i * 512, 512)

    def sched_group(l):
        q, s_in, s_br, s_out, s_eg, s_eu, s_ed = wsrc(l)
        for nm in ("u", "v", "gq", "gv", "gr", "dq", "dk", "dv"):
            sched_kxn(s_in, OFF[nm], 512, q)
        for n in range(3):
            ws_main.push(lambda wb, n=n: P.dma(q,
                wb.v.rearrange("p a b -> p (a b)").rearrange("p (e d) -> p e d", e=4),
                s_br[n].rearrange("(e p) d -> p e d", p=128)))
            for half in range(2):
                sched_kxn(s_in, OFF["gates"] + (n * 2 + half) * 512, 512, q)
        for half in range(2):
            sched_kxn(s_out, half * 512, 512, q)
        for e in range(16):
            def f(wb, e=e):
                P.dma(q, wb[:, :, 0:256], s_eg[e].rearrange("(k p) n -> p k n", p=128))
                P.dma(q, wb[:, :, 256:512], s_eu[e].rearrange("(k p) n -> p k n", p=128))
            ws_main.push(f)

    def next_wb():
        return ws_main.get()

    def rel_wb(b):
        ws_main.release(b)

    P.dma("sp", cf.v, cst[:, 0:512])
    bk = P.ar("bk", [128, 256], F32, 2048)
    P.dma("sp", bk.v, cst[:, 512:768])
    for i4 in range(4):
        P.dma("pool", ident4[:, i4 * 128:(i4 + 1) * 128], cst[:, C_ID:C_ID + 128])
    P.dma("pool", identb.v, cst[:, C_ID:C_ID + 128])
    P.memset(ones1.v, 1.0)
    for it in range(NBIS):
        P.memset(fv[:, it:it + 1], 0.5 ** (it + 1))
    P.memset(v_all.v, 1.0)
    rbb = P.ar("rbb", [128, 128], F32, 0)
    bacc = P.ar("bacc", [128, 128], F32, 512)
    btmp = P.ar("btmp", [128, 128], F32, 1024)
    P.dma("sp", rbb.v, rbd.v.partition_broadcast(128))
    for h in range(4):
        for dist, cb in ((0, 0), (1, 128)):
            for b in range(32):
                P.ts(btmp.v, bk[:, cb:cb + 128], float(b), None, ALU.is_equal)
                if b == 0:
                    P.ts(bacc.v, btmp.v, rbb[:, b * 4 + h:b * 4 + h + 1], None, ALU.mult)
                else:
                    P.stt(bacc.v, btmp.v, rbb[:, b * 4 + h:b * 4 + h + 1], bacc.v, ALU.mult, ALU.add)
            P.ts(biasT[:, dist, h, :], bacc.v, rbb[:, 31 * 4 + h:31 * 4 + h + 1], None, ALU.subtract)

    def small_rstd(dst, src, inv_n):
        P.ts(dst, src, inv_n, EPS, ALU.mult, ALU.add)
        P.act(dst, dst, AF.Sqrt)
        P.recip(dst, dst)

    def norm_to_hT(scol, shcol, fp32_router):
        junk = P.ar("junk", [128, D], BF16, 0)
        ssq = stat[:, 0:4]
        rstd = stat[:, 4:8]
        for bi in range(4):
            P.act(junk.v, xg[:, bi, :], AF.Square, accum=stat[:, bi:bi + 1])
        small_rstd(rstd, ssq, 1.0 / D)
        if not fp32_router:
            xns = [P.ar(f"xn{i}", [128, D], BF16, 2048 + 2048 * i) for i in range(2)]
            for bi in range(4):
                xn = xns[bi % 2]
                P.ts(xn.v, xg[:, bi, :], stat[:, 4 + bi:5 + bi], None, ALU.mult)
                ps = P.next_ps()
                psb = ps.v.bitcast(BF16)
                for k in range(8):
                    P.tr(psb[:, k * 128:(k + 1) * 128], xn[:, k * 128:(k + 1) * 128], identb.v)
                for k in range(8):
                    o = hT[:, k, bi * 128:(bi + 1) * 128]
                    if k % 2 == 0:
                        P.act(o, psb[:, k * 128:(k + 1) * 128], AF.Identity,
                              bias=shcol[:, k:k + 1], scale=scol[:, k:k + 1])
                    else:
                        P.ts(o, psb[:, k * 128:(k + 1) * 128], scol[:, k:k + 1], shcol[:, k:k + 1],
                             ALU.mult, ALU.add)
        else:
            xn32 = P.ar("xn32", [128, D], F32, 2048)
            h32 = P.ar("h32", [128, 8, 128], F32, 6144)
            lg_all = P.ar("lg_all", [128, 4, 20], F32, 10240)
            for bi in range(4):
                P.ts(xn32.v, xg[:, bi, :], stat[:, 4 + bi:5 + bi], None, ALU.mult)
                pss = [P.next_ps(), P.next_ps()]
                for k in range(8):
                    P.mm(pss[k // 4][:, (k % 4) * 128:(k % 4 + 1) * 128],
                         xn32[:, k * 128:(k + 1) * 128], identf)
                for k in range(8):
                    src = pss[k // 4][:, (k % 4) * 128:(k % 4 + 1) * 128]
                    if k % 2 == 0:
                        P.act(h32[:, k, :], src, AF.Identity, bias=shcol[:, k:k + 1], scale=scol[:, k:k + 1])
                    else:
                        P.ts(h32[:, k, :], src, scol[:, k:k + 1], shcol[:, k:k + 1], ALU.mult, ALU.add)
                P.copy(hT[:, :, bi * 128:(bi + 1) * 128], h32.v)
                psr = P.next_ps()
                for k in range(8):
                    P.mm(psr[:, 0:20], h32[:, k, :], wr_sb[:, k, :], start=(k == 0), stop=(k == 7))
                P.tt(lg_all[:, bi, :], psr[:, 0:20], rbias.v, ALU.add)
            router_all()

    def proj_tok(wb, c0, n, bi):
        ps = P.next_ps()
        for k in range(8):
            P.mm(ps[:, 0:n], hT[:, k, bi * 128:(bi + 1) * 128], wb[:, k, c0:c0 + n],
                 start=(k == 0), stop=(k == 7))
        return ps

    def proj_feat(wb, c0, m):
        ps = P.next_ps()
        for k in range(8):
            P.mm(ps[0:m, :], wb[:, k, c0:c0 + m], hT[:, k, :], start=(k == 0), stop=(k == 7))
        return ps

    def router_all():
        BIG = 1.0e30
        o = [10240 + 320]

        def fld(name, n):
            b_ = P.ar("r_" + name, [128, 4, n], F32, o[0])
            o[0] += ((4 * n * 4 + 31) // 32) * 32
            return b_

        lg_all = P.ar("lg_all", [128, 4, 20], F32, 10240)
        gmax, gsum, pg, m1, m2, dd, e21, w1, w1p, w2p = [fld(n_, 1) for n_ in
                                                         ("gmax", "gsum", "pg", "m1", "m2", "dd", "e21", "w1", "w1p", "w2p")]
        d4, ge, oh, negm = [fld(n_, 4) for n_ in ("d4", "ge", "oh", "negm")]
        elm, oh1, elm2, oh2, cmb, cmb2 = [fld(n_, 16) for n_ in ("elm", "oh1", "elm2", "oh2", "cmb", "cmb2")]
        f2 = lambda t: t.v.rearrange("p b o -> p (b o)")
        bc = lambda t, n: t.v.to_broadcast([128, 4, n])
        lgg = lg_all[:, :, 0:4]
        lge = lg_all[:, :, 4:20]
        P.reduce(f2(gmax), lgg, ALU.max)
        P.tt(d4.v, lgg, bc(gmax, 4), ALU.subtract)
        P.act(ge.v, d4.v, AF.Exp)
        P.reduce(f2(gsum), ge.v, ALU.add)
        P.recip(pg.v, gsum.v)
        P.tt(oh.v, lgg, bc(gmax, 4), ALU.is_equal)
        P.ts(negm.v, oh.v, 1.0, BIG, ALU.subtract, ALU.mult)
        P.tt(elm.v.rearrange("p b (g e) -> p b g e", g=4), lge.rearrange("p b (g e) -> p b g e", g=4),
             negm.v.rearrange("p b (g o) -> p b g o", o=1).to_broadcast([128, 4, 4, 4]), ALU.add)
        P.reduce(f2(m1), elm.v, ALU.max)
        P.tt(oh1.v, elm.v, bc(m1, 16), ALU.is_equal)
        P.stt(elm2.v, oh1.v, -BIG, elm.v, ALU.mult, ALU.add)
        P.reduce(f2(m2), elm2.v, ALU.max)
        P.tt(oh2.v, elm2.v, bc(m2, 16), ALU.is_equal)
        P.tt(dd.v, m2.v, m1.v, ALU.subtract)
        P.act(e21.v, dd.v, AF.Exp)
        P.ts(w1.v, e21.v, 1.0, None, ALU.add)
        P.recip(w1.v, w1.v)
        P.tt(w1p.v, w1.v, pg.v, ALU.mult)
        P.tt(w2p.v, w1p.v, e21.v, ALU.mult)
        P.tt(cmb.v, oh1.v, bc(w1p, 16), ALU.mult)
        P.tt(cmb2.v, oh2.v, bc(w2p, 16), ALU.mult)
        P.tt(combb.v, cmb.v, cmb2.v, ALU.add)

    for l in range(L):
        xsrc = x_in if l == 0 else out
        sched_layer_mod(l)
        for _g in range(NG):
            sched_group(l)
        P.dma("sp", prm_sb.v, prm[l])
        P.dma("pool", w2_sb.v, w2[l])
        P.dma("pool", bg_sb.v, prow[l][:, RW_BG:RW_BG + 256])
        P.dma("sp", wr_sb.v, wr[l].rearrange("(k p) n -> p k n", p=128))
        P.dma("sp", rbias.v, prow[l][:, RW_BR:RW_BR + 20].partition_broadcast(128))
        wl = w_in[l].rearrange("(k p) n -> p k n", p=128)
        wload(wsm[:, :, WSM_GA:WSM_GA + 16], wl[:, :, OFF["ga"]:OFF["ga"] + 16])
        wload(wsm[:, :, WSM_IQ:WSM_N], wl[:, :, OFF["diq"]:OFF["diq"] + 324])
        cc32 = P.ar("cc32", [128, 8], F32, 0)
        P.dma("sp", cc32.v, ccol.v)
        P.act(cactT.v, cc32.v, AF.Silu)
        psm = P.next_ps()
        for i in range(12):
            wb = next_wb()
            for jj in range(4):
                j = i * 4 + jj
                for k in range(8):
                    P.mm(psm[:, j:j + 1], wb[:, k, jj * 128:(jj + 1) * 128], cactT[:, k:k + 1],
                         start=(k == 0), stop=(k == 7))
            rel_wb(wb)
        P.tt(modT.v, psm[:, 0:48], prm_sb[:, PR_BMOD:PR_BMOD + 48], ALU.add)
        P.stt(sT[:, 0:8], modT[:, 8:16], 1.0, prm_sb[:, PR_G1:PR_G1 + 8], ALU.add, ALU.mult)
        P.stt(sT[:, 8:16], modT[:, 32:40], 1.0, prm_sb[:, PR_G2:PR_G2 + 8], ALU.add, ALU.mult)
        ws32 = P.ar("ws32", [128, 4, 128], F32, 2048)
        P.dma("sp", ws32.v, wsT[l])
        P.tt(ws32.v, ws32.v, trif.rearrange("p (o t) -> p o t", o=1).to_broadcast([128, 4, 128]), ALU.mult)
        P.copy(WsTm.v, ws32.v)
        bsrow = P.ar("bsrow", [1, 512], F32, 4096)
        P.dma("sp", bsrow.v, prow[l][:, RW_BS:RW_BS + 512])
        bsb = P.ar("bsb", [128, 4, 128], F32, 6144)
        for g in range(4):
            ps1 = P.next_ps()
            P.mm(ps1[:, 0:128], onesf[0:1, :], bsrow[:, g * 128:(g + 1) * 128])
            P.copy(bsb[:, g, :], ps1[:, 0:128], eng="act")
            P.mm(ps1[:, 128:256], onesf, ws32[:, g, :])
            P.stt(Cg[:, g, :], ps1[:, 128:256], prm_sb[:, PR_LNB + g:PR_LNB + g + 1], bsb[:, g, :],
                  ALU.mult, ALU.add)
        P.ts(stat[:, 32:33], prm_sb[:, PR_GQ:PR_GQ + 1], float(128 ** -0.5), None, ALU.mult)
        gq_col = stat[:, 32:33]
        gk_col = prm_sb[:, PR_GK:PR_GK + 1]
        gng_col = prm_sb[:, PR_GNG:PR_GNG + 1]
        P.memset(Sf.v, 0.0)
        P.memset(Sb.v, 0.0)

        def gt_bcast(col0, o_gtb, o_dg):
            gtb = P.ar("gtb", [128, D], F32, o_gtb)
            dg = P.ar("dg", [128, 128], F32, o_dg)
            pss = [P.next_ps(), P.next_ps()]
            for k in range(8):
                P.ts(dg.v, identf, modT[:, col0 + k:col0 + k + 1], None, ALU.mult)
                P.mm(pss[k // 4][:, (k % 4) * 128:(k % 4 + 1) * 128], onesf, dg.v)
            P.copy(gtb[:, 0:512], pss[0].v, eng="act")
            P.copy(gtb[:, 512:1024], pss[1].v, eng="act")
            return gtb

        for gi, col0 in enumerate((16, 40)):
            gtb0 = gt_bcast(col0, 16384, 20480)
            P.dma("sp", gt_dram.sub(gi)[gi], gtb0.v)

        def gt_load(gi, o_gtb):
            gtb_ = P.ar("gtb", [128, D], F32, o_gtb)
            P.dma("sp", gtb_.v, gt_dram.sub(gi)[gi])
            return gtb_

        for g in range(NG):
            for bi in range(4):
                gb = g * 4 + bi
                P.dma("sp", xg[:, bi, :], (xsrc if l == 0 else out.sub(gb))[gb * 128:(gb + 1) * 128, :])
            P.mark(f'g{g}_start')
            norm_to_hT(sT[:, 0:8], modT[:, 0:8], False)
            P.mark(f'g{g}_norm1')
            dump("modT", modT.v, [128, 48], F32)
            dump("hT", hT.v, [128, 8, 512], BF16)

            uT = P.ar("uT", [128, 4, 512], BF16, 8192)
            vg = P.ar("vg", [128, 4, 512], F32, 12288)
            vn = P.ar("vn", [128, 4, 512], BF16, 20480)
            gtmp = [P.ar(f"gtmp{i}", [128, 128], F32, 24576 + 512 * i) for i in range(2)]
            gjunk = P.ar("gjunk", [128, 512], BF16, 25600)
            wu = next_wb()
            for c in range(4):
                ps = proj_feat(wu, c * 128, 128)
                P.act(uT[:, c, :], ps.v, AF.Gelu_apprx_tanh)
            rel_wb(wu)
            wv = next_wb()
            for bi in range(4):
                ps = proj_tok(wv, 0, 512, bi)
                P.act(vg[:, bi, :], ps.v, AF.Gelu_apprx_tanh, accum=stat[:, 8 + bi:9 + bi])
                P.act(gjunk.v, vg[:, bi, :], AF.Square, accum=stat[:, 12 + bi:13 + bi])
            rel_wb(wv)
            mean = stat[:, 16:20]
            P.ts(mean, stat[:, 8:12], 1.0 / 512, None, ALU.mult)
            msq = stat[:, 20:24]
            P.tt(msq, mean, mean, ALU.mult)
            var = stat[:, 24:28]
            P.stt(var, stat[:, 12:16], 1.0 / 512, msq, ALU.mult, ALU.subtract)
            small_rstd(var, var, 1.0)
            for bi in range(4):
                P.ts(vn[:, bi, :], vg[:, bi, :], stat[:, 16 + bi:17 + bi], stat[:, 24 + bi:25 + bi],
                     ALU.subtract, ALU.mult)
            for bi in range(4):
                ps = P.next_ps()
                for gg in range(4):
                    P.mm(ps[:, gg * 128:(gg + 1) * 128], vn[:, bi, gg * 128:(gg + 1) * 128], WsTm[:, gg, :])
                for gg in range(4):
                    tmp = gtmp[gg % 2]
                    P.stt(tmp.v, ps[:, gg * 128:(gg + 1) * 128], prm_sb[:, PR_LNG + gg:PR_LNG + gg + 1],
                          Cg[:, gg, :], ALU.mult, ALU.add)
                    P.tt(ysT[:, 0, gg, bi * 128:(bi + 1) * 128], tmp.v, uT[:, gg, bi * 128:(bi + 1) * 128],
                         ALU.mult)

            P.mark(f'g{g}_gmlp')
            gaT = P.ar("gaT", [16, 512], BF16, 8192)
            ps = proj_feat(wsm, WSM_GA, 16)
            P.copy(gaT.v, ps[0:16, :])
            qk_all = P.ar("qk_all", [128, 4, 512], F32, 9216)
            v_allg = P.ar("v_allg", [128, 4, 512], BF16, 17408)
            r_all = P.ar("r_all", [128, 4, 512], BF16, 21504)
            l_sb = P.ar("l_sb", [128, 256], F32, 25600)
            e_sb = P.ar("e_sb", [128, 256], F32, 26624)
            eb = P.ar("eb", [128, 256], F32, 27648)
            ebp = P.ar("ebp", [128, 256], F32, 28672)
            ebl = P.ar("ebl", [128, 256], F32, 29696)
            tmpk = P.ar("tmpk", [128, 256], F32, 30720)
            qe = P.ar("qe", [128, 256], BF16, 31744)
            ke = P.ar("ke", [128, 256], BF16, 32256)
            kd = P.ar("kd", [128, 256], BF16, 32768)
            qkT = P.ar("qkT", [64, 8, 128], BF16, 33280)
            ATm = P.ar("ATm", [128, 4, 128], BF16, 35328)
            yb = P.ar("yb", [128, 512], BF16, 36352)
            Dcol = P.ar("Dcol", [64, 4], F32, 37376)
            oj = P.ar("oj", [128, 128], BF16, 37888)
            wqk = next_wb()
            for bi in range(4):
                ps = proj_tok(wqk, 0, 512, bi)
                P.copy(qk_all[:, bi, :], ps.v, eng="act")
            rel_wb(wqk)
            wgv = next_wb()
            for bi in range(4):
                ps = proj_tok(wgv, 0, 512, bi)
                P.copy(v_allg[:, bi, :], ps.v)
            rel_wb(wgv)
            wgr = next_wb()
            for bi in range(4):
                ps = proj_tok(wgr, 0, 512, bi)
                P.act(r_all[:, bi, :], ps.v, AF.Silu)
            rel_wb(wgr)
            for bi in range(4):
                bs = slice(bi * 128, (bi + 1) * 128)
                qk_sb = qk_all[:, bi, :]
                v_sb = v_allg[:, bi, :]
                r_sb = r_all[:, bi, :]
                psz = P.next_ps()
                P.mm(psz[:, 0:256], gaT[:, bs], w2_sb.v, start=True, stop=False)
                P.mm(psz[:, 0:256], ones1.v, bg_sb.v, start=False, stop=True)
                P.act(e_sb.v, psz[:, 0:256], AF.Exp, scale=-1.0)
                P.act(l_sb.v, e_sb.v, AF.Ln, bias=1.0)
                psc = P.next_ps()
                P.mm(psc[:, 0:256], trif, l_sb.v)
                P.mm(psc[:, 256:512], onesf, l_sb.v)
                psd = P.next_ps()
                for h in range(4):
                    P.mm(psd[0:64, h:h + 1], l_sb[:, h * 64:(h + 1) * 64], onesf[:, 0:1])
                P.act(eb.v, psc[:, 0:256], AF.Exp, scale=-1.0 / 16)
                P.act(ebp.v, psc[:, 0:256], AF.Exp, scale=1.0 / 16)
                P.act(ebl.v, psc[:, 256:512], AF.Exp, scale=-1.0 / 16)
                P.act(Dcol.v, psd[0:64, 0:4], AF.Exp, scale=-1.0 / 16)
                P.stt(qe.v, qk_sb[:, 0:256], 0.125, eb.v, ALU.mult, ALU.mult)
                P.tt(ke.v, qk_sb[:, 256:512], ebp.v, ALU.mult)
                P.tt(tmpk.v, ebp.v, ebl.v, ALU.mult)
                P.tt(kd.v, qk_sb[:, 256:512], tmpk.v, ALU.mult)
                pst = P.next_ps()
                pstb = pst.v.bitcast(BF16)
                for h in range(4):
                    P.tr(pstb[0:64, h * 128:(h + 1) * 128], qe[:, h * 64:(h + 1) * 64], identb.v)
                    P.tr(pstb[0:64, (4 + h) * 128:(5 + h) * 128], ke[:, h * 64:(h + 1) * 64], identb.v)
                P.copy(qkT.v.rearrange("p a b -> p (a b)"), pstb[0:64, :], eng="act")
                psa = P.next_ps()
                for h in range(4):
                    P.mm(psa[:, h * 128:(h + 1) * 128], qkT[:, 4 + h, :], qkT[:, h, :])
                P.tt(ATm.v, psa.v.rearrange("p (h t) -> p h t", h=4),
                     trif.rearrange("p (o t) -> p o t", o=1).to_broadcast([128, 4, 128]), ALU.mult)
                pso = P.next_ps()
                for h in range(4):
                    hs = slice(h * 128, (h + 1) * 128)
                    P.mm(pso[:, hs], ATm[:, h, :], v_sb[:, hs], start=True, stop=False)
                    P.mm(pso[:, hs], qkT[:, h, :], Sb[:, h, :], start=False, stop=True)
                psu = P.next_ps()
                for h in range(4):
                    P.mm(psu[0:64, h * 128:(h + 1) * 128], kd[:, h * 64:(h + 1) * 64], v_sb[:, h * 128:(h + 1) * 128])
                for h in range(4):
                    P.stt(Sf[:, h, :], Sf[:, h, :], Dcol[:, h:h + 1], psu[0:64, h * 128:(h + 1) * 128],
                          ALU.mult, ALU.add)
                P.copy(Sb.v, Sf.v)
                for h in range(4):
                    P.act(oj.v, pso[:, h * 128:(h + 1) * 128], AF.Square, accum=stat[:, 36 + h:37 + h])
                small_rstd(stat[:, 36:40], stat[:, 36:40], 1.0 / 128)
                for h in range(4):
                    hs = slice(h * 128, (h + 1) * 128)
                    P.stt(yb[:, hs], pso[:, hs], stat[:, 36 + h:37 + h], r_sb[:, hs], ALU.mult, ALU.mult)
                pst2 = P.next_ps()
                pst2b = pst2.v.bitcast(BF16)
                for h in range(4):
                    P.tr(pst2b[:, h * 128:(h + 1) * 128], yb[:, h * 128:(h + 1) * 128], identb.v)
                P.act(ysT[:, 1, :, bs], pst2b[:, 0:512].rearrange("p (h t) -> p h t", h=4), AF.Identity,
                      scale=gng_col)

            P.mark(f'g{g}_gla')
            kn = P.ar("kn", [128, 512], BF16, 8192)
            qT = P.ar("qT", [128, 4, 512], BF16, 9216)
            iqT = P.ar("iqT", [64, 4, 512], BF16, 13312)
            iw_sb = P.ar("iw_sb", [128, 4, 4], F32, 17408)
            relu_t = [P.ar("relu0", [128, 512], F32, 17920)]
            PT = [P.ar(f"PT{i}", [128, 512], BF16, 19968 + 1024 * i) for i in range(2)]
            yc = P.ar("yc", [128, 512], BF16, 22016)
            bjunk = P.ar("bjunk", [128, 128], BF16, 23040)
            score = P.ar("score", [128, S], F32, 23552)
            maskbuf = [P.ar(f"maskbuf{i}", [128, S], BF16, 23552 + 4 * S + 2 * S * i) for i in range(2)]
            cD = max(max(1, n_ * 15 // 32) for n_ in range(1, NBLK + 1)) * 128
            cA = max(n_ - max(1, n_ * 15 // 32) for n_ in range(1, NBLK + 1)) * 128
            assert cD + cA <= S
            jDb = [P.ar(f"jD{i}", [128, cD], BF16, 23552 + 4 * S + 2 * S * i) for i in range(2)]
            jAb = [P.ar(f"jA{i}", [128, cA], BF16, 23552 + 4 * S + 2 * S * i + 2 * cD) for i in range(2)]

            def qk_norm_T(ps, gcol, dst):
                for h in range(4):
                    P.act(bjunk.v, ps[:, h * 128:(h + 1) * 128], AF.Square, accum=stat[:, 40 + h:41 + h])
                small_rstd(stat[:, 40:44], stat[:, 40:44], 1.0 / 128)
                for h in range(4):
                    P.ts(kn[:, h * 128:(h + 1) * 128], ps[:, h * 128:(h + 1) * 128], stat[:, 40 + h:41 + h],
                         None, ALU.mult)
                pt = P.next_ps()
                ptb = pt.v.bitcast(BF16)
                for h in range(4):
                    P.tr(ptb[:, h * 128:(h + 1) * 128], kn[:, h * 128:(h + 1) * 128], identb.v)
                P.act(dst, ptb[:, 0:512].rearrange("p (h t) -> p h t", h=4), AF.Identity, scale=gcol)

            for bi in range(4):
                gb = g * 4 + bi
                bs = slice(bi * 128, (bi + 1) * 128)
                psw = P.next_ps()
                for k in range(8):
                    P.mm(psw[:, 0:4], hT[:, k, bs], wsm[:, k, WSM_IW:WSM_IW + 4], start=(k == 0), stop=(k == 7))
                P.copy(iw_sb[:, bi, :], psw[:, 0:4])
            ps = proj_feat(wsm, WSM_IK, 64)
            P.copy(ikT_all[:, g * 512:(g + 1) * 512], ps[0:64, :], eng="act")
            for h in range(4):
                ps = proj_feat(wsm, WSM_IQ + h * 64, 64)
                P.copy(iqT[:, h, :], ps[0:64, :], eng="act")


            def IB_gen(bi):
                gb = g * 4 + bi
                bs = slice(bi * 128, (bi + 1) * 128)
                nk = (gb + 1) * 128
                nch = (nk + 511) // 512
                mk = maskbuf[bi % 2]
                for c in range(nch):
                    c0, c1 = c * 512, min((c + 1) * 512, nk)
                    w = c1 - c0
                    for h in range(4):
                        psi = P.next_ps()
                        P.mm(psi[:, 0:w], iqT[:, h, bs], ikT_all[:, c0:c1])
                        rl = relu_t[0]
                        P.act(rl[:, 0:w], psi[:, 0:w], AF.Relu)
                        if h == 0:
                            P.ts(score[:, c0:c1], rl[:, 0:w], iw_sb[:, bi, 0:1], None, ALU.mult)
                        else:
                            P.stt(score[:, c0:c1], rl[:, 0:w], iw_sb[:, bi, h:h + 1], score[:, c0:c1],
                                  ALU.mult, ALU.add)
                    yield
                P.tt(score[:, gb * 128:nk], score[:, gb * 128:nk], negtri, ALU.add)
                tau = stat[:, 44:45]
                if gb < 2:
                    P.memset(tau, -1.0e29)
                else:
                    rng = stat[:, 45:46]
                    mid = stat[:, 46:47]
                    cnt = stat[:, 47:48]
                    u = stat[:, 48:49]
                    hi = stat[:, 49:50]
                    lo = stat[:, 50:51]
                    P.ts(mk[:, 0:nk], score[:, 0:nk], 0.0, -3.0e38, ALU.add, ALU.max, accum=hi)
                    P.ts(mk[:, 0:256], score[:, 0:256], 0.0, 3.0e38, ALU.add, ALU.min, accum=lo)
                    P.tt(rng, hi, lo, ALU.subtract)
                    P.ts(offs.v, fv.v, rng, None, ALU.mult)
                    P.tt(mid, lo, offs[:, 0:1], ALU.add)
                    for it in range(NBIS):
                        P.ts(mk[:, 0:nk], score[:, 0:nk], mid, 0.0, ALU.is_ge, ALU.add, accum=cnt)
                        last = (it == NBIS - 1)
                        P.ts(u, cnt, 256.0, 1.0 if last else 0.5, ALU.is_ge, ALU.subtract)
                        P.stt(tau if last else mid, u, offs[:, it:it + 1], mid, ALU.mult, ALU.add)
                        yield
                P.ts(mk[:, 0:nk], score[:, 0:nk], tau, NEG, ALU.is_lt, ALU.mult)
                yield

            def drain(gen):
                for _ in gen:
                    pass

            def interleave(ga, gb_):
                a_done = b_done = False
                while not (a_done and b_done):
                    if not a_done:
                        try:
                            next(ga)
                        except StopIteration:
                            a_done = True
                    if not b_done:
                        try:
                            next(gb_)
                        except StopIteration:
                            b_done = True

            def ATT_gen(bi):
                gb = g * 4 + bi
                bs = slice(bi * 128, (bi + 1) * 128)
                mk = maskbuf[bi % 2]
                P.memset(psA.v, 0.0)
                P.memset(psB.v, 0.0)
                def LG(j):
                    psl = P.next_ps()
                    dist = gb - j
                    P.mm(psl.v, mk[:, j * 128:(j + 1) * 128], ident4.v, start=True, stop=False)
                    if dist <= 1:
                        P.mm(psl.v, identb.v, biasT[:, dist, :, :].rearrange("p h t -> p (h t)"),
                             start=False, stop=False)
                    for h in range(4):
                        hs = slice(h * 128, (h + 1) * 128)
                        P.mm(psl[:, hs], kT_all[:, h, j * 128:(j + 1) * 128], qT[:, h, bs],
                             start=False, stop=(h == 3))
                    return psl

                psl = LG(0)
                P.act(PT[0].v, psl.v, AF.Exp, bias=-6.0)
                for j in range(gb + 1):
                    pt = PT[j % 2]
                    if j + 1 <= gb:
                        psl_next = LG(j + 1)
                    yield
                    for h in range(4):
                        acc = psA if h < 2 else psB
                        a0 = (h % 2) * 130
                        P.mm(acc[:, a0:a0 + 129], pt[:, h * 128:(h + 1) * 128], v_all[:, j, h, 0:129],
                             start=False, stop=False, skip=True)
                    if j + 1 <= gb:
                        P.act(PT[(j + 1) % 2].v, psl_next.v, AF.Exp, bias=-6.0)
                for h in range(4):
                    acc = psA if h < 2 else psB
                    a0 = (h % 2) * 130
                    P.recip(stat[:, 52 + h:53 + h], acc[:, a0 + 128:a0 + 129])
                    P.ts(yc[:, h * 128:(h + 1) * 128], acc[:, a0:a0 + 128], stat[:, 52 + h:53 + h], None, ALU.mult)
                pt2 = P.next_ps()
                pt2b = pt2.v.bitcast(BF16)
                for h in range(4):
                    P.tr(pt2b[:, h * 128:(h + 1) * 128], yc[:, h * 128:(h + 1) * 128], identb.v)
                P.copy(ysT[:, 2, :, bs], pt2b[:, 0:512].rearrange("p (h t) -> p h t", h=4), eng="act")

            def projA_gen():
                wdq = next_wb()
                for bi in range(4):
                    bs = slice(bi * 128, (bi + 1) * 128)
                    ps = proj_tok(wdq, 0, 512, bi)
                    qk_norm_T(ps, gq_col, qT[:, :, bs])
                    yield
                rel_wb(wdq)
                wdk = next_wb()
                for bi in range(4):
                    gb = g * 4 + bi
                    ps = proj_tok(wdk, 0, 512, bi)
                    qk_norm_T(ps, gk_col, kT_all[:, :, gb * 128:(gb + 1) * 128])
                    yield
                rel_wb(wdk)
                wdv = next_wb()
                for bi in range(4):
                    gb = g * 4 + bi
                    ps = proj_tok(wdv, 0, 512, bi)
                    P.copy(v_all[:, gb, :, 0:128], ps.v.rearrange("p (h e) -> p h e", h=4))
                    yield
                rel_wb(wdv)

            interleave(projA_gen(), IB_gen(0))
            if l + 1 < L:
                chs = cast_chunks(l + 1)
                per = (len(chs) + NG - 1) // NG
                for dst_, src_ in chs[g * per:(g + 1) * per]:
                    P.dma("pool", dst_, src_)
            P.mark(f'g{g}_dsaproj')
            for bi in range(4):
                if bi + 1 < 4:
                    interleave(ATT_gen(bi), IB_gen(bi + 1))
                else:
                    drain(ATT_gen(bi))

            P.mark(f'g{g}_dsa')
            dump("ysT", ysT.v, [128, 3, 4, 512], BF16)
            dump("kT", kT_all[:, :, 0:512], [128, 4, 512], BF16)
            gtb = gt_load(0, 40960)
            mergedF = P.ar("mergedF", [128, 8, 512], F32, 8192)
            mergedT = P.ar("mergedT", [128, 8, 512], BF16, 24576)
            sgt = [P.ar(f"sgt{i}", [128, 512], BF16, 32768 + 1024 * i) for i in range(2)]
            mtmp = [P.ar(f"mtmp{i}", [128, 512], F32, 34816 + 2048 * i) for i in range(2)]
            ci = 0
            for n in range(3):
                wbr = next_wb()
                wbrv = wbr.v.rearrange("p a b -> p (a b)").rearrange("p (e d) -> p e d", e=4)
                for half in range(2):
                    wg = next_wb()
                    for cc in range(4):
                        dc = half * 4 + cc
                        psg = proj_feat(wg, cc * 128, 128)
                        sg = sgt[ci % 2]
                        P.act(sg.v, psg.v, AF.Sigmoid, bias=prm_sb[:, PR_BBG + n * 8 + dc:PR_BBG + n * 8 + dc + 1])
                        psu = P.next_ps()
                        for ec in range(4):
                            P.mm(psu.v, wbrv[:, ec, dc * 128:(dc + 1) * 128], ysT[:, n, ec, :],
                                 start=(ec == 0), stop=(ec == 3))
                        if n == 0:
                            P.tt(mergedF[:, dc, :], psu.v, sg.v, ALU.mult)
                        else:
                            mt = mtmp[ci % 2]
                            P.tt(mt.v, psu.v, sg.v, ALU.mult)
                            if n == 1:
                                P.tt(mergedF[:, dc, :], mergedF[:, dc, :], mt.v, ALU.add)
                            else:
                                P.tt(mergedT[:, dc, :], mergedF[:, dc, :], mt.v, ALU.add)
                        ci += 1
                    rel_wb(wg)
                rel_wb(wbr)
            P.mark(f'g{g}_merge')
            for half in range(2):
                wo = next_wb()
                for bi in range(4):
                    ps = P.next_ps()
                    for k in range(8):
                        P.mm(ps.v, mergedT[:, k, bi * 128:(bi + 1) * 128], wo[:, k, :], start=(k == 0), stop=(k == 7))
                    mt = mtmp[bi % 2]
                    P.tt(mt.v, ps.v, gtb[:, half * 512:(half + 1) * 512], ALU.mult)
                    P.tt(xg[:, bi, half * 512:(half + 1) * 512], xg[:, bi, half * 512:(half + 1) * 512], mt.v,
                         ALU.add)
                rel_wb(wo)

            dump("mergedT", mergedT.v, [128, 8, 512], BF16)
            dump("x1", xg.v, [128, 4, D], F32)
            P.mark(f'g{g}_wout')
            gtb = gt_load(1, 16384)
            norm_to_hT(sT[:, 8:16], modT[:, 24:32], True)
            dump("h2T", hT.v, [128, 8, 512], BF16)
            dump("combb", combb.v, [128, 4, 16], BF16)
            P.mark(f'g{g}_norm2')
            cb_sb = P.ar("cb_sb", [128, 512], BF16, 0)
            t1 = [P.ar("t1", [128, 512], BF16, 1024)]
            sgm = [P.ar(f"sgm{i}", [128, 512], BF16, 2048 + 1024 * i) for i in range(2)]
            actT = P.ar("actT", [128, 4, 2, 512], BF16, 4096)
            mtmp = [P.ar(f"mtmpb{i}", [128, 512], F32, 12288 + 2048 * i) for i in range(2)]
            WD = [P.ar(f"wd{i}", [128, 2, D], BF16, 21504 + 4096 * i) for i in range(6)]
            wdi = 0
            for rnd in range(4):
                wds = []
                for ee in range(4):
                    e = rnd * 4 + ee
                    wgu = next_wb()
                    wd = WD[wdi % 6]
                    wdi += 1
                    P.dma(wsrc(l)[0], wd.v, wsrc(l)[6][e].rearrange("(f p) d -> p f d", p=128))
                    wds.append(wd)
                    psc = P.next_ps()
                    for bi in range(4):
                        P.mm(psc[:, bi * 128:(bi + 1) * 128], combb[:, bi, e:e + 1].to_broadcast([128, 128]),
                             identb.v)
                    P.copy(cb_sb.v, psc.v, eng="act")
                    for fc in range(2):
                        psg = proj_feat(wgu, fc * 128, 128)
                        psu = proj_feat(wgu, 256 + fc * 128, 128)
                        sg = sgm[fc]
                        P.act(sg.v, psg.v, AF.Silu)
                        P.tt(t1[0].v, psu.v, sg.v, ALU.mult)
                        P.tt(actT[:, ee, fc, :], t1[0].v, cb_sb.v, ALU.mult)
                    rel_wb(wgu)
                for bi in range(4):
                    for half in range(2):
                        ps = P.next_ps()
                        n = 0
                        for ee in range(4):
                            for fc in range(2):
                                P.mm(ps.v, actT[:, ee, fc, bi * 128:(bi + 1) * 128],
                                     wds[ee][:, fc, half * 512:(half + 1) * 512], start=(n == 0), stop=(n == 7))
                                n += 1
                        mt = mtmp[(bi * 2 + half) % 2]
                        P.tt(mt.v, ps.v, gtb[:, half * 512:(half + 1) * 512], ALU.mult)
                        P.tt(xg[:, bi, half * 512:(half + 1) * 512], xg[:, bi, half * 512:(half + 1) * 512], mt.v,
                             ALU.add)
            P.mark(f'g{g}_moe')
            for bi in range(4):
                gb = g * 4 + bi
                P.dma("sp", out.sub(gb)[gb * 128:(gb + 1) * 128, :], xg[:, bi, :])
    P.fence("sp", [out] + list(dbgo.values()))
    stats = P.emit()
    stats['marks'] = getattr(P, 'marks', [])
    return nc, stats


def _bucket_np(rel):
    import math
    n = np.maximum(rel, 0)
    max_exact = 16
    large = max_exact + (np.log(np.maximum(n, max_exact).astype(np.float32) / max_exact)
                         / math.log(128 / max_exact) * (32 - max_exact)).astype(np.int32)
    large = np.minimum(large, 31)
    return np.where(n < max_exact, n, large)


def host_consts():
    cst = np.zeros((128, NCST), np.float32)
    i = np.arange(128)
    cst[:, C_ID:C_ID + 128] = np.eye(128, dtype=np.float32)
    cst[:, C_TRI:C_TRI + 128] = (i[:, None] <= i[None, :]).astype(np.float32)
    cst[:, C_ONES:C_ONES + 128] = 1.0
    cst[:, C_NEGTRI:C_NEGTRI + 128] = np.where(i[None, :] <= i[:, None], 0.0, BIGNEG)
    cst[:, C_BKD:C_BKD + 128] = _bucket_np(i[None, :] - i[:, None]).astype(np.float32)
    cst[:, C_BKN:C_BKN + 128] = _bucket_np(128 + i[None, :] - i[:, None]).astype(np.float32)
    return cst


def host_layout(inputs, L, b, S):
    f = lambda a: np.ascontiguousarray(np.asarray(a, dtype=np.float32))
    col = lambda v: np.asarray(v, np.float32).reshape(-1, 128).T
    m = {}
    m["x"] = f(inputs["x"][b, :S])
    m["ccol"] = f(col(inputs["c"][b]))
    m["cst"] = host_consts()
    m["rbrow"] = f(np.asarray(inputs["rel_bias"]).reshape(1, 128))
    prm = np.zeros((L, 128, NPRM), np.float32)
    prow = np.zeros((L, 1, NROW), np.float32)
    for l in range(L):
        prm[l, :, PR_G1:PR_G1 + 8] = col(inputs["g_norm1"][l])
        prm[l, :, PR_G2:PR_G2 + 8] = col(inputs["g_norm2"][l])
        prm[l, :, PR_BMOD:PR_BMOD + 48] = col(inputs["b_mod"][l])
        prm[l, :, PR_LNG:PR_LNG + 4] = col(inputs["gmlp_ln_g"][l])
        prm[l, :, PR_LNB:PR_LNB + 4] = col(inputs["gmlp_ln_b"][l])
        prm[l, :, PR_GNG] = np.asarray(inputs["gla_norm_g"][l])
        prm[l, :, PR_GQ] = np.asarray(inputs["dsa_qnorm_g"][l])
        prm[l, :, PR_GK] = np.asarray(inputs["dsa_knorm_g"][l])
        prm[l, :, PR_BBG:PR_BBG + 24] = col(np.asarray(inputs["b_branch_gate"][l]).reshape(-1))
        prow[l, 0, RW_BS:RW_BS + 512] = np.asarray(inputs["gmlp_b_s"][l]).reshape(-1)
        prow[l, 0, RW_BG:RW_BG + 256] = np.asarray(inputs["gla_b_gate"][l])
        prow[l, 0, RW_BR:RW_BR + 4] = np.asarray(inputs["b_group"][l])
        prow[l, 0, RW_BR + 4:RW_BR + 20] = np.asarray(inputs["b_router"][l])
    m["prm"] = prm
    m["prow"] = prow
    m["w_mod"] = f(inputs["w_mod"][:L])
    m["w_in"] = f(inputs["w_in"][:L])
    m["wsT"] = f(np.asarray(inputs["gmlp_w_s"][:L]).transpose(0, 3, 1, 2))
    m["w2"] = f(inputs["gla_w_gate2"][:L])
    m["w_branch"] = f(inputs["w_branch"][:L])
    m["w_out"] = f(inputs["w_out"][:L])
    m["wr"] = f(np.concatenate([np.asarray(inputs["w_group"][:L]), np.asarray(inputs["w_router"][:L])], axis=2))
    m["w_exp_gate"] = f(inputs["w_exp_gate"][:L])
    m["w_exp_up"] = f(inputs["w_exp_up"][:L])
    m["w_exp_down"] = f(inputs["w_exp_down"][:L])
    return m


_PROG_CACHE = {}


def run_model(inputs, L, S, batches, n_cores, want_all=False, active=None):
    key = (L, S)
    if key not in _PROG_CACHE:
        _PROG_CACHE[key] = build_program(L, S)
    nc, stats = _PROG_CACHE[key]
    if active is None:
        active = list(range(min(n_cores, len(batches))))
    shared = host_layout(inputs, L, batches[0], S)
    zeros = None
    in_maps = []
    for ci in range(n_cores):
        if ci in active:
            b = batches[active.index(ci)]
            m = dict(shared)
            m["x"] = np.ascontiguousarray(np.asarray(inputs["x"][b, :S], dtype=np.float32))
            m["ccol"] = np.ascontiguousarray(np.asarray(inputs["c"][b], np.float32).reshape(-1, 128).T)
        else:
            if zeros is None:
                zeros = {k: np.zeros_like(v) for k, v in shared.items()}
            m = dict(zeros)
        in_maps.append(m)
    res = run_bass_kernel_spmd(nc, in_maps, core_ids=list(range(n_cores)))
    if want_all:
        return res.results
    return [res.results[ci]["out"] for ci in active]


def kernel(**inputs):
    L, B, S = 4, 4, 4096
    outs = run_model(inputs, L, S, list(range(B)), 8, active=[0, 1, 4, 5])
    return np.stack(outs, axis=0).astype(np.float32)
```

```python
import numpy as np
import concourse.bass as bass
import concourse.mybir as mybir

F32 = mybir.dt.float32
BF16 = mybir.dt.bfloat16
AF = mybir.ActivationFunctionType
ALU = mybir.AluOpType
AX = mybir.AxisListType

GRAN = 512
SEM_CAP = 30000
STRICT_SAME_ENGINE = True


class Region:
    __slots__ = ("lw", "rd")

    def __init__(self):
        self.lw = None
        self.rd = {}


class View:
    __slots__ = ("buf", "ap")

    def __init__(self, buf, ap):
        self.buf = buf
        self.ap = ap

    def __getitem__(self, idx):
        return View(self.buf, self.ap[idx])

    def rearrange(self, s, **kw):
        return View(self.buf, self.ap.rearrange(s, **kw))

    def bitcast(self, dt):
        return View(self.buf, self.ap.bitcast(dt))

    def to_broadcast(self, shape):
        return View(self.buf, self.ap.to_broadcast(shape))

    def partition_broadcast(self, n):
        return View(self.buf, self.ap.partition_broadcast(n))


class Buf:
    def __init__(self, name, h, regions, is_dram=False):
        self.name = name
        self.h = h
        self.regions = regions
        self.is_dram = is_dram

    def __getitem__(self, idx):
        base = self.h.ap() if self.is_dram else self.h
        return View(self, base[idx])

    @property
    def v(self):
        base = self.h.ap() if self.is_dram else self.h[:]
        return View(self, base)

    def sub(self, i, n=1):
        return Buf(self.name, self.h, self.regions[i:i + n], self.is_dram)


class Op:
    __slots__ = ("eng", "fn", "kind", "deps", "lidx", "flag", "waits", "clock",
                 "fnum", "dma_sem", "dma_val", "dq_n")


class Prog:
    ENGS = ("pe", "act", "dve", "pool", "sp")

    def __init__(self, nc, n_dma_sems=None):
        self.nc = nc
        self.h = {"pe": nc.tensor, "act": nc.scalar, "dve": nc.vector,
                  "pool": nc.gpsimd, "sp": nc.sync}
        self.ops = []
        self.n_dma_sems = n_dma_sems or {"sp": 16, "pool": 16, "act": 4}
        self.dma_count = {e: 0 for e in self.ENGS}
        self.dma_ids = {e: [] for e in self.ENGS}
        self.arena_base = None
        self.arena_regions = None
        self.psum_rot = []
        self.psum_i = 0

    def sb(self, name, shape, dtype):
        h = self.nc.alloc_sbuf_tensor(name, list(shape), dtype)
        return Buf(name, h, [Region()])

    def make_arena(self, nbytes):
        r = self.nc.bump_sbuf(nbytes)
        self.arena_base = r[0]
        self.arena_size = nbytes
        self.arena_regions = [Region() for _ in range((nbytes + GRAN - 1) // GRAN)]

    def ar(self, name, shape, dtype, off):
        key = (name, tuple(shape), str(dtype), off)
        if not hasattr(self, "_arc"):
            self._arc = {}
        if key in self._arc:
            return self._arc[key]
        b = self._ar(f"{name}_{len(self._arc)}", shape, dtype, off)
        self._arc[key] = b
        return b

    def _ar(self, name, shape, dtype, off):
        esz = {F32: 4, BF16: 2}[dtype]
        n = int(np.prod(shape[1:])) * esz
        assert off % 32 == 0 and off + n <= self.arena_size, (name, off, n, self.arena_size)
        h = self.nc.alloc_sbuf_tensor_at(name, list(shape), dtype, offset=self.arena_base + off)
        regs = self.arena_regions[off // GRAN:(off + n + GRAN - 1) // GRAN]
        return Buf(name, h, regs)

    def ps(self, name, shape=(128, 512), dtype=F32):
        h = self.nc.alloc_psum_tensor(name, list(shape), dtype)
        return Buf(name, h, [Region()])

    def dram(self, name, shape, dtype, kind="Internal", nreg=1):
        h = self.nc.dram_tensor(name, list(shape), dtype, kind=kind)
        return Buf(name, h, [Region() for _ in range(nreg)], is_dram=True)

    def fence(self, eng, bufs):
        h = self.h[eng]
        self.add(eng, lambda: h.nop(), bufs, [])

    def mark(self, name):
        if not getattr(self, "marks_on", False):
            return
        last = {}
        for oid in range(len(self.ops) - 1, -1, -1):
            o = self.ops[oid]
            if o.kind == "c" and o.eng != "sp" and o.eng not in last:
                last[o.eng] = oid
            if len(last) == 4:
                break
        h = self.h["sp"]
        oid = self.add("sp", lambda: h.nop(), [], [])
        for e, d in last.items():
            self.ops[oid].deps[d] = True
        if not hasattr(self, "marks"):
            self.marks = []
        self.marks.append(name)

    def next_ps(self):
        b = self.psum_rot[self.psum_i % len(self.psum_rot)]
        self.psum_i += 1
        return b

    def add(self, eng, fn, reads, writes, kind="c"):
        op = Op()
        op.eng = eng
        op.fn = fn
        op.kind = kind
        op.flag = False
        op.deps = {}
        oid = len(self.ops)
        rregs = []
        for b in reads:
            if b is None:
                continue
            b = b.buf if isinstance(b, View) else b
            rregs.extend(b.regions)
        wregs = []
        for b in writes:
            b = b.buf if isinstance(b, View) else b
            wregs.extend(b.regions)
        for r in rregs:
            if r.lw is not None:
                op.deps[r.lw] = True
        for r in wregs:
            if r.lw is not None:
                op.deps.setdefault(r.lw, False)
            for rid in r.rd.values():
                op.deps.setdefault(rid, False)
        op.deps.pop(oid, None)
        for r in rregs:
            key = eng if kind == "c" else ("dma", oid)
            r.rd[key] = oid
        for r in wregs:
            r.lw = oid
            r.rd = {}
        if kind == "dma":
            n = self.dma_count[eng]
            self.dma_count[eng] += 1
            op.dq_n = n
            self.dma_ids[eng].append(oid)
        self.ops.append(op)
        return oid

    @staticmethod
    def _a(x):
        return x.ap if isinstance(x, View) else x

    def mm(self, out, lhsT, rhs, start=True, stop=True, skip=False):
        nc = self.nc
        if skip:
            self.add("pe", lambda: nc.tensor.matmul(out.ap, lhsT.ap, rhs.ap, start=start, stop=stop,
                                                    skip_group_check=True), [lhsT, rhs], [out])
        else:
            self.add("pe", lambda: nc.tensor.matmul(out.ap, lhsT.ap, rhs.ap, start=start, stop=stop),
                     [lhsT, rhs], [out])

    def tr(self, out, in_, ident):
        nc = self.nc
        self.add("pe", lambda: nc.tensor.transpose(out.ap, in_.ap, ident.ap), [in_, ident], [out])

    def act(self, out, in_, func, bias=None, scale=1.0, accum=None):
        nc = self.nc
        a = self._a
        kw = {}
        if bias is not None:
            kw["bias"] = a(bias)
        if accum is not None:
            kw["accum_out"] = a(accum)
        rd = [in_] + [x for x in (bias, scale) if isinstance(x, View)]
        wr = [out] + ([accum] if accum is not None else [])
        self.add("act", lambda: nc.scalar.activation(out=out.ap, in_=in_.ap, func=func,
                                                      scale=a(scale), **kw), rd, wr)

    def _veng(self, eng):
        return self.nc.vector if eng == "dve" else self.nc.gpsimd

    def tt(self, out, in0, in1, op, eng="dve"):
        e = self._veng(eng)
        self.add(eng, lambda: e.tensor_tensor(out=out.ap, in0=in0.ap, in1=in1.ap, op=op),
                 [in0, in1], [out])

    def ts(self, out, in0, s1, s2, op0, op1=None, accum=None, eng="dve"):
        e = self._veng(eng)
        a = self._a
        kw = {}
        if op1 is not None:
            kw["op1"] = op1
        if accum is not None:
            kw["accum_out"] = a(accum)
        rd = [in0] + [x for x in (s1, s2) if isinstance(x, View)]
        wr = [out] + ([accum] if accum is not None else [])
        self.add(eng, lambda: e.tensor_scalar(out=out.ap, in0=in0.ap, scalar1=a(s1), scalar2=a(s2),
                                              op0=op0, **kw), rd, wr)

    def stt(self, out, in0, scalar, in1, op0, op1, eng="dve"):
        e = self._veng(eng)
        a = self._a
        rd = [in0, in1] + ([scalar] if isinstance(scalar, View) else [])
        self.add(eng, lambda: e.scalar_tensor_tensor(out=out.ap, in0=in0.ap, scalar=a(scalar),
                                                     in1=in1.ap, op0=op0, op1=op1), rd, [out])

    def copy(self, out, in_, eng="dve"):
        if eng == "act":
            nc = self.nc
            self.add("act", lambda: nc.scalar.copy(out=out.ap, in_=in_.ap), [in_], [out])
        else:
            e = self._veng(eng)
            self.add(eng, lambda: e.tensor_copy(out=out.ap, in_=in_.ap), [in_], [out])

    def memset(self, out, val, eng="dve"):
        e = self._veng(eng)
        self.add(eng, lambda: e.memset(out.ap, val), [], [out])

    def reduce(self, out, in_, op, eng="dve"):
        e = self._veng(eng)
        self.add(eng, lambda: e.tensor_reduce(out=out.ap, in_=in_.ap, axis=AX.X, op=op), [in_], [out])

    def recip(self, out, in_):
        nc = self.nc
        self.add("dve", lambda: nc.vector.reciprocal(out=out.ap, in_=in_.ap), [in_], [out])

    def dma(self, q, out, in_, **kw):
        h = self.h[q]
        self.add(q, lambda: h.dma_start(out=out.ap, in_=in_.ap, **kw), [in_], [out], kind="dma")

    def emit(self):
        nc = self.nc
        ops = self.ops
        known = {e: {} for e in self.ENGS}
        dma_known = {e: set() for e in self.ENGS}
        count = {e: 0 for e in self.ENGS}
        for oid, op in enumerate(ops):
            E = op.eng
            waits = []
            kn = known[E]
            if op.kind == "dma":
                ns = self.n_dma_sems[E]
                if op.dq_n >= ns:
                    prev = self.dma_ids[E][op.dq_n - ns]
                    if prev not in dma_known[E]:
                        waits.append(prev)
                        dma_known[E].add(prev)
            for d, raw in op.deps.items():
                p = ops[d]
                if p.kind == "dma":
                    if d in dma_known[E]:
                        continue
                    waits.append(d)
                    dma_known[E].add(d)
                else:
                    if p.eng == E and (E == "pe" or (not raw and not STRICT_SAME_ENGINE)):
                        continue
                    if kn.get(p.eng, 0) >= p.lidx:
                        continue
                    waits.append(d)
                    p.flag = True
                    for e2, v in p.clock.items():
                        if kn.get(e2, 0) < v:
                            kn[e2] = v
                    if kn.get(p.eng, 0) < p.lidx:
                        kn[p.eng] = p.lidx
            op.waits = waits
            count[E] += 1
            op.lidx = count[E]
            op.clock = dict(kn) if op.kind == "c" else None
        fcount = {e: 0 for e in self.ENGS}
        for op in ops:
            if op.kind == "c" and op.flag:
                fcount[op.eng] += 1
                op.fnum = fcount[op.eng]
        sems = {}
        for e in self.ENGS:
            n = (fcount[e] + SEM_CAP - 1) // SEM_CAP
            sems[e] = [nc.alloc_semaphore(f"s_{e}_{i}") for i in range(n)]
        dsems = {}
        for e in self.ENGS:
            if self.dma_count[e]:
                dsems[e] = [nc.alloc_semaphore(f"d_{e}_{i}")
                            for i in range(min(self.n_dma_sems[e], self.dma_count[e]))]
        for op in ops:
            if op.kind == "dma":
                ns = self.n_dma_sems[op.eng]
                op.dma_sem = dsems[op.eng][op.dq_n % ns]
                op.dma_val = 16 * (op.dq_n // ns + 1)
        nwaits = 0
        for op in ops:
            h = self.h[op.eng]
            for d in op.waits:
                p = ops[d]
                if p.kind == "dma":
                    h.wait_ge(p.dma_sem, p.dma_val)
                else:
                    n = p.fnum - 1
                    h.wait_ge(sems[p.eng][n // SEM_CAP], n % SEM_CAP + 1)
                nwaits += 1
            ins = op.fn()
            if op.kind == "dma":
                ins.then_inc(op.dma_sem, 16)
            elif op.flag:
                n = op.fnum - 1
                ins.then_inc(sems[op.eng][n // SEM_CAP], 1)
        self.stats = dict(n_ops=len(ops), n_waits=nwaits, flagged=dict(fcount),
                          per_eng=dict(count))
        return self.stats

    def final_wait(self, eng, opids):
        pass


from concourse.bass_utils import run_bass_kernel_spmd

D = 1024
EPS = 1e-6
N_IN = 7508
OFF = dict(u=0, v=512, gq=1024, gk=1280, gv=1536, gr=2048, ga=2560, dq=2576, dk=3088,
           dv=3600, diq=4112, dik=4368, diw=4432, gates=4436)
NEG = -30000.0
BIGNEG = -1.0e30
NBIS = 12
C_ID, C_TRI, C_ONES, C_NEGTRI, C_BKD, C_BKN, NCST = 0, 128, 256, 384, 512, 640, 768
PR_G1, PR_G2, PR_BMOD, PR_LNG, PR_LNB, PR_GNG, PR_GQ, PR_GK, PR_BBG, NPRM = 0, 8, 16, 64, 68, 72, 73, 74, 75, 99
RW_BS, RW_BG, RW_BR, NROW = 0, 512, 768, 788
WSM_GA, WSM_IQ, WSM_IK, WSM_IW, WSM_N = 0, 16, 272, 336, 340


def build_program(L, S, dbg=False, marks=False):
    NBLK = S // 128
    NG = S // 512
    nc = bass.Bass("TRN2", target_bir_lowering=False)
    P = Prog(nc)
    P.marks_on = marks
    EI = "ExternalInput"
    x_in = P.dram("x", [S, D], F32, kind=EI)
    ccol = P.dram("ccol", [128, 8], F32, kind=EI)
    cst = P.dram("cst", [128, NCST], F32, kind=EI)
    rbd = P.dram("rbrow", [1, 128], F32, kind=EI)
    w_mod = P.dram("w_mod", [L, D, 6 * D], F32, kind=EI)
    prm = P.dram("prm", [L, 128, NPRM], F32, kind=EI)
    prow = P.dram("prow", [L, 1, NROW], F32, kind=EI)
    w_in = P.dram("w_in", [L, D, N_IN], F32, kind=EI)
    wsT = P.dram("wsT", [L, 128, 4, 128], F32, kind=EI)
    w2 = P.dram("w2", [L, 16, 256], F32, kind=EI)
    w_br = P.dram("w_branch", [L, 3, 512, D], F32, kind=EI)
    w_out = P.dram("w_out", [L, D, D], F32, kind=EI)
    wr = P.dram("wr", [L, D, 20], F32, kind=EI)
    weg = P.dram("w_exp_gate", [L, 16, D, 256], F32, kind=EI)
    weu = P.dram("w_exp_up", [L, 16, D, 256], F32, kind=EI)
    wed = P.dram("w_exp_down", [L, 16, 256, D], F32, kind=EI)
    out = P.dram("out", [S, D], F32, kind="ExternalOutput", nreg=NBLK)
    gt_dram = P.dram("gt_bc", [2, 128, D], F32, nreg=2)
    w_in_b = P.dram("w_in_b", [2, D, N_IN], BF16, nreg=2)
    w_br_b = P.dram("w_br_b", [2, 3, 512, D], BF16, nreg=2)
    w_out_b = P.dram("w_out_b", [2, D, D], BF16, nreg=2)
    weg_b = P.dram("weg_b", [2, 16, D, 256], BF16, nreg=2)
    weu_b = P.dram("weu_b", [2, 16, D, 256], BF16, nreg=2)
    wed_b = P.dram("wed_b", [2, 16, 256, D], BF16, nreg=2)

    def wsrc(l):
        if l == 0:
            return ("pool", w_in[0], w_br[0], w_out[0], weg[0], weu[0], wed[0])
        p_ = l % 2
        return ("sp", w_in_b.sub(p_)[p_], w_br_b.sub(p_)[p_], w_out_b.sub(p_)[p_],
                weg_b.sub(p_)[p_], weu_b.sub(p_)[p_], wed_b.sub(p_)[p_])

    def cast_chunks(l):
        _, di, db, do, dg, du, dd = wsrc(l)
        ch = []
        for k in range(8):
            ch.append((di[k * 128:(k + 1) * 128, :], w_in[l][k * 128:(k + 1) * 128, :]))
        for n in range(3):
            for e in range(4):
                ch.append((db[n][e * 128:(e + 1) * 128, :], w_br[l][n][e * 128:(e + 1) * 128, :]))
        for k in range(8):
            ch.append((do[k * 128:(k + 1) * 128, :], w_out[l][k * 128:(k + 1) * 128, :]))
        for e in range(16):
            ch.append((dg[e].rearrange("(k p) n -> p k n", p=128), weg[l][e].rearrange("(k p) n -> p k n", p=128)))
            ch.append((du[e].rearrange("(k p) n -> p k n", p=128), weu[l][e].rearrange("(k p) n -> p k n", p=128)))
            ch.append((dd[e].rearrange("(f p) d -> p f d", p=128), wed[l][e].rearrange("(f p) d -> p f d", p=128)))
        return ch
    dbgo = {}

    def dump(name, view, shape, dtype):
        if not dbg or name in dbgo:
            return
        t = P.dram("dbg_" + name, list(shape), dtype, kind="ExternalOutput")
        dbgo[name] = t
        P.dma("sp", t.v, view)

    cf = P.sb("cf", [128, 512], F32)
    ident4 = P.sb("ident4", [128, 512], BF16)
    identb = P.sb("identb", [128, 128], BF16)
    biasT = P.sb("biasT", [128, 2, 4, 128], BF16)
    kT_all = P.sb("kT_all", [128, 4, S], BF16)
    v_all = P.sb("v_all", [128, NBLK, 4, 130], BF16)
    ikT_all = P.sb("ikT_all", [64, S], BF16)
    xg = P.sb("xg", [128, 4, D], F32)
    hT = P.sb("hT", [128, 8, 512], BF16)
    ysT = P.sb("ysT", [128, 3, 4, 512], BF16)
    WB = [P.sb(f"wb{i}", [128, 8, 512], BF16) for i in range(3)]
    wsm = P.sb("wsm", [128, 8, WSM_N], BF16)
    prm_sb = P.sb("prm_sb", [128, NPRM], F32)
    modT = P.sb("modT", [128, 48], F32)
    sT = P.sb("sT", [128, 16], F32)
    WsTm = P.sb("WsTm", [128, 4, 128], BF16)
    Cg = P.sb("Cg", [128, 4, 128], F32)
    w2_sb = P.sb("w2_sb", [16, 256], BF16)
    bg_sb = P.sb("bg_sb", [1, 256], BF16)
    ones1 = P.sb("ones1", [1, 128], BF16)
    wr_sb = P.sb("wr_sb", [128, 8, 20], F32)
    rbias = P.sb("rbias", [128, 20], F32)
    cactT = P.sb("cactT", [128, 8], BF16)
    Sf = P.sb("Sf", [64, 4, 128], F32)
    Sb = P.sb("Sb", [64, 4, 128], BF16)
    stat = P.sb("stat", [128, 64], F32)
    combb = P.sb("combb", [128, 4, 16], BF16)
    fv = P.sb("fv", [128, NBIS], F32)
    offs = P.sb("offs", [128, NBIS], F32)
    PSR = [P.ps(f"psr{i}") for i in range(6)]
    psA = P.ps("psA")
    psB = P.ps("psB")
    P.psum_rot = PSR
    ARENA = max(46 * 1024, 23552 + 8 * S)
    P.make_arena(ARENA)

    identf = cf[:, C_ID:C_ID + 128]
    trif = cf[:, C_TRI:C_TRI + 128]
    onesf = cf[:, C_ONES:C_ONES + 128]
    negtri = cf[:, C_NEGTRI:C_NEGTRI + 128]

    def wload(dst, src):
        P.dma("pool", dst, src)

    class WStream:
        def __init__(self, bufs):
            self.free = list(bufs)
            self.sched = []
            self.issued = []
            self.nissued = 0

        def push(self, fn):
            self.sched.append(fn)

        def _pump(self):
            while self.free and self.nissued < len(self.sched):
                b = self.free.pop(0)
                self.sched[self.nissued](b)
                self.nissued += 1
                self.issued.append(b)

        def get(self):
            self._pump()
            assert self.issued, "weight schedule underflow / no free buffer"
            return self.issued.pop(0)

        def release(self, b):
            self.free.append(b)
            self._pump()

    ws_main = WStream(WB)

    def sched_kxn(src2d, c0, n, q="pool"):
        ws_main.push(lambda wb: P.dma(q, wb[:, :, 0:n],
                                      src2d.rearrange("(k p) n -> p k n", p=128)[:, :, c0:c0 + n]))

    def sched_layer_mod(l):
        for i in range(12):
            sched_kxn(w_mod[l], i * 512, 512)

    def sched_group(l):
        q, s_in, s_br, s_out, s_eg, s_eu, s_ed = wsrc(l)
        for nm in ("u", "v", "gq", "gv", "gr", "dq", "dk", "dv"):
            sched_kxn(s_in, OFF[nm], 512, q)
        for n in range(3):
            ws_main.push(lambda wb, n=n: P.dma(q,
                wb.v.rearrange("p a b -> p (a b)").rearrange("p (e d) -> p e d", e=4),
                s_br[n].rearrange("(e p) d -> p e d", p=128)))
            for half in range(2):
                sched_kxn(s_in, OFF["gates"] + (n * 2 + half) * 512, 512, q)
        for half in range(2):
            sched_kxn(s_out, half * 512, 512, q)
        for e in range(16):
            def f(wb, e=e):
                P.dma(q, wb[:, :, 0:256], s_eg[e].rearrange("(k p) n -> p k n", p=128))
                P.dma(q, wb[:, :, 256:512], s_eu[e].rearrange("(k p) n -> p k n", p=128))
            ws_main.push(f)

    def next_wb():
        return ws_main.get()

    def rel_wb(b):
        ws_main.release(b)

    P.dma("sp", cf.v, cst[:, 0:512])
    bk = P.ar("bk", [128, 256], F32, 2048)
    P.dma("sp", bk.v, cst[:, 512:768])
    for i4 in range(4):
        P.dma("pool", ident4[:, i4 * 128:(i4 + 1) * 128], cst[:, C_ID:C_ID + 128])
    P.dma("pool", identb.v, cst[:, C_ID:C_ID + 128])
    P.memset(ones1.v, 1.0)
    for it in range(NBIS):
        P.memset(fv[:, it:it + 1], 0.5 ** (it + 1))
    P.memset(v_all.v, 1.0)
    rbb = P.ar("rbb", [128, 128], F32, 0)
    bacc = P.ar("bacc", [128, 128], F32, 512)
    btmp = P.ar("btmp", [128, 128], F32, 1024)
    P.dma("sp", rbb.v, rbd.v.partition_broadcast(128))
    for h in range(4):
        for dist, cb in ((0, 0), (1, 128)):
            for b in range(32):
                P.ts(btmp.v, bk[:, cb:cb + 128], float(b), None, ALU.is_equal)
                if b == 0:
                    P.ts(bacc.v, btmp.v, rbb[:, b * 4 + h:b * 4 + h + 1], None, ALU.mult)
                else:
                    P.stt(bacc.v, btmp.v, rbb[:, b * 4 + h:b * 4 + h + 1], bacc.v, ALU.mult, ALU.add)
            P.ts(biasT[:, dist, h, :], bacc.v, rbb[:, 31 * 4 + h:31 * 4 + h + 1], None, ALU.subtract)

    def small_rstd(dst, src, inv_n):
        P.ts(dst, src, inv_n, EPS, ALU.mult, ALU.add)
        P.act(dst, dst, AF.Sqrt)
        P.recip(dst, dst)

    def norm_to_hT(scol, shcol, fp32_router):
        junk = P.ar("junk", [128, D], BF16, 0)
        ssq = stat[:, 0:4]
        rstd = stat[:, 4:8]
        for bi in range(4):
            P.act(junk.v, xg[:, bi, :], AF.Square, accum=stat[:, bi:bi + 1])
        small_rstd(rstd, ssq, 1.0 / D)
        if not fp32_router:
            xns = [P.ar(f"xn{i}", [128, D], BF16, 2048 + 2048 * i) for i in range(2)]
            for bi in range(4):
                xn = xns[bi % 2]
                P.ts(xn.v, xg[:, bi, :], stat[:, 4 + bi:5 + bi], None, ALU.mult)
                ps = P.next_ps()
                psb = ps.v.bitcast(BF16)
                for k in range(8):
                    P.tr(psb[:, k * 128:(k + 1) * 128], xn[:, k * 128:(k + 1) * 128], identb.v)
                for k in range(8):
                    o = hT[:, k, bi * 128:(bi + 1) * 128]
                    if k % 2 == 0:
                        P.act(o, psb[:, k * 128:(k + 1) * 128], AF.Identity,
                              bias=shcol[:, k:k + 1], scale=scol[:, k:k + 1])
                    else:
                        P.ts(o, psb[:, k * 128:(k + 1) * 128], scol[:, k:k + 1], shcol[:, k:k + 1],
                             ALU.mult, ALU.add)
        else:
            xn32 = P.ar("xn32", [128, D], F32, 2048)
            h32 = P.ar("h32", [128, 8, 128], F32, 6144)
            lg_all = P.ar("lg_all", [128, 4, 20], F32, 10240)
            for bi in range(4):
                P.ts(xn32.v, xg[:, bi, :], stat[:, 4 + bi:5 + bi], None, ALU.mult)
                pss = [P.next_ps(), P.next_ps()]
                for k in range(8):
                    P.tr(pss[k // 4][:, (k % 4) * 128:(k % 4 + 1) * 128],
                         xn32[:, k * 128:(k + 1) * 128], identf)
                for k in range(8):
                    src = pss[k // 4][:, (k % 4) * 128:(k % 4 + 1) * 128]
                    if k % 2 == 0:
                        P.act(h32[:, k, :], src, AF.Identity, bias=shcol[:, k:k + 1], scale=scol[:, k:k + 1])
                    else:
                        P.ts(h32[:, k, :], src, scol[:, k:k + 1], shcol[:, k:k + 1], ALU.mult, ALU.add)
                P.copy(hT[:, :, bi * 128:(bi + 1) * 128], h32.v)
                psr = P.next_ps()
                for k in range(8):
                    P.mm(psr[:, 0:20], h32[:, k, :], wr_sb[:, k, :], start=(k == 0), stop=(k == 7))
                P.tt(lg_all[:, bi, :], psr[:, 0:20], rbias.v, ALU.add)
            router_all()

    def proj_tok(wb, c0, n, bi):
        ps = P.next_ps()
        for k in range(8):
            P.mm(ps[:, 0:n], hT[:, k, bi * 128:(bi + 1) * 128], wb[:, k, c0:c0 + n],
                 start=(k == 0), stop=(k == 7))
        return ps

    def proj_feat(wb, c0, m):
        ps = P.next_ps()
        for k in range(8):
            P.mm(ps[0:m, :], wb[:, k, c0:c0 + m], hT[:, k, :], start=(k == 0), stop=(k == 7))
        return ps

    def router_all():
        BIG = 1.0e30
        o = [10240 + 320]

        def fld(name, n):
            b_ = P.ar("r_" + name, [128, 4, n], F32, o[0])
            o[0] += ((4 * n * 4 + 31) // 32) * 32
            return b_

        lg_all = P.ar("lg_all", [128, 4, 20], F32, 10240)
        gmax, gsum, pg, m1, m2, dd, e21, w1, w1p, w2p = [fld(n_, 1) for n_ in
                                                         ("gmax", "gsum", "pg", "m1", "m2", "dd", "e21", "w1", "w1p", "w2p")]
        d4, ge, oh, negm = [fld(n_, 4) for n_ in ("d4", "ge", "oh", "negm")]
        elm, oh1, elm2, oh2, cmb, cmb2 = [fld(n_, 16) for n_ in ("elm", "oh1", "elm2", "oh2", "cmb", "cmb2")]
        f2 = lambda t: t.v.rearrange("p b o -> p (b o)")
        bc = lambda t, n: t.v.to_broadcast([128, 4, n])
        lgg = lg_all[:, :, 0:4]
        lge = lg_all[:, :, 4:20]
        P.reduce(f2(gmax), lgg, ALU.max)
        P.tt(d4.v, lgg, bc(gmax, 4), ALU.subtract)
        P.act(ge.v, d4.v, AF.Exp)
        P.reduce(f2(gsum), ge.v, ALU.add)
        P.recip(pg.v, gsum.v)
        P.tt(oh.v, lgg, bc(gmax, 4), ALU.is_equal)
        P.ts(negm.v, oh.v, 1.0, BIG, ALU.subtract, ALU.mult)
        P.tt(elm.v.rearrange("p b (g e) -> p b g e", g=4), lge.rearrange("p b (g e) -> p b g e", g=4),
             negm.v.rearrange("p b (g o) -> p b g o", o=1).to_broadcast([128, 4, 4, 4]), ALU.add)
        P.reduce(f2(m1), elm.v, ALU.max)
        P.tt(oh1.v, elm.v, bc(m1, 16), ALU.is_equal)
        P.stt(elm2.v, oh1.v, -BIG, elm.v, ALU.mult, ALU.add)
        P.reduce(f2(m2), elm2.v, ALU.max)
        P.tt(oh2.v, elm2.v, bc(m2, 16), ALU.is_equal)
        P.tt(dd.v, m2.v, m1.v, ALU.subtract)
        P.act(e21.v, dd.v, AF.Exp)
        P.ts(w1.v, e21.v, 1.0, None, ALU.add)
        P.recip(w1.v, w1.v)
        P.tt(w1p.v, w1.v, pg.v, ALU.mult)
        P.tt(w2p.v, w1p.v, e21.v, ALU.mult)
        P.tt(cmb.v, oh1.v, bc(w1p, 16), ALU.mult)
        P.tt(cmb2.v, oh2.v, bc(w2p, 16), ALU.mult)
        P.tt(combb.v, cmb.v, cmb2.v, ALU.add)

    for l in range(L):
        xsrc = x_in if l == 0 else out
        sched_layer_mod(l)
        for _g in range(NG):
            sched_group(l)
        P.dma("sp", prm_sb.v, prm[l])
        P.dma("pool", w2_sb.v, w2[l])
        P.dma("pool", bg_sb.v, prow[l][:, RW_BG:RW_BG + 256])
        P.dma("sp", wr_sb.v, wr[l].rearrange("(k p) n -> p k n", p=128))
        P.dma("sp", rbias.v, prow[l][:, RW_BR:RW_BR + 20].partition_broadcast(128))
        wl = w_in[l].rearrange("(k p) n -> p k n", p=128)
        wload(wsm[:, :, WSM_GA:WSM_GA + 16], wl[:, :, OFF["ga"]:OFF["ga"] + 16])
        wload(wsm[:, :, WSM_IQ:WSM_N], wl[:, :, OFF["diq"]:OFF["diq"] + 324])
        cc32 = P.ar("cc32", [128, 8], F32, 0)
        P.dma("sp", cc32.v, ccol.v)
        P.act(cactT.v, cc32.v, AF.Silu)
        psm = P.next_ps()
        for i in range(12):
            wb = next_wb()
            for jj in range(4):
                j = i * 4 + jj
                for k in range(8):
                    P.mm(psm[:, j:j + 1], wb[:, k, jj * 128:(jj + 1) * 128], cactT[:, k:k + 1],
                         start=(k == 0), stop=(k == 7))
            rel_wb(wb)
        P.tt(modT.v, psm[:, 0:48], prm_sb[:, PR_BMOD:PR_BMOD + 48], ALU.add)
        P.stt(sT[:, 0:8], modT[:, 8:16], 1.0, prm_sb[:, PR_G1:PR_G1 + 8], ALU.add, ALU.mult)
        P.stt(sT[:, 8:16], modT[:, 32:40], 1.0, prm_sb[:, PR_G2:PR_G2 + 8], ALU.add, ALU.mult)
        ws32 = P.ar("ws32", [128, 4, 128], F32, 2048)
        P.dma("sp", ws32.v, wsT[l])
        P.tt(ws32.v, ws32.v, trif.rearrange("p (o t) -> p o t", o=1).to_broadcast([128, 4, 128]), ALU.mult)
        P.copy(WsTm.v, ws32.v)
        bsrow = P.ar("bsrow", [1, 512], F32, 4096)
        P.dma("sp", bsrow.v, prow[l][:, RW_BS:RW_BS + 512])
        bsb = P.ar("bsb", [128, 4, 128], F32, 6144)
        for g in range(4):
            ps1 = P.next_ps()
            P.mm(ps1[:, 0:128], onesf[0:1, :], bsrow[:, g * 128:(g + 1) * 128])
            P.copy(bsb[:, g, :], ps1[:, 0:128], eng="act")
            P.mm(ps1[:, 128:256], onesf, ws32[:, g, :])
            P.stt(Cg[:, g, :], ps1[:, 128:256], prm_sb[:, PR_LNB + g:PR_LNB + g + 1], bsb[:, g, :],
                  ALU.mult, ALU.add)
        P.ts(stat[:, 32:33], prm_sb[:, PR_GQ:PR_GQ + 1], float(128 ** -0.5), None, ALU.mult)
        gq_col = stat[:, 32:33]
        gk_col = prm_sb[:, PR_GK:PR_GK + 1]
        gng_col = prm_sb[:, PR_GNG:PR_GNG + 1]
        P.memset(Sf.v, 0.0)
        P.memset(Sb.v, 0.0)

        def gt_bcast(col0, o_gtb, o_dg):
            gtb = P.ar("gtb", [128, D], F32, o_gtb)
            dg = P.ar("dg", [128, 128], F32, o_dg)
            pss = [P.next_ps(), P.next_ps()]
            for k in range(8):
                P.ts(dg.v, identf, modT[:, col0 + k:col0 + k + 1], None, ALU.mult)
                P.mm(pss[k // 4][:, (k % 4) * 128:(k % 4 + 1) * 128], onesf, dg.v)
            P.copy(gtb[:, 0:512], pss[0].v, eng="act")
            P.copy(gtb[:, 512:1024], pss[1].v, eng="act")
            return gtb

        for gi, col0 in enumerate((16, 40)):
            gtb0 = gt_bcast(col0, 16384, 20480)
            P.dma("sp", gt_dram.sub(gi)[gi], gtb0.v)

        def gt_load(gi, o_gtb):
            gtb_ = P.ar("gtb", [128, D], F32, o_gtb)
            P.dma("sp", gtb_.v, gt_dram.sub(gi)[gi])
            return gtb_

        for g in range(NG):
            for bi in range(4):
                gb = g * 4 + bi
                P.dma("sp", xg[:, bi, :], (xsrc if l == 0 else out.sub(gb))[gb * 128:(gb + 1) * 128, :])
            P.mark(f'g{g}_start')
            norm_to_hT(sT[:, 0:8], modT[:, 0:8], False)
            P.mark(f'g{g}_norm1')
            dump("modT", modT.v, [128, 48], F32)
            dump("hT", hT.v, [128, 8, 512], BF16)

            uT = P.ar("uT", [128, 4, 512], BF16, 8192)
            vg = P.ar("vg", [128, 4, 512], F32, 12288)
            vn = P.ar("vn", [128, 4, 512], BF16, 20480)
            gtmp = [P.ar(f"gtmp{i}", [128, 128], F32, 24576 + 512 * i) for i in range(2)]
            gjunk = P.ar("gjunk", [128, 512], BF16, 25600)
            wu = next_wb()
            for c in range(4):
                ps = proj_feat(wu, c * 128, 128)
                P.act(uT[:, c, :], ps.v, AF.Gelu_apprx_tanh)
            rel_wb(wu)
            wv = next_wb()
            for bi in range(4):
                ps = proj_tok(wv, 0, 512, bi)
                P.act(vg[:, bi, :], ps.v, AF.Gelu_apprx_tanh, accum=stat[:, 8 + bi:9 + bi])
                P.act(gjunk.v, vg[:, bi, :], AF.Square, accum=stat[:, 12 + bi:13 + bi])
            rel_wb(wv)
            mean = stat[:, 16:20]
            P.ts(mean, stat[:, 8:12], 1.0 / 512, None, ALU.mult)
            msq = stat[:, 20:24]
            P.tt(msq, mean, mean, ALU.mult)
            var = stat[:, 24:28]
            P.stt(var, stat[:, 12:16], 1.0 / 512, msq, ALU.mult, ALU.subtract)
            small_rstd(var, var, 1.0)
            for bi in range(4):
                P.ts(vn[:, bi, :], vg[:, bi, :], stat[:, 16 + bi:17 + bi], stat[:, 24 + bi:25 + bi],
                     ALU.subtract, ALU.mult)
            for bi in range(4):
                ps = P.next_ps()
                for gg in range(4):
                    P.mm(ps[:, gg * 128:(gg + 1) * 128], vn[:, bi, gg * 128:(gg + 1) * 128], WsTm[:, gg, :])
                for gg in range(4):
                    tmp = gtmp[gg % 2]
                    P.stt(tmp.v, ps[:, gg * 128:(gg + 1) * 128], prm_sb[:, PR_LNG + gg:PR_LNG + gg + 1],
                          Cg[:, gg, :], ALU.mult, ALU.add)
                    P.tt(ysT[:, 0, gg, bi * 128:(bi + 1) * 128], tmp.v, uT[:, gg, bi * 128:(bi + 1) * 128],
                         ALU.mult)

            P.mark(f'g{g}_gmlp')
            gaT = P.ar("gaT", [16, 512], BF16, 8192)
            ps = proj_feat(wsm, WSM_GA, 16)
            P.copy(gaT.v, ps[0:16, :])
            qk_all = P.ar("qk_all", [128, 4, 512], F32, 9216)
            v_allg = P.ar("v_allg", [128, 4, 512], BF16, 17408)
            r_all = P.ar("r_all", [128, 4, 512], BF16, 21504)
            l_sb = P.ar("l_sb", [128, 256], F32, 25600)
            e_sb = P.ar("e_sb", [128, 256], F32, 26624)
            eb = P.ar("eb", [128, 256], F32, 27648)
            ebp = P.ar("ebp", [128, 256], F32, 28672)
            ebl = P.ar("ebl", [128, 256], F32, 29696)
            tmpk = P.ar("tmpk", [128, 256], F32, 30720)
            qe = P.ar("qe", [128, 256], BF16, 31744)
            ke = P.ar("ke", [128, 256], BF16, 32256)
            kd = P.ar("kd", [128, 256], BF16, 32768)
            qkT = P.ar("qkT", [64, 8, 128], BF16, 33280)
            ATm = P.ar("ATm", [128, 4, 128], BF16, 35328)
            yb = P.ar("yb", [128, 512], BF16, 36352)
            Dcol = P.ar("Dcol", [64, 4], F32, 37376)
            oj = P.ar("oj", [128, 128], BF16, 37888)
            wqk = next_wb()
            for bi in range(4):
                ps = proj_tok(wqk, 0, 512, bi)
                P.copy(qk_all[:, bi, :], ps.v, eng="act")
            rel_wb(wqk)
            wgv = next_wb()
            for bi in range(4):
                ps = proj_tok(wgv, 0, 512, bi)
                P.copy(v_allg[:, bi, :], ps.v)
            rel_wb(wgv)
            wgr = next_wb()
            for bi in range(4):
                ps = proj_tok(wgr, 0, 512, bi)
                P.act(r_all[:, bi, :], ps.v, AF.Silu)
            rel_wb(wgr)
            for bi in range(4):
                bs = slice(bi * 128, (bi + 1) * 128)
                qk_sb = qk_all[:, bi, :]
                v_sb = v_allg[:, bi, :]
                r_sb = r_all[:, bi, :]
                psz = P.next_ps()
                P.mm(psz[:, 0:256], gaT[:, bs], w2_sb.v, start=True, stop=False)
                P.mm(psz[:, 0:256], ones1.v, bg_sb.v, start=False, stop=True)
                P.act(e_sb.v, psz[:, 0:256], AF.Exp, scale=-1.0)
                P.act(l_sb.v, e_sb.v, AF.Ln, bias=1.0)
                psc = P.next_ps()
                P.mm(psc[:, 0:256], trif, l_sb.v)
                P.mm(psc[:, 256:512], onesf, l_sb.v)
                psd = P.next_ps()
                for h in range(4):
                    P.mm(psd[0:64, h:h + 1], l_sb[:, h * 64:(h + 1) * 64], onesf[:, 0:1])
                P.act(eb.v, psc[:, 0:256], AF.Exp, scale=-1.0 / 16)
                P.act(ebp.v, psc[:, 0:256], AF.Exp, scale=1.0 / 16)
                P.act(ebl.v, psc[:, 256:512], AF.Exp, scale=-1.0 / 16)
                P.act(Dcol.v, psd[0:64, 0:4], AF.Exp, scale=-1.0 / 16)
                P.stt(qe.v, qk_sb[:, 0:256], 0.125, eb.v, ALU.mult, ALU.mult)
                P.tt(ke.v, qk_sb[:, 256:512], ebp.v, ALU.mult)
                P.tt(tmpk.v, ebp.v, ebl.v, ALU.mult)
                P.tt(kd.v, qk_sb[:, 256:512], tmpk.v, ALU.mult)
                pst = P.next_ps()
                pstb = pst.v.bitcast(BF16)
                for h in range(4):
                    P.tr(pstb[0:64, h * 128:(h + 1) * 128], qe[:, h * 64:(h + 1) * 64], identb.v)
                    P.tr(pstb[0:64, (4 + h) * 128:(5 + h) * 128], ke[:, h * 64:(h + 1) * 64], identb.v)
                P.copy(qkT.v.rearrange("p a b -> p (a b)"), pstb[0:64, :], eng="act")
                psa = P.next_ps()
                for h in range(4):
                    P.mm(psa[:, h * 128:(h + 1) * 128], qkT[:, 4 + h, :], qkT[:, h, :])
                P.tt(ATm.v, psa.v.rearrange("p (h t) -> p h t", h=4),
                     trif.rearrange("p (o t) -> p o t", o=1).to_broadcast([128, 4, 128]), ALU.mult)
                pso = P.next_ps()
                for h in range(4):
                    hs = slice(h * 128, (h + 1) * 128)
                    P.mm(pso[:, hs], ATm[:, h, :], v_sb[:, hs], start=True, stop=False)
                    P.mm(pso[:, hs], qkT[:, h, :], Sb[:, h, :], start=False, stop=True)
                psu = P.next_ps()
                for h in range(4):
                    P.mm(psu[0:64, h * 128:(h + 1) * 128], kd[:, h * 64:(h + 1) * 64], v_sb[:, h * 128:(h + 1) * 128])
                for h in range(4):
                    P.stt(Sf[:, h, :], Sf[:, h, :], Dcol[:, h:h + 1], psu[0:64, h * 128:(h + 1) * 128],
                          ALU.mult, ALU.add)
                P.copy(Sb.v, Sf.v)
                for h in range(4):
                    P.act(oj.v, pso[:, h * 128:(h + 1) * 128], AF.Square, accum=stat[:, 36 + h:37 + h])
                small_rstd(stat[:, 36:40], stat[:, 36:40], 1.0 / 128)
                for h in range(4):
                    hs = slice(h * 128, (h + 1) * 128)
                    P.stt(yb[:, hs], pso[:, hs], stat[:, 36 + h:37 + h], r_sb[:, hs], ALU.mult, ALU.mult)
                pst2 = P.next_ps()
                pst2b = pst2.v.bitcast(BF16)
                for h in range(4):
                    P.tr(pst2b[:, h * 128:(h + 1) * 128], yb[:, h * 128:(h + 1) * 128], identb.v)
                P.act(ysT[:, 1, :, bs], pst2b[:, 0:512].rearrange("p (h t) -> p h t", h=4), AF.Identity,
                      scale=gng_col)

            P.mark(f'g{g}_gla')
            kn = P.ar("kn", [128, 512], BF16, 8192)
            qT = P.ar("qT", [128, 4, 512], BF16, 9216)
            iqT = P.ar("iqT", [64, 4, 512], BF16, 13312)
            iw_sb = P.ar("iw_sb", [128, 4, 4], F32, 17408)
            relu_t = [P.ar("relu0", [128, 512], F32, 17920)]
            PT = [P.ar(f"PT{i}", [128, 512], BF16, 19968 + 1024 * i) for i in range(2)]
            yc = P.ar("yc", [128, 512], BF16, 22016)
            bjunk = P.ar("bjunk", [128, 128], BF16, 23040)
            score = P.ar("score", [128, S], F32, 23552)
            maskbuf = [P.ar(f"maskbuf{i}", [128, S], BF16, 23552 + 4 * S + 2 * S * i) for i in range(2)]
            cD = max(max(1, n_ * 15 // 32) for n_ in range(1, NBLK + 1)) * 128
            cA = max(n_ - max(1, n_ * 15 // 32) for n_ in range(1, NBLK + 1)) * 128
            assert cD + cA <= S
            jDb = [P.ar(f"jD{i}", [128, cD], BF16, 23552 + 4 * S + 2 * S * i) for i in range(2)]
            jAb = [P.ar(f"jA{i}", [128, cA], BF16, 23552 + 4 * S + 2 * S * i + 2 * cD) for i in range(2)]

            def qk_norm_T(ps, gcol, dst):
                for h in range(4):
                    P.act(bjunk.v, ps[:, h * 128:(h + 1) * 128], AF.Square, accum=stat[:, 40 + h:41 + h])
                small_rstd(stat[:, 40:44], stat[:, 40:44], 1.0 / 128)
                for h in range(4):
                    P.ts(kn[:, h * 128:(h + 1) * 128], ps[:, h * 128:(h + 1) * 128], stat[:, 40 + h:41 + h],
                         None, ALU.mult)
                pt = P.next_ps()
                ptb = pt.v.bitcast(BF16)
                for h in range(4):
                    P.tr(ptb[:, h * 128:(h + 1) * 128], kn[:, h * 128:(h + 1) * 128], identb.v)
                P.act(dst, ptb[:, 0:512].rearrange("p (h t) -> p h t", h=4), AF.Identity, scale=gcol)

            for bi in range(4):
                gb = g * 4 + bi
                bs = slice(bi * 128, (bi + 1) * 128)
                psw = P.next_ps()
                for k in range(8):
                    P.mm(psw[:, 0:4], hT[:, k, bs], wsm[:, k, WSM_IW:WSM_IW + 4], start=(k == 0), stop=(k == 7))
                P.copy(iw_sb[:, bi, :], psw[:, 0:4])
            ps = proj_feat(wsm, WSM_IK, 64)
            P.copy(ikT_all[:, g * 512:(g + 1) * 512], ps[0:64, :], eng="act")
            for h in range(4):
                ps = proj_feat(wsm, WSM_IQ + h * 64, 64)
                P.copy(iqT[:, h, :], ps[0:64, :], eng="act")


            def IB_gen(bi):
                gb = g * 4 + bi
                bs = slice(bi * 128, (bi + 1) * 128)
                nk = (gb + 1) * 128
                nch = (nk + 511) // 512
                mk = maskbuf[bi % 2]
                for c in range(nch):
                    c0, c1 = c * 512, min((c + 1) * 512, nk)
                    w = c1 - c0
                    for h in range(4):
                        psi = P.next_ps()
                        P.mm(psi[:, 0:w], iqT[:, h, bs], ikT_all[:, c0:c1])
                        rl = relu_t[0]
                        P.act(rl[:, 0:w], psi[:, 0:w], AF.Relu)
                        if h == 0:
                            P.ts(score[:, c0:c1], rl[:, 0:w], iw_sb[:, bi, 0:1], None, ALU.mult)
                        else:
                            P.stt(score[:, c0:c1], rl[:, 0:w], iw_sb[:, bi, h:h + 1], score[:, c0:c1],
                                  ALU.mult, ALU.add)
                    yield
                P.tt(score[:, gb * 128:nk], score[:, gb * 128:nk], negtri, ALU.add)
                tau = stat[:, 44:45]
                if gb < 2:
                    P.memset(tau, -1.0e29)
                else:
                    rng = stat[:, 45:46]
                    mid = stat[:, 46:47]
                    cnt = stat[:, 47:48]
                    u = stat[:, 48:49]
                    hi = stat[:, 49:50]
                    lo = stat[:, 50:51]
                    P.ts(mk[:, 0:nk], score[:, 0:nk], 0.0, -3.0e38, ALU.add, ALU.max, accum=hi)
                    P.ts(mk[:, 0:256], score[:, 0:256], 0.0, 3.0e38, ALU.add, ALU.min, accum=lo)
                    P.tt(rng, hi, lo, ALU.subtract)
                    P.ts(offs.v, fv.v, rng, None, ALU.mult)
                    P.tt(mid, lo, offs[:, 0:1], ALU.add)
                    for it in range(NBIS):
                        P.ts(mk[:, 0:nk], score[:, 0:nk], mid, 0.0, ALU.is_ge, ALU.add, accum=cnt)
                        last = (it == NBIS - 1)
                        P.ts(u, cnt, 256.0, 1.0 if last else 0.5, ALU.is_ge, ALU.subtract)
                        P.stt(tau if last else mid, u, offs[:, it:it + 1], mid, ALU.mult, ALU.add)
                        yield
                P.ts(mk[:, 0:nk], score[:, 0:nk], tau, NEG, ALU.is_lt, ALU.mult)
                yield

            def drain(gen):
                for _ in gen:
                    pass

            def interleave(ga, gb_):
                a_done = b_done = False
                while not (a_done and b_done):
                    if not a_done:
                        try:
                            next(ga)
                        except StopIteration:
                            a_done = True
                    if not b_done:
                        try:
                            next(gb_)
                        except StopIteration:
                            b_done = True

            def ATT_gen(bi):
                gb = g * 4 + bi
                bs = slice(bi * 128, (bi + 1) * 128)
                mk = maskbuf[bi % 2]
                P.memset(psA.v, 0.0)
                P.memset(psB.v, 0.0)
                def LG(j):
                    psl = P.next_ps()
                    dist = gb - j
                    P.mm(psl.v, mk[:, j * 128:(j + 1) * 128], ident4.v, start=True, stop=False)
                    if dist <= 1:
                        P.mm(psl.v, identb.v, biasT[:, dist, :, :].rearrange("p h t -> p (h t)"),
                             start=False, stop=False)
                    for h in range(4):
                        hs = slice(h * 128, (h + 1) * 128)
                        P.mm(psl[:, hs], kT_all[:, h, j * 128:(j + 1) * 128], qT[:, h, bs],
                             start=False, stop=(h == 3))
                    return psl

                psl = LG(0)
                P.act(PT[0].v, psl.v, AF.Exp, bias=-6.0)
                for j in range(gb + 1):
                    pt = PT[j % 2]
                    if j + 1 <= gb:
                        psl_next = LG(j + 1)
                    yield
                    for h in range(4):
                        acc = psA if h < 2 else psB
                        a0 = (h % 2) * 130
                        P.mm(acc[:, a0:a0 + 129], pt[:, h * 128:(h + 1) * 128], v_all[:, j, h, 0:129],
                             start=False, stop=False, skip=True)
                    if j + 1 <= gb:
                        P.act(PT[(j + 1) % 2].v, psl_next.v, AF.Exp, bias=-6.0)
                for h in range(4):
                    acc = psA if h < 2 else psB
                    a0 = (h % 2) * 130
                    P.recip(stat[:, 52 + h:53 + h], acc[:, a0 + 128:a0 + 129])
                    P.ts(yc[:, h * 128:(h + 1) * 128], acc[:, a0:a0 + 128], stat[:, 52 + h:53 + h], None, ALU.mult)
                pt2 = P.next_ps()
                pt2b = pt2.v.bitcast(BF16)
                for h in range(4):
                    P.tr(pt2b[:, h * 128:(h + 1) * 128], yc[:, h * 128:(h + 1) * 128], identb.v)
                P.copy(ysT[:, 2, :, bs], pt2b[:, 0:512].rearrange("p (h t) -> p h t", h=4), eng="act")

            def projA_gen():
                wdq = next_wb()
                for bi in range(4):
                    bs = slice(bi * 128, (bi + 1) * 128)
                    ps = proj_tok(wdq, 0, 512, bi)
                    qk_norm_T(ps, gq_col, qT[:, :, bs])
                    yield
                rel_wb(wdq)
                wdk = next_wb()
                for bi in range(4):
                    gb = g * 4 + bi
                    ps = proj_tok(wdk, 0, 512, bi)
                    qk_norm_T(ps, gk_col, kT_all[:, :, gb * 128:(gb + 1) * 128])
                    yield
                rel_wb(wdk)
                wdv = next_wb()
                for bi in range(4):
                    gb = g * 4 + bi
                    ps = proj_tok(wdv, 0, 512, bi)
                    P.copy(v_all[:, gb, :, 0:128], ps.v.rearrange("p (h e) -> p h e", h=4))
                    yield
                rel_wb(wdv)

            interleave(projA_gen(), IB_gen(0))
            if l + 1 < L:
                chs = cast_chunks(l + 1)
                per = (len(chs) + NG - 1) // NG
                for dst_, src_ in chs[g * per:(g + 1) * per]:
                    P.dma("pool", dst_, src_)
            P.mark(f'g{g}_dsaproj')
            for bi in range(4):
                if bi + 1 < 4:
                    interleave(ATT_gen(bi), IB_gen(bi + 1))
                else:
                    drain(ATT_gen(bi))

            P.mark(f'g{g}_dsa')
            dump("ysT", ysT.v, [128, 3, 4, 512], BF16)
            dump("kT", kT_all[:, :, 0:512], [128, 4, 512], BF16)
            gtb = gt_load(0, 40960)
            mergedF = P.ar("mergedF", [128, 8, 512], F32, 8192)
            mergedT = P.ar("mergedT", [128, 8, 512], BF16, 24576)
            sgt = [P.ar(f"sgt{i}", [128, 512], BF16, 32768 + 1024 * i) for i in range(2)]
            mtmp = [P.ar(f"mtmp{i}", [128, 512], F32, 34816 + 2048 * i) for i in range(2)]
            ci = 0
            for n in range(3):
                wbr = next_wb()
                wbrv = wbr.v.rearrange("p a b -> p (a b)").rearrange("p (e d) -> p e d", e=4)
                for half in range(2):
                    wg = next_wb()
                    for cc in range(4):
                        dc = half * 4 + cc
                        psg = proj_feat(wg, cc * 128, 128)
                        sg = sgt[ci % 2]
                        P.act(sg.v, psg.v, AF.Sigmoid, bias=prm_sb[:, PR_BBG + n * 8 + dc:PR_BBG + n * 8 + dc + 1])
                        psu = P.next_ps()
                        for ec in range(4):
                            P.mm(psu.v, wbrv[:, ec, dc * 128:(dc + 1) * 128], ysT[:, n, ec, :],
                                 start=(ec == 0), stop=(ec == 3))
                        if n == 0:
                            P.tt(mergedF[:, dc, :], psu.v, sg.v, ALU.mult)
                        else:
                            mt = mtmp[ci % 2]
                            P.tt(mt.v, psu.v, sg.v, ALU.mult)
                            if n == 1:
                                P.tt(mergedF[:, dc, :], mergedF[:, dc, :], mt.v, ALU.add)
                            else:
                                P.tt(mergedT[:, dc, :], mergedF[:, dc, :], mt.v, ALU.add)
                        ci += 1
                    rel_wb(wg)
                rel_wb(wbr)
            P.mark(f'g{g}_merge')
            for half in range(2):
                wo = next_wb()
                for bi in range(4):
                    ps = P.next_ps()
                    for k in range(8):
                        P.mm(ps.v, mergedT[:, k, bi * 128:(bi + 1) * 128], wo[:, k, :], start=(k == 0), stop=(k == 7))
                    mt = mtmp[bi % 2]
                    P.tt(mt.v, ps.v, gtb[:, half * 512:(half + 1) * 512], ALU.mult)
                    P.tt(xg[:, bi, half * 512:(half + 1) * 512], xg[:, bi, half * 512:(half + 1) * 512], mt.v,
                         ALU.add)
                rel_wb(wo)

            dump("mergedT", mergedT.v, [128, 8, 512], BF16)
            dump("x1", xg.v, [128, 4, D], F32)
            P.mark(f'g{g}_wout')
            gtb = gt_load(1, 16384)
            norm_to_hT(sT[:, 8:16], modT[:, 24:32], True)
            dump("h2T", hT.v, [128, 8, 512], BF16)
            dump("combb", combb.v, [128, 4, 16], BF16)
            P.mark(f'g{g}_norm2')
            cb_sb = P.ar("cb_sb", [128, 512], BF16, 0)
            t1 = [P.ar("t1", [128, 512], BF16, 1024)]
            sgm = [P.ar(f"sgm{i}", [128, 512], BF16, 2048 + 1024 * i) for i in range(2)]
            actT = P.ar("actT", [128, 4, 2, 512], BF16, 4096)
            mtmp = [P.ar(f"mtmpb{i}", [128, 512], F32, 12288 + 2048 * i) for i in range(2)]
            WD = [P.ar(f"wd{i}", [128, 2, D], BF16, 21504 + 4096 * i) for i in range(6)]
            wdi = 0
            for rnd in range(4):
                wds = []
                for ee in range(4):
                    e = rnd * 4 + ee
                    wgu = next_wb()
                    wd = WD[wdi % 6]
                    wdi += 1
                    P.dma(wsrc(l)[0], wd.v, wsrc(l)[6][e].rearrange("(f p) d -> p f d", p=128))
                    wds.append(wd)
                    psc = P.next_ps()
                    for bi in range(4):
                        P.mm(psc[:, bi * 128:(bi + 1) * 128], combb[:, bi, e:e + 1].to_broadcast([128, 128]),
                             identb.v)
                    P.copy(cb_sb.v, psc.v, eng="act")
                    for fc in range(2):
                        psg = proj_feat(wgu, fc * 128, 128)
                        psu = proj_feat(wgu, 256 + fc * 128, 128)
                        sg = sgm[fc]
                        P.act(sg.v, psg.v, AF.Silu)
                        P.tt(t1[0].v, psu.v, sg.v, ALU.mult)
                        P.tt(actT[:, ee, fc, :], t1[0].v, cb_sb.v, ALU.mult)
                    rel_wb(wgu)
                for bi in range(4):
                    for half in range(2):
                        ps = P.next_ps()
                        n = 0
                        for ee in range(4):
                            for fc in range(2):
                                P.mm(ps.v, actT[:, ee, fc, bi * 128:(bi + 1) * 128],
                                     wds[ee][:, fc, half * 512:(half + 1) * 512], start=(n == 0), stop=(n == 7))
                                n += 1
                        mt = mtmp[(bi * 2 + half) % 2]
                        P.tt(mt.v, ps.v, gtb[:, half * 512:(half + 1) * 512], ALU.mult)
                        P.tt(xg[:, bi, half * 512:(half + 1) * 512], xg[:, bi, half * 512:(half + 1) * 512], mt.v,
                             ALU.add)
            P.mark(f'g{g}_moe')
            for bi in range(4):
                gb = g * 4 + bi
                P.dma("sp", out.sub(gb)[gb * 128:(gb + 1) * 128, :], xg[:, bi, :])
    P.fence("sp", [out] + list(dbgo.values()))
    stats = P.emit()
    stats['marks'] = getattr(P, 'marks', [])
    return nc, stats


def _bucket_np(rel):
    import math
    n = np.maximum(rel, 0)
    max_exact = 16
    large = max_exact + (np.log(np.maximum(n, max_exact).astype(np.float32) / max_exact)
                         / math.log(128 / max_exact) * (32 - max_exact)).astype(np.int32)
    large = np.minimum(large, 31)
    return np.where(n < max_exact, n, large)


def host_consts():
    cst = np.zeros((128, NCST), np.float32)
    i = np.arange(128)
    cst[:, C_ID:C_ID + 128] = np.eye(128, dtype=np.float32)
    cst[:, C_TRI:C_TRI + 128] = (i[:, None] <= i[None, :]).astype(np.float32)
    cst[:, C_ONES:C_ONES + 128] = 1.0
    cst[:, C_NEGTRI:C_NEGTRI + 128] = np.where(i[None, :] <= i[:, None], 0.0, BIGNEG)
    cst[:, C_BKD:C_BKD + 128] = _bucket_np(i[None, :] - i[:, None]).astype(np.float32)
    cst[:, C_BKN:C_BKN + 128] = _bucket_np(128 + i[None, :] - i[:, None]).astype(np.float32)
    return cst


def host_layout(inputs, L, b, S):
    f = lambda a: np.ascontiguousarray(np.asarray(a, dtype=np.float32))
    col = lambda v: np.asarray(v, np.float32).reshape(-1, 128).T
    m = {}
    m["x"] = f(inputs["x"][b, :S])
    m["ccol"] = f(col(inputs["c"][b]))
    m["cst"] = host_consts()
    m["rbrow"] = f(np.asarray(inputs["rel_bias"]).reshape(1, 128))
    prm = np.zeros((L, 128, NPRM), np.float32)
    prow = np.zeros((L, 1, NROW), np.float32)
    for l in range(L):
        prm[l, :, PR_G1:PR_G1 + 8] = col(inputs["g_norm1"][l])
        prm[l, :, PR_G2:PR_G2 + 8] = col(inputs["g_norm2"][l])
        prm[l, :, PR_BMOD:PR_BMOD + 48] = col(inputs["b_mod"][l])
        prm[l, :, PR_LNG:PR_LNG + 4] = col(inputs["gmlp_ln_g"][l])
        prm[l, :, PR_LNB:PR_LNB + 4] = col(inputs["gmlp_ln_b"][l])
        prm[l, :, PR_GNG] = np.asarray(inputs["gla_norm_g"][l])
        prm[l, :, PR_GQ] = np.asarray(inputs["dsa_qnorm_g"][l])
        prm[l, :, PR_GK] = np.asarray(inputs["dsa_knorm_g"][l])
        prm[l, :, PR_BBG:PR_BBG + 24] = col(np.asarray(inputs["b_branch_gate"][l]).reshape(-1))
        prow[l, 0, RW_BS:RW_BS + 512] = np.asarray(inputs["gmlp_b_s"][l]).reshape(-1)
        prow[l, 0, RW_BG:RW_BG + 256] = np.asarray(inputs["gla_b_gate"][l])
        prow[l, 0, RW_BR:RW_BR + 4] = np.asarray(inputs["b_group"][l])
        prow[l, 0, RW_BR + 4:RW_BR + 20] = np.asarray(inputs["b_router"][l])
    m["prm"] = prm
    m["prow"] = prow
    m["w_mod"] = f(inputs["w_mod"][:L])
    m["w_in"] = f(inputs["w_in"][:L])
    m["wsT"] = f(np.asarray(inputs["gmlp_w_s"][:L]).transpose(0, 3, 1, 2))
    m["w2"] = f(inputs["gla_w_gate2"][:L])
    m["w_branch"] = f(inputs["w_branch"][:L])
    m["w_out"] = f(inputs["w_out"][:L])
    m["wr"] = f(np.concatenate([np.asarray(inputs["w_group"][:L]), np.asarray(inputs["w_router"][:L])], axis=2))
    m["w_exp_gate"] = f(inputs["w_exp_gate"][:L])
    m["w_exp_up"] = f(inputs["w_exp_up"][:L])
    m["w_exp_down"] = f(inputs["w_exp_down"][:L])
    return m


_PROG_CACHE = {}


def run_model(inputs, L, S, batches, n_cores, want_all=False, active=None):
    key = (L, S)
    if key not in _PROG_CACHE:
        _PROG_CACHE[key] = build_program(L, S)
    nc, stats = _PROG_CACHE[key]
    if active is None:
        active = list(range(min(n_cores, len(batches))))
    shared = host_layout(inputs, L, batches[0], S)
    zeros = None
    in_maps = []
    for ci in range(n_cores):
        if ci in active:
            b = batches[active.index(ci)]
            m = dict(shared)
            m["x"] = np.ascontiguousarray(np.asarray(inputs["x"][b, :S], dtype=np.float32))
            m["ccol"] = np.ascontiguousarray(np.asarray(inputs["c"][b], np.float32).reshape(-1, 128).T)
        else:
            if zeros is None:
                zeros = {k: np.zeros_like(v) for k, v in shared.items()}
            m = dict(zeros)
        in_maps.append(m)
    res = run_bass_kernel_spmd(nc, in_maps, core_ids=list(range(n_cores)))
    if want_all:
        return res.results
    return [res.results[ci]["out"] for ci in active]


def kernel(**inputs):
    L, B, S = 4, 4, 4096
    outs = run_model(inputs, L, S, list(range(B)), 8, active=[0, 1, 4, 5])
    return np.stack(outs, axis=0).astype(np.float32)
```

```python
import numpy as np
import concourse.bass as bass
import concourse.mybir as mybir

F32 = mybir.dt.float32
BF16 = mybir.dt.bfloat16
AF = mybir.ActivationFunctionType
ALU = mybir.AluOpType
AX = mybir.AxisListType

GRAN = 512
SEM_CAP = 30000
STRICT_SAME_ENGINE = True


class Region:
    __slots__ = ("lw", "rd")

    def __init__(self):
        self.lw = None
        self.rd = {}


class View:
    __slots__ = ("buf", "ap")

    def __init__(self, buf, ap):
        self.buf = buf
        self.ap = ap

    def __getitem__(self, idx):
        return View(self.buf, self.ap[idx])

    def rearrange(self, s, **kw):
        return View(self.buf, self.ap.rearrange(s, **kw))

    def bitcast(self, dt):
        return View(self.buf, self.ap.bitcast(dt))

    def to_broadcast(self, shape):
        return View(self.buf, self.ap.to_broadcast(shape))

    def partition_broadcast(self, n):
        return View(self.buf, self.ap.partition_broadcast(n))


class Buf:
    def __init__(self, name, h, regions, is_dram=False):
        self.name = name
        self.h = h
        self.regions = regions
        self.is_dram = is_dram

    def __getitem__(self, idx):
        base = self.h.ap() if self.is_dram else self.h
        return View(self, base[idx])

    @property
    def v(self):
        base = self.h.ap() if self.is_dram else self.h[:]
        return View(self, base)

    def sub(self, i, n=1):
        return Buf(self.name, self.h, self.regions[i:i + n], self.is_dram)


class Op:
    __slots__ = ("eng", "fn", "kind", "deps", "lidx", "flag", "waits", "clock",
                 "fnum", "dma_sem", "dma_val", "dq_n")


class Prog:
    ENGS = ("pe", "act", "dve", "pool", "sp")

    def __init__(self, nc, n_dma_sems=None):
        self.nc = nc
        self.h = {"pe": nc.tensor, "act": nc.scalar, "dve": nc.vector,
                  "pool": nc.gpsimd, "sp": nc.sync}
        self.ops = []
        self.n_dma_sems = n_dma_sems or {"sp": 16, "pool": 16, "act": 4}
        self.dma_count = {e: 0 for e in self.ENGS}
        self.dma_ids = {e: [] for e in self.ENGS}
        self.arena_base = None
        self.arena_regions = None
        self.psum_rot = []
        self.psum_i = 0

    def sb(self, name, shape, dtype):
        h = self.nc.alloc_sbuf_tensor(name, list(shape), dtype)
        return Buf(name, h, [Region()])

    def make_arena(self, nbytes):
        r = self.nc.bump_sbuf(nbytes)
        self.arena_base = r[0]
        self.arena_size = nbytes
        self.arena_regions = [Region() for _ in range((nbytes + GRAN - 1) // GRAN)]

    def ar(self, name, shape, dtype, off):
        key = (name, tuple(shape), str(dtype), off)
        if not hasattr(self, "_arc"):
            self._arc = {}
        if key in self._arc:
            return self._arc[key]
        b = self._ar(f"{name}_{len(self._arc)}", shape, dtype, off)
        self._arc[key] = b
        return b

    def _ar(self, name, shape, dtype, off):
        esz = {F32: 4, BF16: 2}[dtype]
        n = int(np.prod(shape[1:])) * esz
        assert off % 32 == 0 and off + n <= self.arena_size, (name, off, n, self.arena_size)
        h = self.nc.alloc_sbuf_tensor_at(name, list(shape), dtype, offset=self.arena_base + off)
        regs = self.arena_regions[off // GRAN:(off + n + GRAN - 1) // GRAN]
        return Buf(name, h, regs)

    def ps(self, name, shape=(128, 512), dtype=F32):
        h = self.nc.alloc_psum_tensor(name, list(shape), dtype)
        return Buf(name, h, [Region()])

    def dram(self, name, shape, dtype, kind="Internal", nreg=1):
        h = self.nc.dram_tensor(name, list(shape), dtype, kind=kind)
        return Buf(name, h, [Region() for _ in range(nreg)], is_dram=True)

    def fence(self, eng, bufs):
        h = self.h[eng]
        self.add(eng, lambda: h.nop(), bufs, [])

    def mark(self, name):
        if not getattr(self, "marks_on", False):
            return
        last = {}
        for oid in range(len(self.ops) - 1, -1, -1):
            o = self.ops[oid]
            if o.kind == "c" and o.eng != "sp" and o.eng not in last:
                last[o.eng] = oid
            if len(last) == 4:
                break
        h = self.h["sp"]
        oid = self.add("sp", lambda: h.nop(), [], [])
        for e, d in last.items():
            self.ops[oid].deps[d] = True
        if not hasattr(self, "marks"):
            self.marks = []
        self.marks.append(name)

    def next_ps(self):
        b = self.psum_rot[self.psum_i % len(self.psum_rot)]
        self.psum_i += 1
        return b

    def add(self, eng, fn, reads, writes, kind="c"):
        op = Op()
        op.eng = eng
        op.fn = fn
        op.kind = kind
        op.flag = False
        op.deps = {}
        oid = len(self.ops)
        rregs = []
        for b in reads:
            if b is None:
                continue
            b = b.buf if isinstance(b, View) else b
            rregs.extend(b.regions)
        wregs = []
        for b in writes:
            b = b.buf if isinstance(b, View) else b
            wregs.extend(b.regions)
        for r in rregs:
            if r.lw is not None:
                op.deps[r.lw] = True
        for r in wregs:
            if r.lw is not None:
                op.deps.setdefault(r.lw, False)
            for rid in r.rd.values():
                op.deps.setdefault(rid, False)
        op.deps.pop(oid, None)
        for r in rregs:
            key = eng if kind == "c" else ("dma", oid)
            r.rd[key] = oid
        for r in wregs:
            r.lw = oid
            r.rd = {}
        if kind == "dma":
            n = self.dma_count[eng]
            self.dma_count[eng] += 1
            op.dq_n = n
            self.dma_ids[eng].append(oid)
        self.ops.append(op)
        return oid

    @staticmethod
    def _a(x):
        return x.ap if isinstance(x, View) else x

    def mm(self, out, lhsT, rhs, start=True, stop=True, skip=False):
        nc = self.nc
        if skip:
            self.add("pe", lambda: nc.tensor.matmul(out.ap, lhsT.ap, rhs.ap, start=start, stop=stop,
                                                    skip_group_check=True), [lhsT, rhs], [out])
        else:
            self.add("pe", lambda: nc.tensor.matmul(out.ap, lhsT.ap, rhs.ap, start=start, stop=stop),
                     [lhsT, rhs], [out])

    def tr(self, out, in_, ident):
        nc = self.nc
        self.add("pe", lambda: nc.tensor.transpose(out.ap, in_.ap, ident.ap), [in_, ident], [out])

    def act(self, out, in_, func, bias=None, scale=1.0, accum=None):
        nc = self.nc
        a = self._a
        kw = {}
        if bias is not None:
            kw["bias"] = a(bias)
        if accum is not None:
            kw["accum_out"] = a(accum)
        rd = [in_] + [x for x in (bias, scale) if isinstance(x, View)]
        wr = [out] + ([accum] if accum is not None else [])
        self.add("act", lambda: nc.scalar.activation(out=out.ap, in_=in_.ap, func=func,
                                                      scale=a(scale), **kw), rd, wr)

    def _veng(self, eng):
        return self.nc.vector if eng == "dve" else self.nc.gpsimd

    def tt(self, out, in0, in1, op, eng="dve"):
        e = self._veng(eng)
        self.add(eng, lambda: e.tensor_tensor(out=out.ap, in0=in0.ap, in1=in1.ap, op=op),
                 [in0, in1], [out])

    def ts(self, out, in0, s1, s2, op0, op1=None, accum=None, eng="dve"):
        e = self._veng(eng)
        a = self._a
        kw = {}
        if op1 is not None:
            kw["op1"] = op1
        if accum is not None:
            kw["accum_out"] = a(accum)
        rd = [in0] + [x for x in (s1, s2) if isinstance(x, View)]
        wr = [out] + ([accum] if accum is not None else [])
        self.add(eng, lambda: e.tensor_scalar(out=out.ap, in0=in0.ap, scalar1=a(s1), scalar2=a(s2),
                                              op0=op0, **kw), rd, wr)

    def stt(self, out, in0, scalar, in1, op0, op1, eng="dve"):
        e = self._veng(eng)
        a = self._a
        rd = [in0, in1] + ([scalar] if isinstance(scalar, View) else [])
        self.add(eng, lambda: e.scalar_tensor_tensor(out=out.ap, in0=in0.ap, scalar=a(scalar),
                                                     in1=in1.ap, op0=op0, op1=op1), rd, [out])

    def copy(self, out, in_, eng="dve"):
        if eng == "act":
            nc = self.nc
            self.add("act", lambda: nc.scalar.copy(out=out.ap, in_=in_.ap), [in_], [out])
        else:
            e = self._veng(eng)
            self.add(eng, lambda: e.tensor_copy(out=out.ap, in_=in_.ap), [in_], [out])

    def memset(self, out, val, eng="dve"):
        e = self._veng(eng)
        self.add(eng, lambda: e.memset(out.ap, val), [], [out])

    def reduce(self, out, in_, op, eng="dve"):
        e = self._veng(eng)
        self.add(eng, lambda: e.tensor_reduce(out=out.ap, in_=in_.ap, axis=AX.X, op=op), [in_], [out])

    def recip(self, out, in_):
        nc = self.nc
        self.add("dve", lambda: nc.vector.reciprocal(out=out.ap, in_=in_.ap), [in_], [out])

    def dma(self, q, out, in_, **kw):
        h = self.h[q]
        self.add(q, lambda: h.dma_start(out=out.ap, in_=in_.ap, **kw), [in_], [out], kind="dma")

    def emit(self):
        nc = self.nc
        ops = self.ops
        known = {e: {} for e in self.ENGS}
        dma_known = {e: set() for e in self.ENGS}
        count = {e: 0 for e in self.ENGS}
        for oid, op in enumerate(ops):
            E = op.eng
            waits = []
            kn = known[E]
            if op.kind == "dma":
                ns = self.n_dma_sems[E]
                if op.dq_n >= ns:
                    prev = self.dma_ids[E][op.dq_n - ns]
                    if prev not in dma_known[E]:
                        waits.append(prev)
                        dma_known[E].add(prev)
            for d, raw in op.deps.items():
                p = ops[d]
                if p.kind == "dma":
                    if d in dma_known[E]:
                        continue
                    waits.append(d)
                    dma_known[E].add(d)
                else:
                    if p.eng == E and (E == "pe" or (not raw and not STRICT_SAME_ENGINE)):
                        continue
                    if kn.get(p.eng, 0) >= p.lidx:
                        continue
                    waits.append(d)
                    p.flag = True
                    for e2, v in p.clock.items():
                        if kn.get(e2, 0) < v:
                            kn[e2] = v
                    if kn.get(p.eng, 0) < p.lidx:
                        kn[p.eng] = p.lidx
            op.waits = waits
            count[E] += 1
            op.lidx = count[E]
            op.clock = dict(kn) if op.kind == "c" else None
        fcount = {e: 0 for e in self.ENGS}
        for op in ops:
            if op.kind == "c" and op.flag:
                fcount[op.eng] += 1
                op.fnum = fcount[op.eng]
        sems = {}
        for e in self.ENGS:
            n = (fcount[e] + SEM_CAP - 1) // SEM_CAP
            sems[e] = [nc.alloc_semaphore(f"s_{e}_{i}") for i in range(n)]
        dsems = {}
        for e in self.ENGS:
            if self.dma_count[e]:
                dsems[e] = [nc.alloc_semaphore(f"d_{e}_{i}")
                            for i in range(min(self.n_dma_sems[e], self.dma_count[e]))]
        for op in ops:
            if op.kind == "dma":
                ns = self.n_dma_sems[op.eng]
                op.dma_sem = dsems[op.eng][op.dq_n % ns]
                op.dma_val = 16 * (op.dq_n // ns + 1)
        nwaits = 0
        for op in ops:
            h = self.h[op.eng]
            for d in op.waits:
                p = ops[d]
                if p.kind == "dma":
                    h.wait_ge(p.dma_sem, p.dma_val)
                else:
                    n = p.fnum - 1
                    h.wait_ge(sems[p.eng][n // SEM_CAP], n % SEM_CAP + 1)
                nwaits += 1
            ins = op.fn()
            if op.kind == "dma":
                ins.then_inc(op.dma_sem, 16)
            elif op.flag:
                n = op.fnum - 1
                ins.then_inc(sems[op.eng][n // SEM_CAP], 1)
        self.stats = dict(n_ops=len(ops), n_waits=nwaits, flagged=dict(fcount),
                          per_eng=dict(count))
        return self.stats

    def final_wait(self, eng, opids):
        pass


from concourse.bass_utils import run_bass_kernel_spmd

D = 1024
EPS = 1e-6
N_IN = 7508
OFF = dict(u=0, v=512, gq=1024, gk=1280, gv=1536, gr=2048, ga=2560, dq=2576, dk=3088,
           dv=3600, diq=4112, dik=4368, diw=4432, gates=4436)
NEG = -30000.0
BIGNEG = -1.0e30
NBIS = 11
C_ID, C_TRI, C_ONES, C_NEGTRI, C_BKD, C_BKN, NCST = 0, 128, 256, 384, 512, 640, 768
PR_G1, PR_G2, PR_BMOD, PR_LNG, PR_LNB, PR_GNG, PR_GQ, PR_GK, PR_BBG, NPRM = 0, 8, 16, 64, 68, 72, 73, 74, 75, 99
RW_BS, RW_BG, RW_BR, NROW = 0, 512, 768, 788
WSM_GA, WSM_IQ, WSM_IK, WSM_IW, WSM_N = 0, 16, 272, 336, 340


def build_program(L, S, dbg=False, marks=False):
    NBLK = S // 128
    NG = S // 512
    nc = bass.Bass("TRN2", target_bir_lowering=False)
    P = Prog(nc)
    P.marks_on = marks
    EI = "ExternalInput"
    x_in = P.dram("x", [S, D], F32, kind=EI)
    ccol = P.dram("ccol", [128, 8], F32, kind=EI)
    cst = P.dram("cst", [128, NCST], F32, kind=EI)
    rbd = P.dram("rbrow", [1, 128], F32, kind=EI)
    w_mod = P.dram("w_mod", [L, D, 6 * D], F32, kind=EI)
    prm = P.dram("prm", [L, 128, NPRM], F32, kind=EI)
    prow = P.dram("prow", [L, 1, NROW], F32, kind=EI)
    w_in = P.dram("w_in", [L, D, N_IN], F32, kind=EI)
    wsT = P.dram("wsT", [L, 128, 4, 128], F32, kind=EI)
    w2 = P.dram("w2", [L, 16, 256], F32, kind=EI)
    w_br = P.dram("w_branch", [L, 3, 512, D], F32, kind=EI)
    w_out = P.dram("w_out", [L, D, D], F32, kind=EI)
    wr = P.dram("wr", [L, D, 20], F32, kind=EI)
    weg = P.dram("w_exp_gate", [L, 16, D, 256], F32, kind=EI)
    weu = P.dram("w_exp_up", [L, 16, D, 256], F32, kind=EI)
    wed = P.dram("w_exp_down", [L, 16, 256, D], F32, kind=EI)
    out = P.dram("out", [S, D], F32, kind="ExternalOutput", nreg=NBLK)
    gt_dram = P.dram("gt_bc", [2, 128, D], F32, nreg=2)
    w_in_b = P.dram("w_in_b", [2, D, N_IN], BF16, nreg=2)
    w_br_b = P.dram("w_br_b", [2, 3, 512, D], BF16, nreg=2)
    w_out_b = P.dram("w_out_b", [2, D, D], BF16, nreg=2)
    weg_b = P.dram("weg_b", [2, 16, D, 256], BF16, nreg=2)
    weu_b = P.dram("weu_b", [2, 16, D, 256], BF16, nreg=2)
    wed_b = P.dram("wed_b", [2, 16, 256, D], BF16, nreg=2)

    def wsrc(l):
        if l == 0:
            return ("pool", w_in[0], w_br[0], w_out[0], weg[0], weu[0], wed[0])
        p_ = l % 2
        return ("sp", w_in_b.sub(p_)[p_], w_br_b.sub(p_)[p_], w_out_b.sub(p_)[p_],
                weg_b.sub(p_)[p_], weu_b.sub(p_)[p_], wed_b.sub(p_)[p_])

    def cast_chunks(l):
        _, di, db, do, dg, du, dd = wsrc(l)
        ch = []
        for k in range(8):
            ch.append((di[k * 128:(k + 1) * 128, :], w_in[l][k * 128:(k + 1) * 128, :]))
        for n in range(3):
            for e in range(4):
                ch.append((db[n][e * 128:(e + 1) * 128, :], w_br[l][n][e * 128:(e + 1) * 128, :]))
        for k in range(8):
            ch.append((do[k * 128:(k + 1) * 128, :], w_out[l][k * 128:(k + 1) * 128, :]))
        for e in range(16):
            ch.append((dg[e].rearrange("(k p) n -> p k n", p=128), weg[l][e].rearrange("(k p) n -> p k n", p=128)))
            ch.append((du[e].rearrange("(k p) n -> p k n", p=128), weu[l][e].rearrange("(k p) n -> p k n", p=128)))
            ch.append((dd[e].rearrange("(f p) d -> p f d", p=128), wed[l][e].rearrange("(f p) d -> p f d", p=128)))
        return ch
    dbgo = {}

    def dump(name, view, shape, dtype):
        if not dbg or name in dbgo:
            return
        t = P.dram("dbg_" + name, list(shape), dtype, kind="ExternalOutput")
        dbgo[name] = t
        P.dma("sp", t.v, view)

    cf = P.sb("cf", [128, 512], F32)
    ident4 = P.sb("ident4", [128, 512], BF16)
    identb = P.sb("identb", [128, 128], BF16)
    biasT = P.sb("biasT", [128, 2, 4, 128], BF16)
    kT_all = P.sb("kT_all", [128, 4, S], BF16)
    v_all = P.sb("v_all", [128, NBLK, 4, 130], BF16)
    ikT_all = P.sb("ikT_all", [64, S], BF16)
    xg = P.sb("xg", [128, 4, D], F32)
    hT = P.sb("hT", [128, 8, 512], BF16)
    ysT = P.sb("ysT", [128, 3, 4, 512], BF16)
    WB = [P.sb(f"wb{i}", [128, 8, 512], BF16) for i in range(3)]
    wsm = P.sb("wsm", [128, 8, WSM_N], BF16)
    prm_sb = P.sb("prm_sb", [128, NPRM], F32)
    modT = P.sb("modT", [128, 48], F32)
    sT = P.sb("sT", [128, 16], F32)
    WsTm = P.sb("WsTm", [128, 4, 128], BF16)
    Cg = P.sb("Cg", [128, 4, 128], F32)
    w2_sb = P.sb("w2_sb", [16, 256], BF16)
    bg_sb = P.sb("bg_sb", [1, 256], BF16)
    ones1 = P.sb("ones1", [1, 128], BF16)
    wr_sb = P.sb("wr_sb", [128, 8, 20], F32)
    rbias = P.sb("rbias", [128, 20], F32)
    cactT = P.sb("cactT", [128, 8], BF16)
    Sf = P.sb("Sf", [64, 4, 128], F32)
    Sb = P.sb("Sb", [64, 4, 128], BF16)
    stat = P.sb("stat", [128, 64], F32)
    combb = P.sb("combb", [128, 4, 16], BF16)
    fv = P.sb("fv", [128, NBIS], F32)
    offs = P.sb("offs", [128, NBIS], F32)
    PSR = [P.ps(f"psr{i}") for i in range(6)]
    psA = P.ps("psA")
    psB = P.ps("psB")
    P.psum_rot = PSR + [psA, psB]
    ARENA = max(46 * 1024, 23552 + 8 * S)
    P.make_arena(ARENA)

    identf = cf[:, C_ID:C_ID + 128]
    trif = cf[:, C_TRI:C_TRI + 128]
    onesf = cf[:, C_ONES:C_ONES + 128]
    negtri = cf[:, C_NEGTRI:C_NEGTRI + 128]

    def wload(dst, src):
        P.dma("pool", dst, src)

    class WStream:
        def __init__(self, bufs):
            self.free = list(bufs)
            self.sched = []
            self.issued = []
            self.nissued = 0

        def push(self, fn):
            self.sched.append(fn)

        def _pump(self):
            while self.free and self.nissued < len(self.sched):
                b = self.free.pop(0)
                self.sched[self.nissued](b)
                self.nissued += 1
                self.issued.append(b)

        def get(self):
            self._pump()
            assert self.issued, "weight schedule underflow / no free buffer"
            return self.issued.pop(0)

        def release(self, b):
            self.free.append(b)
            self._pump()

    ws_main = WStream(WB)

    def sched_kxn(src2d, c0, n, q="pool"):
        ws_main.push(lambda wb: P.dma(q, wb[:, :, 0:n],
                                      src2d.rearrange("(k p) n -> p k n", p=128)[:, :, c0:c0 + n]))

    def sched_layer_mod(l):
        for i in range(12):
            sched_kxn(w_mod[l], i * 512, 512)

    def sched_group(l):
        q, s_in, s_br, s_out, s_eg, s_eu, s_ed = wsrc(l)
        for nm in ("u", "v", "gq", "gv", "gr", "dq", "dk", "dv"):
            sched_kxn(s_in, OFF[nm], 512, q)
        for n in range(3):
            ws_main.push(lambda wb, n=n: P.dma(q,
                wb.v.rearrange("p a b -> p (a b)").rearrange("p (e d) -> p e d", e=4),
                s_br[n].rearrange("(e p) d -> p e d", p=128)))
            for half in range(2):
                sched_kxn(s_in, OFF["gates"] + (n * 2 + half) * 512, 512, q)
        for half in range(2):
            sched_kxn(s_out, half * 512, 512, q)
        for e in range(16):
            def f(wb, e=e):
                P.dma(q, wb[:, :, 0:256], s_eg[e].rearrange("(k p) n -> p k n", p=128))
                P.dma(q, wb[:, :, 256:512], s_eu[e].rearrange("(k p) n -> p k n", p=128))
            ws_main.push(f)

    def next_wb():
        return ws_main.get()

    def rel_wb(b):
        ws_main.release(b)

    P.dma("sp", cf.v, cst[:, 0:512])
    bk = P.ar("bk", [128, 256], F32, 2048)
    P.dma("sp", bk.v, cst[:, 512:768])
    for i4 in range(4):
        P.dma("pool", ident4[:, i4 * 128:(i4 + 1) * 128], cst[:, C_ID:C_ID + 128])
    P.dma("pool", identb.v, cst[:, C_ID:C_ID + 128])
    P.memset(ones1.v, 1.0)
    for it in range(NBIS):
        P.memset(fv[:, it:it + 1], 0.5 ** (it + 1))
    P.memset(v_all.v, 1.0)
    rbb = P.ar("rbb", [128, 128], F32, 0)
    bacc = P.ar("bacc", [128, 128], F32, 512)
    btmp = P.ar("btmp", [128, 128], F32, 1024)
    P.dma("sp", rbb.v, rbd.v.partition_broadcast(128))
    for h in range(4):
        for dist, cb in ((0, 0), (1, 128)):
            for b in range(32):
                P.ts(btmp.v, bk[:, cb:cb + 128], float(b), None, ALU.is_equal)
                if b == 0:
                    P.ts(bacc.v, btmp.v, rbb[:, b * 4 + h:b * 4 + h + 1], None, ALU.mult)
                else:
                    P.stt(bacc.v, btmp.v, rbb[:, b * 4 + h:b * 4 + h + 1], bacc.v, ALU.mult, ALU.add)
            P.ts(biasT[:, dist, h, :], bacc.v, rbb[:, 31 * 4 + h:31 * 4 + h + 1], None, ALU.subtract)

    def small_rstd(dst, src, inv_n):
        P.ts(dst, src, inv_n, EPS, ALU.mult, ALU.add)
        P.act(dst, dst, AF.Sqrt)
        P.recip(dst, dst)

    def norm_to_hT(scol, shcol, fp32_router):
        junk = P.ar("junk", [128, D], BF16, 0)
        ssq = stat[:, 0:4]
        rstd = stat[:, 4:8]
        for bi in range(4):
            P.act(junk.v, xg[:, bi, :], AF.Square, accum=stat[:, bi:bi + 1])
        small_rstd(rstd, ssq, 1.0 / D)
        if not fp32_router:
            xns = [P.ar(f"xn{i}", [128, D], BF16, 2048 + 2048 * i) for i in range(2)]
            for bi in range(4):
                xn = xns[bi % 2]
                P.ts(xn.v, xg[:, bi, :], stat[:, 4 + bi:5 + bi], None, ALU.mult)
                ps = P.next_ps()
                psb = ps.v.bitcast(BF16)
                for k in range(8):
                    P.tr(psb[:, k * 128:(k + 1) * 128], xn[:, k * 128:(k + 1) * 128], identb.v)
                for k in range(8):
                    o = hT[:, k, bi * 128:(bi + 1) * 128]
                    if k % 2 == 0:
                        P.act(o, psb[:, k * 128:(k + 1) * 128], AF.Identity,
                              bias=shcol[:, k:k + 1], scale=scol[:, k:k + 1])
                    else:
                        P.ts(o, psb[:, k * 128:(k + 1) * 128], scol[:, k:k + 1], shcol[:, k:k + 1],
                             ALU.mult, ALU.add)
        else:
            xn32 = P.ar("xn32", [128, D], F32, 2048)
            h32 = P.ar("h32", [128, 8, 128], F32, 6144)
            lg_all = P.ar("lg_all", [128, 4, 20], F32, 10240)
            for bi in range(4):
                P.ts(xn32.v, xg[:, bi, :], stat[:, 4 + bi:5 + bi], None, ALU.mult)
                pss = [P.next_ps(), P.next_ps()]
                for k in range(8):
                    P.tr(pss[k // 4][:, (k % 4) * 128:(k % 4 + 1) * 128],
                         xn32[:, k * 128:(k + 1) * 128], identf)
                for k in range(8):
                    src = pss[k // 4][:, (k % 4) * 128:(k % 4 + 1) * 128]
                    if k % 2 == 0:
                        P.act(h32[:, k, :], src, AF.Identity, bias=shcol[:, k:k + 1], scale=scol[:, k:k + 1])
                    else:
                        P.ts(h32[:, k, :], src, scol[:, k:k + 1], shcol[:, k:k + 1], ALU.mult, ALU.add)
                P.copy(hT[:, :, bi * 128:(bi + 1) * 128], h32.v)
                psr = P.next_ps()
                for k in range(8):
                    P.mm(psr[:, 0:20], h32[:, k, :], wr_sb[:, k, :], start=(k == 0), stop=(k == 7))
                P.tt(lg_all[:, bi, :], psr[:, 0:20], rbias.v, ALU.add)
            router_all()

    def proj_tok(wb, c0, n, bi):
        ps = P.next_ps()
        for k in range(8):
            P.mm(ps[:, 0:n], hT[:, k, bi * 128:(bi + 1) * 128], wb[:, k, c0:c0 + n],
                 start=(k == 0), stop=(k == 7))
        return ps

    def proj_feat(wb, c0, m):
        ps = P.next_ps()
        for k in range(8):
            P.mm(ps[0:m, :], wb[:, k, c0:c0 + m], hT[:, k, :], start=(k == 0), stop=(k == 7))
        return ps

    def router_all():
        BIG = 1.0e30
        o = [10240 + 320]

        def fld(name, n):
            b_ = P.ar("r_" + name, [128, 4, n], F32, o[0])
            o[0] += ((4 * n * 4 + 31) // 32) * 32
            return b_

        lg_all = P.ar("lg_all", [128, 4, 20], F32, 10240)
        gmax, gsum, pg, m1, m2, dd, e21, w1, w1p, w2p = [fld(n_, 1) for n_ in
                                                         ("gmax", "gsum", "pg", "m1", "m2", "dd", "e21", "w1", "w1p", "w2p")]
        d4, ge, oh, negm = [fld(n_, 4) for n_ in ("d4", "ge", "oh", "negm")]
        elm, oh1, elm2, oh2, cmb, cmb2 = [fld(n_, 16) for n_ in ("elm", "oh1", "elm2", "oh2", "cmb", "cmb2")]
        f2 = lambda t: t.v.rearrange("p b o -> p (b o)")
        bc = lambda t, n: t.v.to_broadcast([128, 4, n])
        lgg = lg_all[:, :, 0:4]
        lge = lg_all[:, :, 4:20]
        P.reduce(f2(gmax), lgg, ALU.max)
        P.tt(d4.v, lgg, bc(gmax, 4), ALU.subtract)
        P.act(ge.v, d4.v, AF.Exp)
        P.reduce(f2(gsum), ge.v, ALU.add)
        P.recip(pg.v, gsum.v)
        P.tt(oh.v, lgg, bc(gmax, 4), ALU.is_equal)
        P.ts(negm.v, oh.v, 1.0, BIG, ALU.subtract, ALU.mult)
        P.tt(elm.v.rearrange("p b (g e) -> p b g e", g=4), lge.rearrange("p b (g e) -> p b g e", g=4),
             negm.v.rearrange("p b (g o) -> p b g o", o=1).to_broadcast([128, 4, 4, 4]), ALU.add)
        P.reduce(f2(m1), elm.v, ALU.max)
        P.tt(oh1.v, elm.v, bc(m1, 16), ALU.is_equal)
        P.stt(elm2.v, oh1.v, -BIG, elm.v, ALU.mult, ALU.add)
        P.reduce(f2(m2), elm2.v, ALU.max)
        P.tt(oh2.v, elm2.v, bc(m2, 16), ALU.is_equal)
        P.tt(dd.v, m2.v, m1.v, ALU.subtract)
        P.act(e21.v, dd.v, AF.Exp)
        P.ts(w1.v, e21.v, 1.0, None, ALU.add)
        P.recip(w1.v, w1.v)
        P.tt(w1p.v, w1.v, pg.v, ALU.mult)
        P.tt(w2p.v, w1p.v, e21.v, ALU.mult)
        P.tt(cmb.v, oh1.v, bc(w1p, 16), ALU.mult)
        P.tt(cmb2.v, oh2.v, bc(w2p, 16), ALU.mult)
        P.tt(combb.v, cmb.v, cmb2.v, ALU.add)

    for l in range(L):
        xsrc = x_in if l == 0 else out
        sched_layer_mod(l)
        for _g in range(NG):
            sched_group(l)
        P.dma("sp", prm_sb.v, prm[l])
        P.dma("pool", w2_sb.v, w2[l])
        P.dma("pool", bg_sb.v, prow[l][:, RW_BG:RW_BG + 256])
        P.dma("sp", wr_sb.v, wr[l].rearrange("(k p) n -> p k n", p=128))
        P.dma("sp", rbias.v, prow[l][:, RW_BR:RW_BR + 20].partition_broadcast(128))
        wl = w_in[l].rearrange("(k p) n -> p k n", p=128)
        wload(wsm[:, :, WSM_GA:WSM_GA + 16], wl[:, :, OFF["ga"]:OFF["ga"] + 16])
        wload(wsm[:, :, WSM_IQ:WSM_N], wl[:, :, OFF["diq"]:OFF["diq"] + 324])
        cc32 = P.ar("cc32", [128, 8], F32, 0)
        P.dma("sp", cc32.v, ccol.v)
        P.act(cactT.v, cc32.v, AF.Silu)
        psm = P.next_ps()
        for i in range(12):
            wb = next_wb()
            for jj in range(4):
                j = i * 4 + jj
                for k in range(8):
                    P.mm(psm[:, j:j + 1], wb[:, k, jj * 128:(jj + 1) * 128], cactT[:, k:k + 1],
                         start=(k == 0), stop=(k == 7))
            rel_wb(wb)
        P.tt(modT.v, psm[:, 0:48], prm_sb[:, PR_BMOD:PR_BMOD + 48], ALU.add)
        P.stt(sT[:, 0:8], modT[:, 8:16], 1.0, prm_sb[:, PR_G1:PR_G1 + 8], ALU.add, ALU.mult)
        P.stt(sT[:, 8:16], modT[:, 32:40], 1.0, prm_sb[:, PR_G2:PR_G2 + 8], ALU.add, ALU.mult)
        ws32 = P.ar("ws32", [128, 4, 128], F32, 2048)
        P.dma("sp", ws32.v, wsT[l])
        P.tt(ws32.v, ws32.v, trif.rearrange("p (o t) -> p o t", o=1).to_broadcast([128, 4, 128]), ALU.mult)
        P.copy(WsTm.v, ws32.v)
        bsrow = P.ar("bsrow", [1, 512], F32, 4096)
        P.dma("sp", bsrow.v, prow[l][:, RW_BS:RW_BS + 512])
        bsb = P.ar("bsb", [128, 4, 128], F32, 6144)
        for g in range(4):
            ps1 = P.next_ps()
            P.mm(ps1[:, 0:128], onesf[0:1, :], bsrow[:, g * 128:(g + 1) * 128])
            P.copy(bsb[:, g, :], ps1[:, 0:128], eng="act")
            P.mm(ps1[:, 128:256], onesf, ws32[:, g, :])
            P.stt(Cg[:, g, :], ps1[:, 128:256], prm_sb[:, PR_LNB + g:PR_LNB + g + 1], bsb[:, g, :],
                  ALU.mult, ALU.add)
        P.ts(stat[:, 32:33], prm_sb[:, PR_GQ:PR_GQ + 1], float(128 ** -0.5), None, ALU.mult)
        gq_col = stat[:, 32:33]
        gk_col = prm_sb[:, PR_GK:PR_GK + 1]
        gng_col = prm_sb[:, PR_GNG:PR_GNG + 1]
        P.memset(Sf.v, 0.0)
        P.memset(Sb.v, 0.0)

        def gt_bcast(col0, o_gtb, o_dg):
            gtb = P.ar("gtb", [128, D], F32, o_gtb)
            dg = P.ar("dg", [128, 128], F32, o_dg)
            pss = [P.next_ps(), P.next_ps()]
            for k in range(8):
                P.ts(dg.v, identf, modT[:, col0 + k:col0 + k + 1], None, ALU.mult)
                P.mm(pss[k // 4][:, (k % 4) * 128:(k % 4 + 1) * 128], onesf, dg.v)
            P.copy(gtb[:, 0:512], pss[0].v, eng="act")
            P.copy(gtb[:, 512:1024], pss[1].v, eng="act")
            return gtb

        for gi, col0 in enumerate((16, 40)):
            gtb0 = gt_bcast(col0, 16384, 20480)
            P.dma("sp", gt_dram.sub(gi)[gi], gtb0.v)

        def gt_load(gi, o_gtb):
            gtb_ = P.ar("gtb", [128, D], F32, o_gtb)
            P.dma("sp", gtb_.v, gt_dram.sub(gi)[gi])
            return gtb_

        for g in range(NG):
            for bi in range(4):
                gb = g * 4 + bi
                P.dma("sp", xg[:, bi, :], (xsrc if l == 0 else out.sub(gb))[gb * 128:(gb + 1) * 128, :])
            P.mark(f'g{g}_start')
            norm_to_hT(sT[:, 0:8], modT[:, 0:8], False)
            P.mark(f'g{g}_norm1')
            dump("modT", modT.v, [128, 48], F32)
            dump("hT", hT.v, [128, 8, 512], BF16)

            uT = P.ar("uT", [128, 4, 512], BF16, 8192)
            vg = P.ar("vg", [128, 4, 512], F32, 12288)
            vn = P.ar("vn", [128, 4, 512], BF16, 20480)
            gtmp = [P.ar(f"gtmp{i}", [128, 128], F32, 24576 + 512 * i) for i in range(2)]
            gjunk = P.ar("gjunk", [128, 512], BF16, 25600)
            wu = next_wb()
            for c in range(4):
                ps = proj_feat(wu, c * 128, 128)
                P.act(uT[:, c, :], ps.v, AF.Gelu_apprx_tanh)
            rel_wb(wu)
            wv = next_wb()
            for bi in range(4):
                ps = proj_tok(wv, 0, 512, bi)
                P.act(vg[:, bi, :], ps.v, AF.Gelu_apprx_tanh, accum=stat[:, 8 + bi:9 + bi])
                P.act(gjunk.v, vg[:, bi, :], AF.Square, accum=stat[:, 12 + bi:13 + bi])
            rel_wb(wv)
            mean = stat[:, 16:20]
            P.ts(mean, stat[:, 8:12], 1.0 / 512, None, ALU.mult)
            msq = stat[:, 20:24]
            P.tt(msq, mean, mean, ALU.mult)
            var = stat[:, 24:28]
            P.stt(var, stat[:, 12:16], 1.0 / 512, msq, ALU.mult, ALU.subtract)
            small_rstd(var, var, 1.0)
            for bi in range(4):
                P.ts(vn[:, bi, :], vg[:, bi, :], stat[:, 16 + bi:17 + bi], stat[:, 24 + bi:25 + bi],
                     ALU.subtract, ALU.mult)
            for bi in range(4):
                ps = P.next_ps()
                for gg in range(4):
                    P.mm(ps[:, gg * 128:(gg + 1) * 128], vn[:, bi, gg * 128:(gg + 1) * 128], WsTm[:, gg, :])
                for gg in range(4):
                    tmp = gtmp[gg % 2]
                    P.stt(tmp.v, ps[:, gg * 128:(gg + 1) * 128], prm_sb[:, PR_LNG + gg:PR_LNG + gg + 1],
                          Cg[:, gg, :], ALU.mult, ALU.add)
                    P.tt(ysT[:, 0, gg, bi * 128:(bi + 1) * 128], tmp.v, uT[:, gg, bi * 128:(bi + 1) * 128],
                         ALU.mult)

            P.mark(f'g{g}_gmlp')
            gaT = P.ar("gaT", [16, 512], BF16, 8192)
            ps = proj_feat(wsm, WSM_GA, 16)
            P.copy(gaT.v, ps[0:16, :])
            qk_all = P.ar("qk_all", [128, 4, 512], F32, 9216)
            v_allg = P.ar("v_allg", [128, 4, 512], BF16, 17408)
            r_all = P.ar("r_all", [128, 4, 512], BF16, 21504)
            l_sb = P.ar("l_sb", [128, 256], F32, 25600)
            e_sb = P.ar("e_sb", [128, 256], F32, 26624)
            eb = P.ar("eb", [128, 256], F32, 27648)
            ebp = P.ar("ebp", [128, 256], F32, 28672)
            ebl = P.ar("ebl", [128, 256], F32, 29696)
            tmpk = P.ar("tmpk", [128, 256], F32, 30720)
            qe = P.ar("qe", [128, 256], BF16, 31744)
            ke = P.ar("ke", [128, 256], BF16, 32256)
            kd = P.ar("kd", [128, 256], BF16, 32768)
            qkT = P.ar("qkT", [64, 8, 128], BF16, 33280)
            ATm = P.ar("ATm", [128, 4, 128], BF16, 35328)
            yb = P.ar("yb", [128, 512], BF16, 36352)
            Dcol = P.ar("Dcol", [64, 4], F32, 37376)
            oj = P.ar("oj", [128, 128], BF16, 37888)
            wqk = next_wb()
            for bi in range(4):
                ps = proj_tok(wqk, 0, 512, bi)
                P.copy(qk_all[:, bi, :], ps.v, eng="act")
            rel_wb(wqk)
            wgv = next_wb()
            for bi in range(4):
                ps = proj_tok(wgv, 0, 512, bi)
                P.copy(v_allg[:, bi, :], ps.v)
            rel_wb(wgv)
            wgr = next_wb()
            for bi in range(4):
                ps = proj_tok(wgr, 0, 512, bi)
                P.act(r_all[:, bi, :], ps.v, AF.Silu)
            rel_wb(wgr)
            for bi in range(4):
                bs = slice(bi * 128, (bi + 1) * 128)
                qk_sb = qk_all[:, bi, :]
                v_sb = v_allg[:, bi, :]
                r_sb = r_all[:, bi, :]
                psz = P.next_ps()
                P.mm(psz[:, 0:256], gaT[:, bs], w2_sb.v, start=True, stop=False)
                P.mm(psz[:, 0:256], ones1.v, bg_sb.v, start=False, stop=True)
                P.act(e_sb.v, psz[:, 0:256], AF.Exp, scale=-1.0)
                P.act(l_sb.v, e_sb.v, AF.Ln, bias=1.0)
                psc = P.next_ps()
                P.mm(psc[:, 0:256], trif, l_sb.v)
                P.mm(psc[:, 256:512], onesf, l_sb.v)
                psd = P.next_ps()
                for h in range(4):
                    P.mm(psd[0:64, h:h + 1], l_sb[:, h * 64:(h + 1) * 64], onesf[:, 0:1])
                P.act(eb.v, psc[:, 0:256], AF.Exp, scale=-1.0 / 16)
                P.act(ebp.v, psc[:, 0:256], AF.Exp, scale=1.0 / 16)
                P.act(ebl.v, psc[:, 256:512], AF.Exp, scale=-1.0 / 16)
                P.act(Dcol.v, psd[0:64, 0:4], AF.Exp, scale=-1.0 / 16)
                P.stt(qe.v, qk_sb[:, 0:256], 0.125, eb.v, ALU.mult, ALU.mult)
                P.tt(ke.v, qk_sb[:, 256:512], ebp.v, ALU.mult)
                P.tt(tmpk.v, ebp.v, ebl.v, ALU.mult)
                P.tt(kd.v, qk_sb[:, 256:512], tmpk.v, ALU.mult)
                pst = P.next_ps()
                pstb = pst.v.bitcast(BF16)
                for h in range(4):
                    P.tr(pstb[0:64, h * 128:(h + 1) * 128], qe[:, h * 64:(h + 1) * 64], identb.v)
                    P.tr(pstb[0:64, (4 + h) * 128:(5 + h) * 128], ke[:, h * 64:(h + 1) * 64], identb.v)
                P.copy(qkT.v.rearrange("p a b -> p (a b)"), pstb[0:64, :], eng="act")
                psa = P.next_ps()
                for h in range(4):
                    P.mm(psa[:, h * 128:(h + 1) * 128], qkT[:, 4 + h, :], qkT[:, h, :])
                P.tt(ATm.v, psa.v.rearrange("p (h t) -> p h t", h=4),
                     trif.rearrange("p (o t) -> p o t", o=1).to_broadcast([128, 4, 128]), ALU.mult)
                pso = P.next_ps()
                for h in range(4):
                    hs = slice(h * 128, (h + 1) * 128)
                    P.mm(pso[:, hs], ATm[:, h, :], v_sb[:, hs], start=True, stop=False)
                    P.mm(pso[:, hs], qkT[:, h, :], Sb[:, h, :], start=False, stop=True)
                psu = P.next_ps()
                for h in range(4):
                    P.mm(psu[0:64, h * 128:(h + 1) * 128], kd[:, h * 64:(h + 1) * 64], v_sb[:, h * 128:(h + 1) * 128])
                for h in range(4):
                    P.stt(Sf[:, h, :], Sf[:, h, :], Dcol[:, h:h + 1], psu[0:64, h * 128:(h + 1) * 128],
                          ALU.mult, ALU.add)
                P.copy(Sb.v, Sf.v)
                for h in range(4):
                    P.act(oj.v, pso[:, h * 128:(h + 1) * 128], AF.Square, accum=stat[:, 36 + h:37 + h])
                small_rstd(stat[:, 36:40], stat[:, 36:40], 1.0 / 128)
                for h in range(4):
                    hs = slice(h * 128, (h + 1) * 128)
                    P.stt(yb[:, hs], pso[:, hs], stat[:, 36 + h:37 + h], r_sb[:, hs], ALU.mult, ALU.mult)
                pst2 = P.next_ps()
                pst2b = pst2.v.bitcast(BF16)
                for h in range(4):
                    P.tr(pst2b[:, h * 128:(h + 1) * 128], yb[:, h * 128:(h + 1) * 128], identb.v)
                P.act(ysT[:, 1, :, bs], pst2b[:, 0:512].rearrange("p (h t) -> p h t", h=4), AF.Identity,
                      scale=gng_col)

            P.mark(f'g{g}_gla')
            P.psum_rot = PSR
            kn = P.ar("kn", [128, 512], BF16, 8192)
            qT = P.ar("qT", [128, 4, 512], BF16, 9216)
            iqT = P.ar("iqT", [64, 4, 512], BF16, 13312)
            iw_sb = P.ar("iw_sb", [128, 4, 4], F32, 17408)
            relu_t = [P.ar("relu0", [128, 512], F32, 17920)]
            PT = [P.ar(f"PT{i}", [128, 512], BF16, 19968 + 1024 * i) for i in range(2)]
            yc = P.ar("yc", [128, 512], BF16, 22016)
            bjunk = P.ar("bjunk", [128, 128], BF16, 23040)
            score = P.ar("score", [128, S], F32, 23552)
            maskbuf = [P.ar(f"maskbuf{i}", [128, S], BF16, 23552 + 4 * S + 2 * S * i) for i in range(2)]
            cD = max(max(1, n_ * 15 // 32) for n_ in range(1, NBLK + 1)) * 128
            cA = max(n_ - max(1, n_ * 15 // 32) for n_ in range(1, NBLK + 1)) * 128
            assert cD + cA <= S
            jDb = [P.ar(f"jD{i}", [128, cD], BF16, 23552 + 4 * S + 2 * S * i) for i in range(2)]
            jAb = [P.ar(f"jA{i}", [128, cA], BF16, 23552 + 4 * S + 2 * S * i + 2 * cD) for i in range(2)]

            def qk_norm_T(ps, gcol, dst):
                for h in range(4):
                    P.act(bjunk.v, ps[:, h * 128:(h + 1) * 128], AF.Square, accum=stat[:, 40 + h:41 + h])
                small_rstd(stat[:, 40:44], stat[:, 40:44], 1.0 / 128)
                for h in range(4):
                    P.ts(kn[:, h * 128:(h + 1) * 128], ps[:, h * 128:(h + 1) * 128], stat[:, 40 + h:41 + h],
                         None, ALU.mult)
                pt = P.next_ps()
                ptb = pt.v.bitcast(BF16)
                for h in range(4):
                    P.tr(ptb[:, h * 128:(h + 1) * 128], kn[:, h * 128:(h + 1) * 128], identb.v)
                P.act(dst, ptb[:, 0:512].rearrange("p (h t) -> p h t", h=4), AF.Identity, scale=gcol)

            for bi in range(4):
                gb = g * 4 + bi
                bs = slice(bi * 128, (bi + 1) * 128)
                psw = P.next_ps()
                for k in range(8):
                    P.mm(psw[:, 0:4], hT[:, k, bs], wsm[:, k, WSM_IW:WSM_IW + 4], start=(k == 0), stop=(k == 7))
                P.copy(iw_sb[:, bi, :], psw[:, 0:4])
            ps = proj_feat(wsm, WSM_IK, 64)
            P.copy(ikT_all[:, g * 512:(g + 1) * 512], ps[0:64, :], eng="act")
            for h in range(4):
                ps = proj_feat(wsm, WSM_IQ + h * 64, 64)
                P.copy(iqT[:, h, :], ps[0:64, :], eng="act")


            def IB_gen(bi):
                gb = g * 4 + bi
                bs = slice(bi * 128, (bi + 1) * 128)
                nk = (gb + 1) * 128
                nch = (nk + 511) // 512
                mk = maskbuf[bi % 2]
                for c in range(nch):
                    c0, c1 = c * 512, min((c + 1) * 512, nk)
                    w = c1 - c0
                    for h in range(4):
                        psi = P.next_ps()
                        P.mm(psi[:, 0:w], iqT[:, h, bs], ikT_all[:, c0:c1])
                        rl = relu_t[0]
                        P.act(rl[:, 0:w], psi[:, 0:w], AF.Relu)
                        if h == 0:
                            P.ts(score[:, c0:c1], rl[:, 0:w], iw_sb[:, bi, 0:1], None, ALU.mult)
                        else:
                            P.stt(score[:, c0:c1], rl[:, 0:w], iw_sb[:, bi, h:h + 1], score[:, c0:c1],
                                  ALU.mult, ALU.add)
                    yield
                P.tt(score[:, gb * 128:nk], score[:, gb * 128:nk], negtri, ALU.add)
                tau = stat[:, 44:45]
                if gb < 2:
                    P.memset(tau, -1.0e29)
                else:
                    rng = stat[:, 45:46]
                    mid = stat[:, 46:47]
                    cnt = stat[:, 47:48]
                    u = stat[:, 48:49]
                    hi = stat[:, 49:50]
                    lo = stat[:, 50:51]
                    P.ts(mk[:, 0:nk], score[:, 0:nk], 0.0, -3.0e38, ALU.add, ALU.max, accum=hi)
                    P.ts(mk[:, 0:256], score[:, 0:256], 0.0, 3.0e38, ALU.add, ALU.min, accum=lo)
                    P.tt(rng, hi, lo, ALU.subtract)
                    P.ts(offs.v, fv.v, rng, None, ALU.mult)
                    P.tt(mid, lo, offs[:, 0:1], ALU.add)
                    for it in range(NBIS):
                        P.ts(mk[:, 0:nk], score[:, 0:nk], mid, 0.0, ALU.is_ge, ALU.add, accum=cnt)
                        last = (it == NBIS - 1)
                        P.ts(u, cnt, 256.0, 1.0 if last else 0.5, ALU.is_ge, ALU.subtract)
                        P.stt(tau if last else mid, u, offs[:, it:it + 1], mid, ALU.mult, ALU.add)
                        yield
                P.ts(mk[:, 0:nk], score[:, 0:nk], tau, NEG, ALU.is_lt, ALU.mult)
                yield

            def drain(gen):
                for _ in gen:
                    pass

            def interleave(ga, gb_):
                a_done = b_done = False
                while not (a_done and b_done):
                    if not a_done:
                        try:
                            next(ga)
                        except StopIteration:
                            a_done = True
                    if not b_done:
                        try:
                            next(gb_)
                        except StopIteration:
                            b_done = True

            def ATT_gen(bi):
                gb = g * 4 + bi
                bs = slice(bi * 128, (bi + 1) * 128)
                mk = maskbuf[bi % 2]
                P.memset(psA.v, 0.0)
                P.memset(psB.v, 0.0)
                def LG(j):
                    psl = P.next_ps()
                    dist = gb - j
                    P.mm(psl.v, mk[:, j * 128:(j + 1) * 128], ident4.v, start=True, stop=False)
                    if dist <= 1:
                        P.mm(psl.v, identb.v, biasT[:, dist, :, :].rearrange("p h t -> p (h t)"),
                             start=False, stop=False)
                    for h in range(4):
                        hs = slice(h * 128, (h + 1) * 128)
                        P.mm(psl[:, hs], kT_all[:, h, j * 128:(j + 1) * 128], qT[:, h, bs],
                             start=False, stop=(h == 3))
                    return psl

                psl = LG(0)
                P.act(PT[0].v, psl.v, AF.Exp, bias=-6.0)
                for j in range(gb + 1):
                    pt = PT[j % 2]
                    if j + 1 <= gb:
                        psl_next = LG(j + 1)
                    yield
                    for h in range(4):
                        acc = psA if h < 2 else psB
                        a0 = (h % 2) * 130
                        P.mm(acc[:, a0:a0 + 129], pt[:, h * 128:(h + 1) * 128], v_all[:, j, h, 0:129],
                             start=False, stop=False, skip=True)
                    if j + 1 <= gb:
                        P.act(PT[(j + 1) % 2].v, psl_next.v, AF.Exp, bias=-6.0)
                for h in range(4):
                    acc = psA if h < 2 else psB
                    a0 = (h % 2) * 130
                    P.recip(stat[:, 52 + h:53 + h], acc[:, a0 + 128:a0 + 129])
                    P.ts(yc[:, h * 128:(h + 1) * 128], acc[:, a0:a0 + 128], stat[:, 52 + h:53 + h], None, ALU.mult)
                pt2 = P.next_ps()
                pt2b = pt2.v.bitcast(BF16)
                for h in range(4):
                    P.tr(pt2b[:, h * 128:(h + 1) * 128], yc[:, h * 128:(h + 1) * 128], identb.v)
                P.copy(ysT[:, 2, :, bs], pt2b[:, 0:512].rearrange("p (h t) -> p h t", h=4), eng="act")

            def projA_gen():
                wdq = next_wb()
                for bi in range(4):
                    bs = slice(bi * 128, (bi + 1) * 128)
                    ps = proj_tok(wdq, 0, 512, bi)
                    qk_norm_T(ps, gq_col, qT[:, :, bs])
                    yield
                rel_wb(wdq)
                wdk = next_wb()
                for bi in range(4):
                    gb = g * 4 + bi
                    ps = proj_tok(wdk, 0, 512, bi)
                    qk_norm_T(ps, gk_col, kT_all[:, :, gb * 128:(gb + 1) * 128])
                    yield
                rel_wb(wdk)
                wdv = next_wb()
                for bi in range(4):
                    gb = g * 4 + bi
                    ps = proj_tok(wdv, 0, 512, bi)
                    P.copy(v_all[:, gb, :, 0:128], ps.v.rearrange("p (h e) -> p h e", h=4))
                    yield
                rel_wb(wdv)

            interleave(projA_gen(), IB_gen(0))
            if l + 1 < L:
                chs = cast_chunks(l + 1)
                per = (len(chs) + NG - 1) // NG
                for dst_, src_ in chs[g * per:(g + 1) * per]:
                    P.dma("pool", dst_, src_)
            P.mark(f'g{g}_dsaproj')
            for bi in range(4):
                if bi + 1 < 4:
                    interleave(ATT_gen(bi), IB_gen(bi + 1))
                else:
                    drain(ATT_gen(bi))

            P.mark(f'g{g}_dsa')
            dump("ysT", ysT.v, [128, 3, 4, 512], BF16)
            dump("kT", kT_all[:, :, 0:512], [128, 4, 512], BF16)
            P.psum_rot = PSR + [psA, psB]
            gtb = gt_load(0, 40960)
            mergedF = P.ar("mergedF", [128, 8, 512], F32, 8192)
            mergedT = P.ar("mergedT", [128, 8, 512], BF16, 24576)
            sgt = [P.ar(f"sgt{i}", [128, 512], BF16, 32768 + 1024 * i) for i in range(2)]
            mtmp = [P.ar(f"mtmp{i}", [128, 512], F32, 34816 + 2048 * i) for i in range(2)]
            ci = 0
            for n in range(3):
                wbr = next_wb()
                wbrv = wbr.v.rearrange("p a b -> p (a b)").rearrange("p (e d) -> p e d", e=4)
                for half in range(2):
                    wg = next_wb()
                    for cc in range(4):
                        dc = half * 4 + cc
                        psg = proj_feat(wg, cc * 128, 128)
                        sg = sgt[ci % 2]
                        P.act(sg.v, psg.v, AF.Sigmoid, bias=prm_sb[:, PR_BBG + n * 8 + dc:PR_BBG + n * 8 + dc + 1])
                        psu = P.next_ps()
                        for ec in range(4):
                            P.mm(psu.v, wbrv[:, ec, dc * 128:(dc + 1) * 128], ysT[:, n, ec, :],
                                 start=(ec == 0), stop=(ec == 3))
                        if n == 0:
                            P.tt(mergedF[:, dc, :], psu.v, sg.v, ALU.mult)
                        else:
                            mt = mtmp[ci % 2]
                            P.tt(mt.v, psu.v, sg.v, ALU.mult)
                            if n == 1:
                                P.tt(mergedF[:, dc, :], mergedF[:, dc, :], mt.v, ALU.add)
                            else:
                                P.tt(mergedT[:, dc, :], mergedF[:, dc, :], mt.v, ALU.add)
                        ci += 1
                    rel_wb(wg)
                rel_wb(wbr)
            P.mark(f'g{g}_merge')
            for half in range(2):
                wo = next_wb()
                for bi in range(4):
                    ps = P.next_ps()
                    for k in range(8):
                        P.mm(ps.v, mergedT[:, k, bi * 128:(bi + 1) * 128], wo[:, k, :], start=(k == 0), stop=(k == 7))
                    mt = mtmp[bi % 2]
                    P.tt(mt.v, ps.v, gtb[:, half * 512:(half + 1) * 512], ALU.mult)
                    P.tt(xg[:, bi, half * 512:(half + 1) * 512], xg[:, bi, half * 512:(half + 1) * 512], mt.v,
                         ALU.add)
                rel_wb(wo)

            dump("mergedT", mergedT.v, [128, 8, 512], BF16)
            dump("x1", xg.v, [128, 4, D], F32)
            P.mark(f'g{g}_wout')
            gtb = gt_load(1, 16384)
            norm_to_hT(sT[:, 8:16], modT[:, 24:32], True)
            dump("h2T", hT.v, [128, 8, 512], BF16)
            dump("combb", combb.v, [128, 4, 16], BF16)
            P.mark(f'g{g}_norm2')
            cb_sb = P.ar("cb_sb", [128, 512], BF16, 0)
            t1 = [P.ar("t1", [128, 512], BF16, 1024)]
            sgm = [P.ar(f"sgm{i}", [128, 512], BF16, 2048 + 1024 * i) for i in range(2)]
            actT = P.ar("actT", [128, 4, 2, 512], BF16, 4096)
            mtmp = [P.ar(f"mtmpb{i}", [128, 512], F32, 12288 + 2048 * i) for i in range(2)]
            WD = [P.ar(f"wd{i}", [128, 2, D], BF16, 21504 + 4096 * i) for i in range(6)]
            wdi = 0
            for rnd in range(4):
                wds = []
                for ee in range(4):
                    e = rnd * 4 + ee
                    wgu = next_wb()
                    wd = WD[wdi % 6]
                    wdi += 1
                    P.dma(wsrc(l)[0], wd.v, wsrc(l)[6][e].rearrange("(f p) d -> p f d", p=128))
                    wds.append(wd)
                    psc = P.next_ps()
                    for bi in range(4):
                        P.mm(psc[:, bi * 128:(bi + 1) * 128], combb[:, bi, e:e + 1].to_broadcast([128, 128]),
                             identb.v)
                    P.copy(cb_sb.v, psc.v, eng="act")
                    for fc in range(2):
                        psg = proj_feat(wgu, fc * 128, 128)
                        psu = proj_feat(wgu, 256 + fc * 128, 128)
                        sg = sgm[fc]
                        P.act(sg.v, psg.v, AF.Silu)
                        P.tt(t1[0].v, psu.v, sg.v, ALU.mult)
                        P.tt(actT[:, ee, fc, :], t1[0].v, cb_sb.v, ALU.mult)
                    rel_wb(wgu)
                for bi in range(4):
                    for half in range(2):
                        ps = P.next_ps()
                        n = 0
                        for ee in range(4):
                            for fc in range(2):
                                P.mm(ps.v, actT[:, ee, fc, bi * 128:(bi + 1) * 128],
                                     wds[ee][:, fc, half * 512:(half + 1) * 512], start=(n == 0), stop=(n == 7))
                                n += 1
                        mt = mtmp[(bi * 2 + half) % 2]
                        P.tt(mt.v, ps.v, gtb[:, half * 512:(half + 1) * 512], ALU.mult)
                        P.tt(xg[:, bi, half * 512:(half + 1) * 512], xg[:, bi, half * 512:(half + 1) * 512], mt.v,
                             ALU.add)
            P.mark(f'g{g}_moe')
            for bi in range(4):
                gb = g * 4 + bi
                P.dma("sp", out.sub(gb)[gb * 128:(gb + 1) * 128, :], xg[:, bi, :])
    P.fence("sp", [out] + list(dbgo.values()))
    stats = P.emit()
    stats['marks'] = getattr(P, 'marks', [])
    return nc, stats


def _bucket_np(rel):
    import math
    n = np.maximum(rel, 0)
    max_exact = 16
    large = max_exact + (np.log(np.maximum(n, max_exact).astype(np.float32) / max_exact)
                         / math.log(128 / max_exact) * (32 - max_exact)).astype(np.int32)
    large = np.minimum(large, 31)
    return np.where(n < max_exact, n, large)


def host_consts():
    cst = np.zeros((128, NCST), np.float32)
    i = np.arange(128)
    cst[:, C_ID:C_ID + 128] = np.eye(128, dtype=np.float32)
    cst[:, C_TRI:C_TRI + 128] = (i[:, None] <= i[None, :]).astype(np.float32)
    cst[:, C_ONES:C_ONES + 128] = 1.0
    cst[:, C_NEGTRI:C_NEGTRI + 128] = np.where(i[None, :] <= i[:, None], 0.0, BIGNEG)
    cst[:, C_BKD:C_BKD + 128] = _bucket_np(i[None, :] - i[:, None]).astype(np.float32)
    cst[:, C_BKN:C_BKN + 128] = _bucket_np(128 + i[None, :] - i[:, None]).astype(np.float32)
    return cst


def host_layout(inputs, L, b, S):
    f = lambda a: np.ascontiguousarray(np.asarray(a, dtype=np.float32))
    col = lambda v: np.asarray(v, np.float32).reshape(-1, 128).T
    m = {}
    m["x"] = f(inputs["x"][b, :S])
    m["ccol"] = f(col(inputs["c"][b]))
    m["cst"] = host_consts()
    m["rbrow"] = f(np.asarray(inputs["rel_bias"]).reshape(1, 128))
    prm = np.zeros((L, 128, NPRM), np.float32)
    prow = np.zeros((L, 1, NROW), np.float32)
    for l in range(L):
        prm[l, :, PR_G1:PR_G1 + 8] = col(inputs["g_norm1"][l])
        prm[l, :, PR_G2:PR_G2 + 8] = col(inputs["g_norm2"][l])
        prm[l, :, PR_BMOD:PR_BMOD + 48] = col(inputs["b_mod"][l])
        prm[l, :, PR_LNG:PR_LNG + 4] = col(inputs["gmlp_ln_g"][l])
        prm[l, :, PR_LNB:PR_LNB + 4] = col(inputs["gmlp_ln_b"][l])
        prm[l, :, PR_GNG] = np.asarray(inputs["gla_norm_g"][l])
        prm[l, :, PR_GQ] = np.asarray(inputs["dsa_qnorm_g"][l])
        prm[l, :, PR_GK] = np.asarray(inputs["dsa_knorm_g"][l])
        prm[l, :, PR_BBG:PR_BBG + 24] = col(np.asarray(inputs["b_branch_gate"][l]).reshape(-1))
        prow[l, 0, RW_BS:RW_BS + 512] = np.asarray(inputs["gmlp_b_s"][l]).reshape(-1)
        prow[l, 0, RW_BG:RW_BG + 256] = np.asarray(inputs["gla_b_gate"][l])
        prow[l, 0, RW_BR:RW_BR + 4] = np.asarray(inputs["b_group"][l])
        prow[l, 0, RW_BR + 4:RW_BR + 20] = np.asarray(inputs["b_router"][l])
    m["prm"] = prm
    m["prow"] = prow
    m["w_mod"] = f(inputs["w_mod"][:L])
    m["w_in"] = f(inputs["w_in"][:L])
    m["wsT"] = f(np.asarray(inputs["gmlp_w_s"][:L]).transpose(0, 3, 1, 2))
    m["w2"] = f(inputs["gla_w_gate2"][:L])
    m["w_branch"] = f(inputs["w_branch"][:L])
    m["w_out"] = f(inputs["w_out"][:L])
    m["wr"] = f(np.concatenate([np.asarray(inputs["w_group"][:L]), np.asarray(inputs["w_router"][:L])], axis=2))
    m["w_exp_gate"] = f(inputs["w_exp_gate"][:L])
    m["w_exp_up"] = f(inputs["w_exp_up"][:L])
    m["w_exp_down"] = f(inputs["w_exp_down"][:L])
    return m


_PROG_CACHE = {}


def run_model(inputs, L, S, batches, n_cores, want_all=False, active=None):
    key = (L, S)
    if key not in _PROG_CACHE:
        _PROG_CACHE[key] = build_program(L, S)
    nc, stats = _PROG_CACHE[key]
    if active is None:
        active = list(range(min(n_cores, len(batches))))
    shared = host_layout(inputs, L, batches[0], S)
    zeros = None
    in_maps = []
    for ci in range(n_cores):
        if ci in active:
            b = batches[active.index(ci)]
            m = dict(shared)
            m["x"] = np.ascontiguousarray(np.asarray(inputs["x"][b, :S], dtype=np.float32))
            m["ccol"] = np.ascontiguousarray(np.asarray(inputs["c"][b], np.float32).reshape(-1, 128).T)
        else:
            if zeros is None:
                zeros = {k: np.zeros_like(v) for k, v in shared.items()}
            m = dict(zeros)
        in_maps.append(m)
    res = run_bass_kernel_spmd(nc, in_maps, core_ids=list(range(n_cores)))
    if want_all:
        return res.results
    return [res.results[ci]["out"] for ci in active]


def kernel(**inputs):
    L, B, S = 4, 4, 4096
    outs = run_model(inputs, L, S, list(range(B)), 8, active=[0, 1, 4, 5])
    return np.stack(outs, axis=0).astype(np.float32)
```
